# Optimizing a Trainium2 kernel written in Bass

```python
import math
import jax
import jax.numpy as jnp
from jax import lax
import numpy as np

D_MODEL = 1024
BATCH = 4
SEQ = 8192
DEPTH = 1

DILATED_GROUPS = ((128, 1), (512, 4), (2048, 16))
N_GROUPS_A = len(DILATED_GROUPS)
HEADS_PER_GROUP_A = 4
HEAD_DIM_A = 128
N_HEADS_A = N_GROUPS_A * HEADS_PER_GROUP_A
WIDTH_A = HEADS_PER_GROUP_A * HEAD_DIM_A
NUM_BUCKETS = 32
MAX_DISTANCE = 2048
HGRN_HEAD_DIM = 128
N_HEADS_B = D_MODEL // HGRN_HEAD_DIM
WIDTH_B = N_HEADS_B * HGRN_HEAD_DIM
CHUNK_B = 64
MEM_LEN = 256
N_HEADS_C = 4
HEAD_DIM_C = 128
WIDTH_C = N_HEADS_C * HEAD_DIM_C
N_BRANCHES = 3
COLS_A = N_GROUPS_A * 3 * WIDTH_A
COLS_B = 4 * WIDTH_B
COLS_C = WIDTH_C
COLS_GATE = N_BRANCHES * D_MODEL
N_IN = COLS_A + COLS_B + COLS_C + COLS_GATE
N_EXPERT_GROUPS = 4
EXPERTS_PER_GROUP = 8
N_EXPERTS = N_EXPERT_GROUPS * EXPERTS_PER_GROUP
TOP_K_INNER = 2
D_EXPERT = D_MODEL // 4
MOE_BLOCK = 128
DN_ALPHA = (2 * DEPTH) ** 0.25
DN_BETA = (8 * DEPTH) ** -0.25
LN_EPS = 1e-5
RMS_EPS = 1e-6

kernel_name = 'hybrid_dilated_hgrn2_mem_hmoe_block'


def _layer_norm(x, g, b):
    xf = x.astype(jnp.float32)
    mu = jnp.mean(xf, axis=-1, keepdims=True)
    var = jnp.mean(jnp.square(xf - mu), axis=-1, keepdims=True)
    return ((xf - mu) * lax.rsqrt(var + LN_EPS) * g.astype(jnp.float32) + b.astype(jnp.float32)).astype(x.dtype)


def _t5_bucket_np(dist):
    dist = np.asarray(dist, np.int32)
    max_exact = NUM_BUCKETS // 2
    d = np.maximum(dist, 1).astype(np.float32)
    large = max_exact + (np.log(d / max_exact) / math.log(MAX_DISTANCE / max_exact) * (NUM_BUCKETS - max_exact)).astype(np.int32)
    large = np.minimum(large, NUM_BUCKETS - 1)
    return np.where(dist < max_exact, dist, large).astype(np.int32)


def _dilated_band_attention(q, k, v, bias_tab, window, dilation):
    B, S, H, Dh = q.shape
    band = window // dilation
    L = S // dilation
    nb = -(-L // band)
    Lp = nb * band

    def to_blocks(t):
        t = t.reshape(B, L, dilation, H, Dh).transpose(0, 2, 1, 3, 4)
        t = jnp.pad(t, ((0, 0), (0, 0), (0, Lp - L), (0, 0), (0, 0)))
        return t.reshape(B, dilation, nb, band, H, Dh)

    def with_prev(t):
        prev = jnp.pad(t, ((0, 0), (0, 0), (1, 0), (0, 0), (0, 0), (0, 0)))[:, :, :-1]
        return jnp.concatenate([prev, t], axis=3)

    qb = to_blocks(q)
    kc = with_prev(to_blocks(k))
    vc = with_prev(to_blocks(v))

    i = np.arange(band)[:, None]
    j = np.arange(2 * band)[None, :]
    u = i + band - j
    in_band = (u >= 0) & (u <= band)
    first_block = (np.arange(nb)[:, None, None] == 0) & (j[None] < band)
    valid = in_band[None] & ~first_block
    bucket = _t5_bucket_np(np.clip(u, 0, band) * dilation)
    bias = jnp.take(bias_tab, jnp.asarray(bucket), axis=0).astype(jnp.float32).transpose(2, 0, 1)

    s = jnp.einsum('bcnihd,bcnjhd->bcnhij', qb, kc).astype(jnp.float32) * (Dh ** -0.5)
    s = jnp.where(valid[None, None, :, None], s + bias[None, None, None], -jnp.inf)
    m = jnp.max(s, axis=-1, keepdims=True)
    p = jnp.exp(s - m)
    l = jnp.sum(p, axis=-1, keepdims=True)
    o = jnp.einsum('bcnhij,bcnjhd->bcnihd', p, vc.astype(jnp.float32))
    o = o / l.transpose(0, 1, 2, 4, 3, 5)
    lse = (m + jnp.log(l))[..., 0].transpose(0, 1, 2, 4, 3)

    def from_blocks(t):
        t = t.reshape((B, dilation, Lp) + t.shape[4:])[:, :, :L]
        t = jnp.swapaxes(t, 1, 2)
        return t.reshape((B, S) + t.shape[3:])

    return from_blocks(o), from_blocks(lse)


def _hgrn2(q, f_raw, i_in, g, lb, norm_g):
    B, S, H, Dk = q.shape
    n_chunks = S // CHUNK_B
    qf = jax.nn.silu(q.astype(jnp.float32))
    f = lb + (1.0 - lb) * jax.nn.sigmoid(f_raw.astype(jnp.float32))
    logf = jnp.log(f)
    kf = 1.0 - f
    vf = i_in.astype(jnp.float32)

    def chunks(t):
        return t.reshape(B, n_chunks, CHUNK_B, H, t.shape[-1]).transpose(1, 0, 3, 2, 4)

    qc, kc, vc, lc = chunks(qf), chunks(kf), chunks(vf), chunks(logf)
    bc = jnp.cumsum(lc, axis=3)
    causal = np.tril(np.ones((CHUNK_B, CHUNK_B), dtype=bool))

    def step(state, inp):
        qt, kt, vt, bt = inp
        o_inter = jnp.einsum('bhtk,bhkv->bhtv', qt * jnp.exp(bt), state)
        diff = jnp.where(causal[:, :, None], bt[:, :, :, None, :] - bt[:, :, None, :, :], -jnp.inf)
        att = jnp.einsum('bhtk,bhsk,bhtsk->bhts', qt, kt, jnp.exp(diff))
        o_intra = jnp.einsum('bhts,bhsv->bhtv', att, vt)
        b_last = bt[:, :, -1:, :]
        new_state = jnp.exp(b_last[:, :, 0, :, None]) * state + jnp.einsum('bhsk,bhsv->bhkv', kt * jnp.exp(b_last - bt), vt)
        return new_state, o_inter + o_intra

    s0 = jnp.zeros((B, H, Dk, vf.shape[-1]), jnp.float32)
    _, o = lax.scan(step, s0, (qc, kc, vc, bc))
    o = o.transpose(1, 0, 3, 2, 4).reshape(B, S, H, -1)
    o = o * lax.rsqrt(jnp.mean(o * o, axis=-1, keepdims=True) + RMS_EPS)
    o = o.reshape(B, S, -1) * norm_g.astype(jnp.float32) * jax.nn.silu(g.reshape(B, S, -1).astype(jnp.float32))
    return o.astype(q.dtype)


def _memory_attention(q, mem, w_kv):
    B, S = q.shape[0], q.shape[1]
    M = mem.shape[1]
    kv = (mem @ w_kv).reshape(B, M, 2, N_HEADS_C, HEAD_DIM_C)
    k, v = kv[:, :, 0], kv[:, :, 1]
    s = jnp.einsum('bshd,bmhd->bhsm', q, k).astype(jnp.float32) * (HEAD_DIM_C ** -0.5)
    p = jax.nn.softmax(s, axis=-1)
    o = jnp.einsum('bhsm,bmhd->bshd', p, v.astype(jnp.float32))
    return o.reshape(B, S, WIDTH_C).astype(q.dtype)


def _hier_moe(h, w_rg, b_rg, w_re, b_re, w_gate, w_up, w_down):
    B, S, D = h.shape
    T = B * S
    ht = h.reshape(T, D)
    glog = (ht @ w_rg).astype(jnp.float32) + b_rg.astype(jnp.float32)
    gprob = jax.nn.softmax(glog, axis=-1)
    gsel = jnp.argmax(glog, axis=-1)
    elog = ((ht @ w_re).astype(jnp.float32) + b_re.astype(jnp.float32)).reshape(T, N_EXPERT_GROUPS, EXPERTS_PER_GROUP)
    elog_sel = elog[jnp.arange(T), gsel]
    top_v, top_i = lax.top_k(elog_sel, TOP_K_INNER)
    weights = jax.nn.softmax(top_v, axis=-1) * jnp.take_along_axis(gprob, gsel[:, None], axis=1)
    eid = gsel[:, None] * EXPERTS_PER_GROUP + top_i

    M = T * TOP_K_INNER
    e_flat = eid.reshape(M)
    w_flat = weights.reshape(M)
    tok = jnp.arange(M) // TOP_K_INNER
    order = jnp.argsort(e_flat)
    e_s, tok_s, w_s = e_flat[order], tok[order], w_flat[order]
    counts = jnp.bincount(e_flat, length=N_EXPERTS)
    starts = jnp.cumsum(counts) - counts
    pcounts = (counts + MOE_BLOCK - 1) // MOE_BLOCK * MOE_BLOCK
    pends = jnp.cumsum(pcounts)
    pstarts = pends - pcounts
    dest = pstarts[e_s] + (jnp.arange(M) - starts[e_s])
    n_blocks = (M + N_EXPERTS * (MOE_BLOCK - 1) + MOE_BLOCK - 1) // MOE_BLOCK
    P = n_blocks * MOE_BLOCK
    xbuf = jnp.zeros((P, D), h.dtype).at[dest].set(ht[tok_s])
    block_e = jnp.minimum(jnp.searchsorted(pends, jnp.arange(n_blocks) * MOE_BLOCK, side='right'), N_EXPERTS - 1)

    def expert_block(args):
        xb, e = args
        return (jax.nn.silu(xb @ w_gate[e]) * (xb @ w_up[e])) @ w_down[e]

    ybuf = lax.map(expert_block, (xbuf.reshape(n_blocks, MOE_BLOCK, D), block_e)).reshape(P, D)
    y = jnp.zeros((T, D), h.dtype).at[tok_s].add(w_s[:, None].astype(h.dtype) * ybuf[dest])
    return y.reshape(B, S, D)


def setup_inputs(seed: int = 0) -> dict:
    key = jax.random.key(seed)
    ks = jax.random.split(key, 24)

    def nrm(k, shape, scale):
        return jax.random.normal(k, shape, jnp.float32) * scale

    scale_a = np.ones((N_GROUPS_A, 3, WIDTH_A), np.float32)
    scale_a[:, 2] = DN_BETA
    scale_b = np.ones((4, WIDTH_B), np.float32)
    scale_b[2] = DN_BETA
    col_scale = jnp.asarray(np.concatenate([scale_a.ravel(), scale_b.ravel(), np.ones(COLS_C + COLS_GATE, np.float32)]))
    kv_scale = jnp.asarray(np.concatenate([np.ones(WIDTH_C, np.float32), np.full(WIDTH_C, DN_BETA, np.float32)]))
    return {
        'x': nrm(ks[0], (BATCH, SEQ, D_MODEL), 1.0),
        'mem': nrm(ks[1], (BATCH, MEM_LEN, D_MODEL), 1.0),
        'rel_bias': nrm(ks[2], (NUM_BUCKETS, N_HEADS_A), 0.2),
        'hgrn_lb_logits': nrm(ks[3], (DEPTH + 1, WIDTH_B), 1.0),
        'w_in': nrm(ks[4], (DEPTH, D_MODEL, N_IN), D_MODEL ** -0.5) * col_scale,
        'w_mem_kv': nrm(ks[5], (DEPTH, D_MODEL, 2 * WIDTH_C), D_MODEL ** -0.5) * kv_scale,
        'hgrn_norm_g': 1.0 + nrm(ks[6], (DEPTH, WIDTH_B), 0.02),
        'w_branch_a': nrm(ks[7], (DEPTH, WIDTH_A, D_MODEL), WIDTH_A ** -0.5 * DN_BETA),
        'w_branch_b': nrm(ks[8], (DEPTH, WIDTH_B, D_MODEL), WIDTH_B ** -0.5 * DN_BETA),
        'w_branch_c': nrm(ks[9], (DEPTH, WIDTH_C, D_MODEL), WIDTH_C ** -0.5 * DN_BETA),
        'w_out': nrm(ks[10], (DEPTH, D_MODEL, D_MODEL), D_MODEL ** -0.5 * DN_BETA),
        'ln1_g': 1.0 + nrm(ks[11], (DEPTH, D_MODEL), 0.02),
        'ln1_b': nrm(ks[12], (DEPTH, D_MODEL), 0.02),
        'w_router_group': nrm(ks[13], (DEPTH, D_MODEL, N_EXPERT_GROUPS), D_MODEL ** -0.5),
        'b_router_group': nrm(ks[14], (DEPTH, N_EXPERT_GROUPS), 0.01),
        'w_router_expert': nrm(ks[15], (DEPTH, D_MODEL, N_EXPERTS), D_MODEL ** -0.5),
        'b_router_expert': nrm(ks[16], (DEPTH, N_EXPERTS), 0.01),
        'w_exp_gate': nrm(ks[17], (DEPTH, N_EXPERTS, D_MODEL, D_EXPERT), D_MODEL ** -0.5 * DN_BETA),
        'w_exp_up': nrm(ks[18], (DEPTH, N_EXPERTS, D_MODEL, D_EXPERT), D_MODEL ** -0.5 * DN_BETA),
        'w_exp_down': nrm(ks[19], (DEPTH, N_EXPERTS, D_EXPERT, D_MODEL), D_EXPERT ** -0.5 * DN_BETA),
        'ln2_g': 1.0 + nrm(ks[20], (DEPTH, D_MODEL), 0.02),
        'ln2_b': nrm(ks[21], (DEPTH, D_MODEL), 0.02),
    }


def reference(x, mem, rel_bias, hgrn_lb_logits, w_in, w_mem_kv, hgrn_norm_g, w_branch_a, w_branch_b, w_branch_c, w_out, ln1_g, ln1_b, w_router_group, b_router_group, w_router_expert, b_router_expert, w_exp_gate, w_exp_up, w_exp_down, ln2_g, ln2_b):
    B, S, D = x.shape
    lower_bounds = jnp.cumsum(jax.nn.softmax(hgrn_lb_logits.astype(jnp.float32), axis=0), axis=0)
    for l in range(DEPTH):
        proj = x @ w_in[l]
        pa = proj[..., :COLS_A].reshape(B, S, N_GROUPS_A, 3, HEADS_PER_GROUP_A, HEAD_DIM_A)
        pb = proj[..., COLS_A:COLS_A + COLS_B].reshape(B, S, 4, N_HEADS_B, HGRN_HEAD_DIM)
        pc = proj[..., COLS_A + COLS_B:COLS_A + COLS_B + COLS_C].reshape(B, S, N_HEADS_C, HEAD_DIM_C)
        pg = proj[..., COLS_A + COLS_B + COLS_C:].reshape(B, S, N_BRANCHES, D)

        outs, lses = [], []
        for g, (window, dilation) in enumerate(DILATED_GROUPS):
            o_g, lse_g = _dilated_band_attention(pa[:, :, g, 0], pa[:, :, g, 1], pa[:, :, g, 2], rel_bias[:, g * HEADS_PER_GROUP_A:(g + 1) * HEADS_PER_GROUP_A], window, dilation)
            outs.append(o_g)
            lses.append(lse_g)
        mix_w = jax.nn.softmax(jnp.stack(lses), axis=0)[..., None]
        o_a = jnp.sum(mix_w * jnp.stack(outs), axis=0).reshape(B, S, WIDTH_A).astype(x.dtype)

        o_b = _hgrn2(pb[:, :, 0], pb[:, :, 1], pb[:, :, 2], pb[:, :, 3], lower_bounds[l].reshape(N_HEADS_B, HGRN_HEAD_DIM), hgrn_norm_g[l])

        o_c = _memory_attention(pc, mem, w_mem_kv[l])

        gates = jax.nn.sigmoid(pg.astype(jnp.float32)).astype(x.dtype)
        merged = gates[:, :, 0] * (o_a @ w_branch_a[l]) + gates[:, :, 1] * (o_b @ w_branch_b[l]) + gates[:, :, 2] * (o_c @ w_branch_c[l])
        x = _layer_norm(DN_ALPHA * x + merged @ w_out[l], ln1_g[l], ln1_b[l])

        y = _hier_moe(x, w_router_group[l], b_router_group[l], w_router_expert[l], b_router_expert[l], w_exp_gate[l], w_exp_up[l], w_exp_down[l])
        x = _layer_norm(DN_ALPHA * x + y, ln2_g[l], ln2_b[l])
    return x
```

```python
import math
import numpy as np
from contextlib import ExitStack
import concourse.bass as bass
import concourse.mybir as mybir
from concourse.bass_utils import run_bass_kernel_spmd

F32 = mybir.dt.float32
BF16 = mybir.dt.bfloat16
I32 = mybir.dt.int32
ALU = mybir.AluOpType
AF = mybir.ActivationFunctionType
AX = mybir.AxisListType

SEM_EPOCH = 4000
DMA_RING = 8
DMA_EPOCH = 200


class Op:
    __slots__ = ("stream", "fn", "deps", "is_dma", "signal", "sig", "know", "oid")

    def __init__(self, stream, fn, is_dma):
        self.stream = stream
        self.fn = fn
        self.deps = set()
        self.is_dma = is_dma
        self.signal = is_dma
        self.sig = None
        self.know = None


class Sched:
    def __init__(self, nc, same_engine_sync=True):
        self.nc = nc
        self.ops = []
        self.last_w = {}
        self.readers = {}
        self.same_engine_sync = same_engine_sync
        self.last_op = {}
        self.dmas = []
        self._cap = None

    def capture(self):
        self._cap = []

    def end_capture(self):
        c = self._cap
        self._cap = None
        return c

    def replay(self, lists):
        lists = [l for l in lists if l]
        idx = [0] * len(lists)
        while True:
            best = None
            for i, l in enumerate(lists):
                if idx[i] < len(l):
                    frac = idx[i] / len(l)
                    if best is None or frac < best[0]:
                        best = (frac, i)
            if best is None:
                break
            i = best[1]
            stream, fn, reads, writes, dma, nobar = lists[i][idx[i]]
            idx[i] += 1
            self.add(stream, fn, reads, writes, dma, nobar)

    def barrier(self):
        deps = set(v for v in self.last_op.values())
        deps |= set(self.dmas)
        self.dmas = []
        keep = dict(self.last_op)
        for s_ in ("pe", "act", "dve", "pool", "sp"):
            op = self.add(s_, lambda e: None)
            op.deps |= deps
        self.last_op = keep

    def add(self, stream, fn, reads=(), writes=(), dma=False, nobar=False):
        if self._cap is not None:
            self._cap.append((stream, fn, tuple(reads), tuple(writes), dma, nobar))
            return None
        op = Op(stream, fn, dma)
        op.oid = len(self.ops)
        for k in reads:
            w = self.last_w.get(k)
            if w is not None:
                op.deps.add(w)
        for k in writes:
            w = self.last_w.get(k)
            if w is not None:
                op.deps.add(w)
            for r in self.readers.get(k, ()):
                op.deps.add(r)
        for k in writes:
            self.last_w[k] = op.oid
            self.readers[k] = []
        for k in reads:
            self.readers.setdefault(k, []).append(op.oid)
        op.deps.discard(op.oid)
        self.ops.append(op)
        if dma:
            if not nobar:
                self.dmas.append(op.oid)
        else:
            self.last_op[stream] = op.oid
        return op

    def pe(self, fn, reads=(), writes=()):
        return self.add("pe", fn, reads, writes)

    def act(self, fn, reads=(), writes=()):
        return self.add("act", fn, reads, writes)

    def dve(self, fn, reads=(), writes=()):
        return self.add("dve", fn, reads, writes)

    def pool(self, fn, reads=(), writes=()):
        return self.add("pool", fn, reads, writes)

    def dma(self, fn, reads=(), writes=(), queue="sp", nobar=False):
        return self.add(queue, fn, reads, writes, dma=True, nobar=nobar)

    def emit(self, stack):
        nc = self.nc
        ops = self.ops
        for op in ops:
            for d in op.deps:
                dop = ops[d]
                if dop.is_dma:
                    continue
                if dop.stream == op.stream and not op.is_dma:
                    if dop.stream == "pe" or not self.same_engine_sync:
                        continue
                dop.signal = True
        sems = {}
        cnt = {}
        for op in ops:
            if op.is_dma:
                q = op.stream
                j = cnt.get(("dma", q), 0)
                cnt[("dma", q)] = j + 1
                ring = j % DMA_RING
                n = j // DMA_RING
                ep = n // DMA_EPOCH
                op.sig = ("d_%s_%d_%d" % (q, ring, ep), 16 * (n % DMA_EPOCH + 1))
            elif op.signal:
                s = op.stream
                j = cnt.get(s, 0)
                cnt[s] = j + 1
                ep = j // SEM_EPOCH
                op.sig = ("c_%s_%d" % (s, ep), j % SEM_EPOCH + 1)
        know = {s: {} for s in ("pe", "act", "dve", "pool", "sp")}
        plan = {s: [] for s in know}
        dma_hist = {}
        for op in ops:
            s = op.stream
            K = know[s]
            waits = {}
            for d in sorted(op.deps):
                dop = ops[d]
                if not dop.is_dma and dop.stream == s and not op.is_dma:
                    if s == "pe" or not self.same_engine_sync:
                        continue
                if dop.sig is None:
                    continue
                src, val = dop.sig
                if K.get(src, 0) >= val:
                    continue
                if waits.get(src, 0) < val:
                    waits[src] = val
            if op.is_dma:
                hist = dma_hist.setdefault(s, [])
                if len(hist) >= DMA_RING:
                    pop = ops[hist[-DMA_RING]]
                    src, val = pop.sig
                    if K.get(src, 0) < val and waits.get(src, 0) < val:
                        waits[src] = val
                    op.deps.add(pop.oid)
                hist.append(op.oid)
            for d in op.deps:
                dop = ops[d]
                if dop.sig is None or dop.know is None:
                    continue
                src, val = dop.sig
                if waits.get(src, 0) >= val or K.get(src, 0) >= val:
                    for k2, v2 in dop.know.items():
                        if K.get(k2, 0) < v2:
                            K[k2] = v2
            for src, val in waits.items():
                if K.get(src, 0) < val:
                    K[src] = val
            if op.sig is not None:
                kn = dict(K)
                kn[op.sig[0]] = op.sig[1]
                op.know = kn
                if not op.is_dma and (s == "pe" or not self.same_engine_sync):
                    K[op.sig[0]] = op.sig[1]
            plan[s].append((op, sorted(waits.items())))
        for s in plan:
            for op, waits in plan[s]:
                if op.sig is not None and op.sig[0] not in sems:
                    sems[op.sig[0]] = stack.enter_context(nc.semaphore(op.sig[0]))
        block = stack.enter_context(nc.Block())

        def runner(s):
            def body(eng):
                for op, waits in plan[s]:
                    for src, val in waits:
                        eng.wait_ge(sems[src], val)
                    ins = op.fn(eng)
                    if op.sig is not None and ins is not None:
                        ins.then_inc(sems[op.sig[0]], 16 if op.is_dma else 1)
            return body

        block.tensor(runner("pe"))
        block.scalar(runner("act"))
        block.vector(runner("dve"))
        block.gpsimd(runner("pool"))
        block.sync(runner("sp"))
        self.stats = {s: len(plan[s]) for s in plan}
        self.stats["waits"] = sum(len(w) for s in plan for _, w in plan[s])
        self.stats["sems"] = len(sems)


D = 1024
SEQ = 8192
HALF = 4096
NT = 512
NTILES = HALF // NT
N_IN = 12288
COLS_A = 4608
COLS_B = 4096
G_BQ, G_BF, G_BI, G_BG = 9, 11, 13, 15
G_C = 17
G_GATE = 18
G_WB, G_WA, G_WC, G_WO, G_KV = 24, 26, 27, 28, 30
NGROUPS = 32
DN_ALPHA = 2 ** 0.25
LN_EPS = 1e-5
RMS_EPS = 1e-6
GROUPS_A = ((128, 1), (512, 4), (2048, 16))
NEG = -30000.0


def _t5_bucket_np(dist):
    dist = np.asarray(dist, np.int32)
    max_exact = 16
    d = np.maximum(dist, 1).astype(np.float32)
    large = max_exact + (np.log(d / max_exact) / math.log(2048 / max_exact) * (32 - max_exact)).astype(np.int32)
    large = np.minimum(large, 31)
    return np.where(dist < max_exact, dist, large).astype(np.int32)


def moe_phase(nc, S, L):
    PB, PT, HB, HF, lg, ones_bf, ustr, cb, ident = L["PB"], L["PT"], L["HB"], L["HF"], L["lg"], L["ones_bf"], L["ustr"], L["cb"], L["ident"]
    xres, lnrow, out, xbuf, ybuf, weg, weu, wed, vecs = L["xres"], L["lnrow"], L["out"], L["xbuf"], L["ybuf"], L["weg"], L["weu"], L["wed"], L["vecs"]
    N, NBLK, wt, sb, next_pb, layer_norm = L["NSUB"], L["NBLK"], L["wt"], L["sb"], L["next_pb"], L["layer_norm"]
    msm = sb("msm", [128, 1024])
    d1i = sb("d1i", [128, 32], I32)
    d2i = sb("d2i", [128, 32], I32)
    wix = sb("wix", [128, 96], I32)
    S.barrier()
    S.dma(lambda e: e.dma_start(out=lnrow[:], in_=vecs[5:7, :].partition_broadcast(128)), writes=["lnrow"])
    elm = HF[:, 0:N * 32].rearrange("p (s e) -> p s e", e=32)
    oh1 = HF[:, 1024:1024 + N * 32].rearrange("p (s e) -> p s e", e=32)
    oh2 = HF[:, 2048:2048 + N * 32].rearrange("p (s e) -> p s e", e=32)
    rank = HF[:, 3072:3072 + N * 32].rearrange("p (s e) -> p s e", e=32)
    trr = HF[:, 4096:4096 + N * 32].rearrange("p (s e) -> p s e", e=32)
    sm = lambda i, n=32: msm[:, i * 32:i * 32 + n]
    gmax, gsum, gp, m1, m2, w1, w2, d1, d2 = [sm(i, N) for i in range(9)]
    cnt, pc, pend, pstart, t1, one32 = [sm(i) for i in range(9, 15)]
    g1h = msm[:, 480:480 + N * 4].rearrange("p (s g) -> p s g", g=4)
    te = msm[:, 608:608 + N * 4].rearrange("p (s g) -> p s g", g=4)
    be = msm[:, 736:736 + 96]
    Mb = HB[:, 0:N * 32].rearrange("p (s e) -> p s e", e=32)
    Mcum = HB[:, 1024:1024 + (N + 1) * 32].rearrange("p (s e) -> p s e", e=32)
    K_ = ["moe"]
    gl = lg[:, 0:N, 0:4]
    bc = lambda ap, shape: ap.to_broadcast(shape)
    A3 = lambda ap: ap.rearrange("p (s o) -> p s o", o=1)
    S.dve(lambda e: e.tensor_reduce(out=gmax, in_=gl, axis=AX.X, op=ALU.max), reads=["lg"], writes=K_)
    S.dve(lambda e: e.tensor_tensor(out=g1h, in0=gl, in1=bc(A3(gmax), [128, N, 4]), op=ALU.is_equal), reads=K_, writes=K_)
    S.dve(lambda e: e.tensor_tensor(out=te, in0=gl, in1=bc(A3(gmax), [128, N, 4]), op=ALU.subtract), reads=K_, writes=K_)
    S.act(lambda e: e.activation(out=te, in_=te, func=AF.Exp), reads=K_, writes=K_)
    S.dve(lambda e: e.tensor_reduce(out=gsum, in_=te, axis=AX.X, op=ALU.add), reads=K_, writes=K_)
    S.dve(lambda e: e.reciprocal(out=gp, in_=gsum), reads=K_, writes=K_)
    S.dve(lambda e: e.tensor_scalar(out=g1h, in0=g1h, scalar1=BIG, scalar2=-BIG, op0=ALU.mult, op1=ALU.add), reads=K_, writes=K_)
    S.dve(lambda e: e.tensor_copy(out=elm, in_=lg[:, 0:N, 4:36]), reads=K_, writes=K_)
    S.dve(lambda e: e.tensor_tensor(out=elm.rearrange("p s (g e) -> p (s g) e", g=4), in0=elm.rearrange("p s (g e) -> p (s g) e", g=4),
                                    in1=bc(g1h.rearrange("p s (g o) -> p (s g) o", o=1), [128, N * 4, 8]), op=ALU.add), reads=K_, writes=K_)
    S.dve(lambda e: e.tensor_reduce(out=m1, in_=elm, axis=AX.X, op=ALU.max), reads=K_, writes=K_)
    S.dve(lambda e: e.tensor_tensor(out=oh1, in0=elm, in1=bc(A3(m1), [128, N, 32]), op=ALU.is_equal), reads=K_, writes=K_)
    S.dve(lambda e: e.scalar_tensor_tensor(out=elm, in0=oh1, scalar=-BIG, in1=elm, op0=ALU.mult, op1=ALU.add), reads=K_, writes=K_)
    S.dve(lambda e: e.tensor_reduce(out=m2, in_=elm, axis=AX.X, op=ALU.max), reads=K_, writes=K_)
    S.dve(lambda e: e.tensor_tensor(out=oh2, in0=elm, in1=bc(A3(m2), [128, N, 32]), op=ALU.is_equal), reads=K_, writes=K_)
    S.dve(lambda e: e.tensor_tensor(out=w1, in0=m2, in1=m1, op=ALU.subtract), reads=K_, writes=K_)
    S.act(lambda e: e.activation(out=w1, in_=w1, func=AF.Exp), reads=K_, writes=K_)
    S.dve(lambda e: e.tensor_scalar(out=w1, in0=w1, scalar1=1.0, scalar2=None, op0=ALU.add), reads=K_, writes=K_)
    S.dve(lambda e: e.reciprocal(out=w1, in_=w1), reads=K_, writes=K_)
    S.dve(lambda e: e.tensor_scalar(out=w2, in0=w1, scalar1=-1.0, scalar2=1.0, op0=ALU.mult, op1=ALU.add), reads=K_, writes=K_)
    S.dve(lambda e: e.tensor_tensor(out=w1, in0=w1, in1=gp, op=ALU.mult), reads=K_, writes=K_)
    S.dve(lambda e: e.tensor_tensor(out=w2, in0=w2, in1=gp, op=ALU.mult), reads=K_, writes=K_)
    S.dve(lambda e: e.tensor_tensor(out=Mb, in0=oh1, in1=oh2, op=ALU.add), reads=K_, writes=K_)
    S.dve(lambda e: e.memset(Mcum[:, 0, :], 0.0), reads=K_, writes=K_)
    S.dve(lambda e: e.memset(one32, 1.0), reads=K_, writes=K_)
    for s in range(N):
        S.dve(lambda e, s=s: e.tensor_tensor(out=Mcum[:, s + 1, :], in0=Mcum[:, s, :], in1=Mb[:, s, :], op=ALU.add), reads=K_, writes=K_)
    for s0 in range(0, N, 16):
        pr = next_pb()

        def rk(e, s0=s0, pr=pr):
            for s in range(s0, min(N, s0 + 16)):
                e.matmul(PB[pr][:, (s - s0) * 32:(s - s0 + 1) * 32], lhsT=ustr[:], rhs=Mb[:, s, :], start=True, stop=False)
                ins = e.matmul(PB[pr][:, (s - s0) * 32:(s - s0 + 1) * 32], lhsT=ones_bf[:], rhs=Mcum[:, s, :], start=False, stop=True)
            return ins
        S.pe(rk, reads=K_ + ["ustr", "ones_bf"], writes=[("pb", pr)])
        n_ = min(N, s0 + 16) - s0
        S.dve(lambda e, s0=s0, pr=pr, n_=n_: e.tensor_copy(out=rank[:, s0:s0 + n_, :], in_=PB[pr][:, 0:n_ * 32].rearrange("p (s e) -> p s e", e=32)), reads=[("pb", pr)] + K_, writes=K_)
    pcn = next_pb()
    S.pe(lambda e: e.matmul(PB[pcn][:, 0:32], lhsT=ones_bf[:], rhs=Mcum[:, N, :], start=True, stop=True), reads=K_ + ["ones_bf"], writes=[("pb", pcn)])
    S.dve(lambda e: e.tensor_copy(out=cnt, in_=PB[pcn][:, 0:32]), reads=[("pb", pcn)] + K_, writes=K_)
    cmpc = HB[:, 8192:8192 + 2048].rearrange("p (e k) -> p e k", k=64)
    S.dve(lambda e: e.tensor_tensor(out=cmpc, in0=bc(cnt.rearrange("p (e o) -> p e o", o=1), [128, 32, 64]),
                                    in1=bc(cb[:, 0:64].rearrange("p (o k) -> p o k", o=1), [128, 32, 64]), op=ALU.is_gt), reads=K_ + ["cb"], writes=K_)
    S.dve(lambda e: e.tensor_reduce(out=pc, in_=cmpc, axis=AX.X, op=ALU.add), reads=K_, writes=K_)
    S.dve(lambda e: e.tensor_scalar(out=pc, in0=pc, scalar1=128.0, scalar2=None, op0=ALU.mult), reads=K_, writes=K_)
    S.dve(lambda e: e.tensor_tensor_scan(out=pend, data0=one32, data1=pc, initial=0.0, op0=ALU.mult, op1=ALU.add), reads=K_, writes=K_)
    S.dve(lambda e: e.tensor_tensor(out=pstart, in0=pend, in1=pc, op=ALU.subtract), reads=K_, writes=K_)
    psb = lambda: bc(pstart.rearrange("p (o e) -> p o e", o=1), [128, N, 32])
    S.dve(lambda e: e.tensor_tensor(out=rank, in0=rank, in1=psb(), op=ALU.add), reads=K_, writes=K_)
    S.dve(lambda e: e.tensor_tensor(out=trr, in0=rank, in1=oh1, op=ALU.mult), reads=K_, writes=K_)
    S.dve(lambda e: e.tensor_reduce(out=d1, in_=trr, axis=AX.X, op=ALU.add), reads=K_, writes=K_)
    S.dve(lambda e: e.tensor_tensor(out=trr, in0=rank, in1=oh2, op=ALU.mult), reads=K_, writes=K_)
    S.dve(lambda e: e.tensor_reduce(out=d2, in_=trr, axis=AX.X, op=ALU.add), reads=K_, writes=K_)
    S.dve(lambda e: e.tensor_copy(out=d1i[:, 0:N], in_=d1), reads=K_, writes=K_)
    S.dve(lambda e: e.tensor_copy(out=d2i[:, 0:N], in_=d2), reads=K_, writes=K_)
    cmp3 = HF[:, 0:NBLK * 32].rearrange("p (b e) -> p b e", e=32)
    S.dve(lambda e: e.tensor_tensor(out=cmp3, in0=bc(cb[:, 0:NBLK].rearrange("p (b o) -> p b o", o=1), [128, NBLK, 32]),
                                    in1=bc(pend.rearrange("p (o e) -> p o e", o=1), [128, NBLK, 32]), op=ALU.is_ge), reads=K_ + ["cb"], writes=K_)
    S.dve(lambda e: e.tensor_reduce(out=be[:, 0:NBLK], in_=cmp3, axis=AX.X, op=ALU.add), reads=K_, writes=K_)
    S.dve(lambda e: e.tensor_scalar(out=be[:, 0:NBLK], in0=be[:, 0:NBLK], scalar1=31.0, scalar2=128.0, op0=ALU.min, op1=ALU.mult), reads=K_, writes=K_)
    S.dve(lambda e: e.tensor_scalar(out=be[:, 0:NBLK], in0=be[:, 0:NBLK], scalar1=cb[:, 96:97], scalar2=None, op0=ALU.add), reads=K_ + ["cb"], writes=K_)
    S.dve(lambda e: e.tensor_copy(out=wix[:, 0:NBLK], in_=be[:, 0:NBLK]), reads=K_, writes=K_)

    sc_keys = []
    xbr = [HB[:, 4096:5120], HB[:, 5120:6144]]
    for s in range(N):
        k = s % 4
        S.dma(lambda e, s=s, k=k: e.dma_start(out=xres[:, k, :], in_=out[s * 128:(s + 1) * 128, :]), reads=[("out", s)], writes=[("xres", k)])
        S.act(lambda e, s=s, k=k: e.activation(out=xbr[s % 2], in_=xres[:, k, :], func=AF.Copy), reads=[("xres", k)], writes=[("xbr", s % 2)])
        for di in (d1i, d2i):
            S.dma(lambda e, s=s, di=di: e.indirect_dma_start(out=xbuf[:, :], out_offset=bass.IndirectOffsetOnAxis(ap=di[:, s:s + 1], axis=0), in_=xbr[s % 2], in_offset=None),
                  reads=[("xbr", s % 2), "xbuf"] + K_, writes=[("xbufw", s, id(di))], queue="pool")
            sc_keys.append(("xbufw", s, id(di)))

    webf = L["webf"]
    wb3 = [HB[:, 6144:12288], HB[:, 12288:18432], HF[:, 0:3072].bitcast(BF16)]
    wbb = [[wb3[j][:, i * 2048:(i + 1) * 2048] for i in range(3)] for j in range(3)]
    S.barrier()
    xbk2 = [HB[:, 18432:19456], HB[:, 0:1024]]
    xbT2 = [HB[:, 19456:20480].rearrange("p (k t) -> p k t", k=8), HB[:, 1024:2048].rearrange("p (k t) -> p k t", k=8)]
    hT2 = [HB[:, 20480:20736].rearrange("p (n t) -> p n t", n=2), HB[:, 2048:2304].rearrange("p (n t) -> p n t", n=2)]
    sg22 = [HB[:, 20736:20992], HB[:, 2304:2560]]
    yb2 = [HB[:, 20992:22016], HB[:, 2560:3584]]
    def blk_gather(b):
        j3 = b % 3
        S.dma(lambda e, b=b, j3=j3: e.indirect_dma_start(out=wb3[j3], out_offset=None, in_=webf[:, :], in_offset=bass.IndirectOffsetOnAxis(ap=wix[:, b:b + 1], axis=0)),
              reads=K_ + [("webf", k) for k in range(3 * N_EXP)], writes=[("wbb", j3, 0), ("wbb", j3, 1), ("wbb", j3, 2)], queue="pool")

    def blk_front(b):
        j = b % 2
        j3 = b % 3
        xbk, xbT, hT, sg2 = xbk2[j], xbT2[j], hT2[j], sg22[j]
        S.dma(lambda e, b=b, xbk=xbk: e.dma_start(out=xbk, in_=xbuf[b * 128:(b + 1) * 128, :]), reads=["xbuf"] + sc_keys, writes=[("xbk", j)])

        def trb(e, xbk=xbk):
            for k in range(8):
                ins = e.transpose(PT[:, k * 128:(k + 1) * 128], xbk[:, k * 128:(k + 1) * 128], ident[:])
            return ins
        S.pe(trb, reads=[("xbk", j), "ident"], writes=["PT"])
        S.dve(lambda e, xbT=xbT: e.tensor_copy(out=xbT, in_=PT[:].rearrange("p (k t) -> p k t", k=8)), reads=["PT"], writes=[("xbT", j)])
        pg = next_pb()
        wg3 = wbb[j3][0].rearrange("p (k n) -> p k n", k=8)
        wu3 = wbb[j3][1].rearrange("p (k n) -> p k n", k=8)

        def gum(e, pg=pg, wg3=wg3, wu3=wu3, xbT=xbT):
            for q, w3 in enumerate((wg3, wu3)):
                for n_ in range(2):
                    for kc in range(8):
                        ins = e.matmul(PB[pg][:, (q * 2 + n_) * 128:(q * 2 + n_ + 1) * 128], lhsT=w3[:, kc, n_ * 128:(n_ + 1) * 128], rhs=xbT[:, kc, :], start=(kc == 0), stop=(kc == 7))
            return ins
        S.pe(gum, reads=[("wbb", j3, 0), ("wbb", j3, 1), ("xbT", j)], writes=[("pb", pg)])
        S.act(lambda e, pg=pg, sg2=sg2: e.activation(out=sg2, in_=PB[pg][:, 0:256], func=AF.Silu), reads=[("pb", pg)], writes=[("sg2", j)])
        S.dve(lambda e, pg=pg, sg2=sg2, hT=hT: e.tensor_tensor(out=hT, in0=sg2.rearrange("p (n t) -> p n t", n=2), in1=PB[pg][:, 256:512].rearrange("p (n t) -> p n t", n=2), op=ALU.mult), reads=[("pb", pg), ("sg2", j)], writes=[("hT", j)])

    def blk_back(b):
        j = b % 2
        hT, yb = hT2[j], yb2[j]
        j3 = b % 3
        wd3 = wbb[j3][2].rearrange("p (k n) -> p k n", k=2)
        for half in range(2):
            py = next_pb()

            def ym(e, py=py, half=half, wd3=wd3, hT=hT):
                e.matmul(PB[py][:], lhsT=hT[:, 0, :], rhs=wd3[:, 0, half * 512:(half + 1) * 512], start=True, stop=False)
                return e.matmul(PB[py][:], lhsT=hT[:, 1, :], rhs=wd3[:, 1, half * 512:(half + 1) * 512], start=False, stop=True)
            S.pe(ym, reads=[("hT", j), ("wbb", j3, 2)], writes=[("pb", py)])
            if half == 0:
                S.act(lambda e, py=py, yb=yb: e.activation(out=yb[:, 0:512], in_=PB[py][:], func=AF.Copy), reads=[("pb", py)], writes=[("yb", j, 0)])
            else:
                S.dve(lambda e, py=py, yb=yb: e.tensor_copy(out=yb[:, 512:1024], in_=PB[py][:]), reads=[("pb", py)], writes=[("yb", j, 1)])
        S.dma(lambda e, b=b, yb=yb: e.dma_start(out=ybuf[b * 128:(b + 1) * 128, :], in_=yb), reads=[("yb", j, 0), ("yb", j, 1)], writes=[("ybufw", b)])

    blk_gather(0)
    if NBLK > 1:
        blk_gather(1)
    blk_front(0)
    for b in range(NBLK):
        if b + 2 < NBLK:
            blk_gather(b + 2)
        if b + 1 < NBLK:
            blk_front(b + 1)
        blk_back(b)
    yb_keys = [("ybufw", b) for b in range(NBLK)]
    S.barrier()

    r12 = [HB[:, 0:1024], HB[:, 1024:2048], HB[:, 2048:3072], HB[:, 3072:4096]]

    def cmb_fetch(s):
        k = s % 4
        S.dma(lambda e, s=s, k=k: e.dma_start(out=xres[:, k, :], in_=out[s * 128:(s + 1) * 128, :]), reads=[("out", s)], writes=[("xres", k)])
        for q, di in enumerate((d1i, d2i)):
            S.dma(lambda e, s=s, di=di, q=q: e.indirect_dma_start(out=r12[(s % 2) * 2 + q], out_offset=None, in_=ybuf[:, :], in_offset=bass.IndirectOffsetOnAxis(ap=di[:, s:s + 1], axis=0)),
                  reads=yb_keys + K_, writes=[("r12", (s % 2) * 2 + q)], queue="pool")

    def cmb_compute(s):
        k = s % 4
        ya = HF[:, (s % 2) * 1024:(s % 2 + 1) * 1024]
        S.dve(lambda e, s=s, ya=ya: e.tensor_scalar(out=ya, in0=r12[(s % 2) * 2], scalar1=w1[:, s:s + 1], scalar2=None, op0=ALU.mult), reads=[("r12", (s % 2) * 2)] + K_, writes=[("ya", s % 2)])
        S.dve(lambda e, s=s, ya=ya: e.scalar_tensor_tensor(out=ya, in0=r12[(s % 2) * 2 + 1], scalar=w2[:, s:s + 1], in1=ya, op0=ALU.mult, op1=ALU.add), reads=[("r12", (s % 2) * 2 + 1), ("ya", s % 2)] + K_, writes=[("ya", s % 2)])
        S.dve(lambda e, k=k, ya=ya: e.scalar_tensor_tensor(out=xres[:, k, :], in0=xres[:, k, :], scalar=DN_ALPHA, in1=ya, op0=ALU.mult, op1=ALU.add), reads=[("xres", k), ("ya", s % 2)], writes=[("xres", k)])
        layer_norm(k, 1)
        S.dma(lambda e, s=s, k=k: e.dma_start(out=out[s * 128:(s + 1) * 128, :], in_=xres[:, k, :]), reads=[("xres", k)], writes=[("out", s)])

    for s in range(min(2, N)):
        cmb_fetch(s)
    for s in range(N):
        cmb_compute(s)
        if s + 2 < N:
            cmb_fetch(s + 2)


A_OFF = {"g2A": 0, "g1A": 640, "g0A": 1664, "g2B": 2688, "g1B": 2816, "g0B": 3328}
A_NB = 3840
N_EXP = 32
BIG = 1.0e4


def build_nc(stage="full"):
    quick = stage == "quick"
    n_ctx = 4 if quick else NTILES
    n_own = 2 if quick else NTILES
    NSUB = n_own * 4
    NBLK = (2 * 128 * NSUB + N_EXP * 127 + 127) // 128
    NSLOT = NBLK * 128

    nc = bass.Bass("TRN2", target_bir_lowering=False)
    din = lambda name, shape, dt=F32: nc.dram_tensor(name, list(shape), dt, kind="ExternalInput").ap()
    xT = din("xT", [D, 2 * HALF])
    xo = din("xo", [HALF, D])
    memT = din("memT", [D, 256])
    w_in = din("w_in", [D, N_IN])
    w_bb = din("w_bb", [D, D])
    w_ba = din("w_ba", [512, D])
    w_bc = din("w_bc", [512, D])
    w_o = din("w_o", [D, D])
    w_kv = din("w_kv", [D, D])
    w_r = din("w_r", [128, 8, 36])
    b_r = din("b_r", [1, 36])
    weg = din("weg", [N_EXP * 128, 2048])
    weu = din("weu", [N_EXP * 128, 2048])
    wed = din("wed", [N_EXP * 128, 2048])
    vecs = din("vecs", [8, D])
    pvec = din("pvec", [128, 3, 8])
    cmask = din("cmask", [128, 1024 + 128 + 97])
    ident_in = din("ident", [128, 128])
    abias = din("abias", [128, A_NB])
    out = nc.dram_tensor("out", [HALF, D], F32, kind="ExternalOutput").ap()
    wbf = nc.dram_tensor("wbf", [NGROUPS, 128, 4096], BF16).ap()
    KTd = nc.dram_tensor("KTd", [3, 4, 128, 2 * HALF], BF16).ap()
    Vd = nc.dram_tensor("Vd", [3, 2 * HALF, 512], BF16).ap()
    xbuf = nc.dram_tensor("xbuf", [NSLOT, D], BF16).ap()
    ybuf = nc.dram_tensor("ybuf", [NSLOT, D], BF16).ap()
    webf = nc.dram_tensor("webf", [N_EXP * 128, 6144], BF16).ap()

    with ExitStack() as st:
        def sb(name, shape, dt=F32):
            return st.enter_context(nc.sbuf_tensor("s_" + name, list(shape), dt))

        def psum(name, shape, dt=F32):
            return st.enter_context(nc.psum_tensor("p_" + name, list(shape), dt))

        S = Sched(nc)
        ident = sb("ident", [128, 128], BF16)
        identf = sb("identf", [128, 128])
        rmask = sb("rmask", [128, 512])
        cm4 = sb("cm4", [128, 512], BF16)
        ustr = sb("ustr", [128, 128], BF16)
        cb = sb("cb", [128, 97])
        ones_bf = sb("ones_bf", [128, 128], BF16)
        lbv = sb("lbv", [128, 8])
        omlv = sb("omlv", [128, 8])
        lgt = sb("lgt", [128, 2, 8])
        ngv = sb("ngv", [128, 8])
        lnrow = sb("lnrow", [128, 2, D])
        ab = sb("ab", [128, A_NB], BF16)
        wr = sb("wr", [128, 8, 36])
        brow = sb("brow", [128, 36])
        S.dma(lambda e: e.dma_start(out=ident[:], in_=ident_in), writes=["ident"], queue="pool")
        S.dma(lambda e: e.dma_start(out=identf[:], in_=ident_in), writes=["identf"])
        S.dma(lambda e: e.dma_start(out=rmask[:], in_=cmask[:, 0:512]), writes=["rmask"])
        S.dma(lambda e: e.dma_start(out=cm4[:], in_=cmask[:, 512:1024]), writes=["cm4"], queue="pool")
        S.dma(lambda e: e.dma_start(out=ustr[:], in_=cmask[:, 1024:1152]), writes=["ustr"], queue="pool")
        S.dma(lambda e: e.dma_start(out=cb[:], in_=cmask[:, 1152:1249]), writes=["cb"])
        S.dma(lambda e: e.dma_start(out=ab[:], in_=abias), writes=["ab"], queue="pool")
        S.dma(lambda e: e.dma_start(out=wr[:], in_=w_r), writes=["wr"])
        S.dma(lambda e: e.dma_start(out=brow[:], in_=b_r.partition_broadcast(128)), writes=["brow"])
        S.pool(lambda e: e.memset(ones_bf[:], 1.0), writes=["ones_bf"])
        S.dma(lambda e: e.dma_start(out=lgt[:], in_=pvec[:, 0:2, :]), writes=["lgt"])
        S.dma(lambda e: e.dma_start(out=ngv[:], in_=pvec[:, 2, :]), writes=["ngv"])
        S.dma(lambda e: e.dma_start(out=lnrow[:], in_=vecs[3:5, :].partition_broadcast(128)), writes=["lnrow"])
        S.dve(lambda e: e.tensor_tensor(out=lbv[:], in0=lgt[:, 0, :], in1=lgt[:, 1, :], op=ALU.subtract), reads=["lgt"], writes=["lbv"])
        S.act(lambda e: e.activation(out=lbv[:], in_=lbv[:], func=AF.Sigmoid), reads=["lbv"], writes=["lbv"])
        S.dve(lambda e: e.tensor_scalar(out=omlv[:], in0=lbv[:], scalar1=-1.0, scalar2=1.0, op0=ALU.mult, op1=ALU.add), reads=["lbv"], writes=["omlv"])

        xTb = [sb("xTb0", [128, 8, NT], BF16), sb("xTb1", [128, 8, NT], BF16)]
        wt = [sb("wt%d" % i, [128, 4096], BF16) for i in range(3)]
        srcs = []
        for j in range(24):
            srcs.append((w_in[:, j * 512:(j + 1) * 512].rearrange("(kc p) n -> p kc n", p=128), 8))
        srcs.append((w_bb[:, 0:512].rearrange("(kc p) n -> p kc n", p=128), 8))
        srcs.append((w_bb[:, 512:1024].rearrange("(kc p) n -> p kc n", p=128), 8))
        srcs.append((w_ba.rearrange("(kc p) n -> p kc n", p=128), 4))
        srcs.append((w_bc.rearrange("(kc p) n -> p kc n", p=128), 4))
        srcs.append((w_o[:, 0:512].rearrange("(kc p) n -> p kc n", p=128), 8))
        srcs.append((w_o[:, 512:1024].rearrange("(kc p) n -> p kc n", p=128), 8))
        srcs.append((w_kv[:, 0:512].rearrange("(kc p) n -> p kc n", p=128), 8))
        srcs.append((w_kv[:, 512:1024].rearrange("(kc p) n -> p kc n", p=128), 8))
        first = [30, 31, 11, 12, 13, 14, 9, 10, 15, 16, 20, 21, 24, 25, 28, 29]
        order = first + [g for g in range(NGROUPS) if g not in first]
        for n, g in enumerate(order):
            src, kcs = srcs[g]
            sg_ = wt[n % 3]
            S.dma(lambda e, src=src, sg_=sg_, kcs=kcs: e.dma_start(out=sg_[:].rearrange("p (kc n) -> p kc n", kc=kcs), in_=src),
                  writes=[("wt", n % 3)], queue="pool")
            S.dma(lambda e, g=g, sg_=sg_: e.dma_start(out=wbf[g], in_=sg_[:]), reads=[("wt", n % 3)], writes=[("wbf", g)])

        wt_n = [0]
        PB = [psum("pb%d" % i, [128, 512]) for i in range(7)]
        PT = psum("ptr", [128, 1024], BF16)
        HB = sb("HB", [128, 22528], BF16)
        HF = sb("HF", [128, 5120])
        khatT = HB[:, 0:4096].rearrange("p (h t) -> p h t", h=8)
        kt = HB[:, 4096:8192].rearrange("p (h t) -> p h t", h=8)
        qt = HB[:, 8192:12288].rearrange("p (h t) -> p h t", h=8)
        khat = HB[:, 12288:16384].rearrange("p (s d) -> p s d", s=4)
        Vt = HB[:, 16384:20480].rearrange("p (s d) -> p s d", s=4)
        ATs = HB[:, 20480:21504].rearrange("p (h t) -> p h t", h=8)
        tmp = [[HF[:, (i * 5 + j) * 512:(i * 5 + j + 1) * 512] for j in range(5)] for i in range(2)]
        QT = HB[:, 0:6144].rearrange("p (g t) -> p g t", g=12)
        Kwin = [HB[:, 6144:8704], HB[:, 8704:11264]]
        VA = [HB[:, 11264:11392], HB[:, 11392:11520], HB[:, 22016:22144]]
        VB = [HB[:, 11520:11648], HB[:, 11648:11776], HB[:, 22144:22272]]
        PTa = [HB[:, 11776:12032], HB[:, 12032:12288], HB[:, 22272:22528]]
        oaT = HB[:, 12288:14336].rearrange("p (h t) -> p h t", h=4)
        ocT = HB[:, 14336:16384].rearrange("p (h t) -> p h t", h=4)
        qcT = HB[:, 16384:16896]
        PcT = HB[:, 16896:17920].rearrange("p (m t) -> p m t", m=2)
        KTt = HB[:, 17920:19968].rearrange("p (h t) -> p h t", h=4)
        Vtl = HB[:, 19968:22016].rearrange("p (s d) -> p s d", s=4)
        acc_o = HF[:, 0:512]
        acc_l = HF[:, 512:1024]
        recA = HF[:, 1024:1536]
        recC = HF[:, 1536:2048]
        x1Ts = sb("x1Ts", [128, 8, 128])
        x1T4 = [x1Ts, x1Ts]
        Sst = sb("Sst", [128, 8, 128])
        Sbf = sb("Sbf", [128, 8, 128], BF16)
        dec = sb("dec", [128, 8, 8])
        OT = sb("OT", [128, 8, NT])
        mrg = OT
        sgt = sb("sgt", [128, NT], BF16)
        obT = sb("obT", [128, 8, NT], BF16)
        mrgb = sb("mrgb", [128, 8, NT], BF16)
        gsb = sb("gsb", [128, NT])
        xres = sb("xres", [128, 4, D])
        bnst = sb("bnst", [128, 2, 6])
        mv = sb("mv", [128, 2])
        rstd = sb("rstd", [128, 1])
        sqb = sb("sqb", [128, NT], BF16)
        KcT = sb("KcT", [128, 4, 256], BF16)
        Vc = sb("Vc", [128, 2, 512], BF16)
        lg = sb("lg", [128, 32, 36])

        S.pool(lambda e: e.memset(gsb[:], 0.0), writes=["gsb"])
        S.dma(lambda e: e.dma_start(out=xbuf.rearrange("(b p) d -> p b d", p=128), in_=gsb[:].bitcast(BF16).rearrange("p (o d) -> p o d", o=1).to_broadcast([128, NBLK, 1024])), reads=["gsb"], writes=["xbuf"], nobar=True)
        S.pool(lambda e: e.memset(Sst[:], 0.0), writes=[("S", h) for h in range(8)])
        S.pool(lambda e: e.memset(Sbf[:], 0.0), writes=[("Sbf", h) for h in range(8)])

        wt_pool = [[0, 1, 2]]

        def load_w(g):
            i = wt_pool[0][wt_n[0] % len(wt_pool[0])]
            wt_n[0] += 1
            S.dma(lambda e, g=g, i=i: e.dma_start(out=wt[i][:], in_=wbf[g]), reads=[("wbf", g)], writes=[("wt", i)], nobar=True)
            return i

        def wview(i, kcs):
            return wt[i][:].rearrange("p (kc n) -> p kc n", kc=kcs)

        pb_n = [0]

        pb_pool = [3]
        POOLS = {3: [0, 1, 2], 7: [0, 1, 2, 3, 4, 5, 6], "H": [0, 2], "P": [1], "L2": [0, 1, 2], "AT": [3, 4, 5, 6]}

        def next_pb():
            pl = POOLS[pb_pool[0]]
            pbi = pl[pb_n[0] % len(pl)]
            pb_n[0] += 1
            return pbi

        def proj_fm(wi, blk, xb, xkey, ncols=NT):
            pbi = next_pb()
            w3 = wview(wi, 8)

            def mm(e, pbi=pbi, blk=blk, xb=xb, w3=w3):
                for kc in range(8):
                    ins = e.matmul(PB[pbi][:, 0:ncols], lhsT=w3[:, kc, blk * 128:(blk + 1) * 128], rhs=xb[:, kc, 0:ncols], start=(kc == 0), stop=(kc == 7))
                return ins
            S.pe(mm, reads=[("wt", wi), xkey], writes=[("pb", pbi)])
            return pbi

        def proj_tm(wi, sub, xb, xkey):
            pbi = next_pb()
            w3 = wview(wi, 8)

            def mm(e, pbi=pbi, sub=sub, xb=xb, w3=w3):
                for kc in range(8):
                    ins = e.matmul(PB[pbi][:], lhsT=xb[:, kc, sub * 128:(sub + 1) * 128], rhs=w3[:, kc, :], start=(kc == 0), stop=(kc == 7))
                return ins
            S.pe(mm, reads=[("wt", wi), xkey], writes=[("pb", pbi)])
            return pbi

        memb = HB[:, 0:2048].rearrange("p (k m) -> p k m", k=8)
        S.dma(lambda e: e.dma_start(out=memb, in_=memT.rearrange("(kc p) m -> p kc m", p=128)), writes=["memb"], queue="pool")
        wk_ = load_w(G_KV)
        for hc in range(4):
            pk = proj_fm(wk_, hc, memb, "memb", ncols=256)
            S.act(lambda e, pk=pk, hc=hc: e.activation(out=KcT[:, hc, :], in_=PB[pk][:, 0:256], func=AF.Copy), reads=[("pb", pk)], writes=["KcT"])
        wv_ = load_w(G_KV + 1)
        for ms in range(2):
            pv = proj_tm(wv_, ms, memb, "memb")
            S.act(lambda e, pv=pv, ms=ms: e.activation(out=Vc[:, ms, :], in_=PB[pv][:], func=AF.Copy), reads=[("pb", pv)], writes=["Vc"])
        S.barrier()

        def phase_h(tix, is_ctx, xb, xkey):
            for hg in range(2):
                wf = load_w(G_BF + hg)
                wq = load_w(G_BQ + hg) if not is_ctx else None
                for pr_ in range(2):
                    hs = [hg * 4 + pr_ * 2, hg * 4 + pr_ * 2 + 1]
                    Ts = {h: tmp[h % 2] for h in hs}
                    tk = lambda h, j: ("tmp", h % 2, j)
                    for h in hs:
                        T = Ts[h]
                        pf = proj_fm(wf, h % 4, xb, xkey)
                        S.act(lambda e, T=T, pf=pf: e.activation(out=T[0], in_=PB[pf][:], func=AF.Sigmoid), reads=[("pb", pf)], writes=[tk(h, 0)])
                        if not is_ctx:
                            pq = proj_fm(wq, h % 4, xb, xkey)
                            S.act(lambda e, T=T, pq=pq: e.activation(out=T[4], in_=PB[pq][:], func=AF.Silu), reads=[("pb", pq)], writes=[tk(h, 4)])
                    for h in hs:
                        T = Ts[h]
                        S.dve(lambda e, T=T, h=h: e.tensor_scalar(out=T[0], in0=T[0], scalar1=omlv[:, h:h + 1], scalar2=lbv[:, h:h + 1], op0=ALU.mult, op1=ALU.add),
                              reads=[tk(h, 0), "lbv", "omlv"], writes=[tk(h, 0)])
                    for h in hs:
                        T = Ts[h]
                        S.act(lambda e, T=T: e.activation(out=T[1], in_=T[0], func=AF.Ln), reads=[tk(h, 0)], writes=[tk(h, 1)])
                    for h in hs:
                        T = Ts[h]
                        S.dve(lambda e, T=T: e.tensor_tensor_scan(out=T[2], data0=rmask[:], data1=T[1], initial=0.0, op0=ALU.mult, op1=ALU.add),
                              reads=[tk(h, 1), "rmask"], writes=[tk(h, 2)])
                        S.pool(lambda e, T=T: e.tensor_scalar(out=T[0], in0=T[0], scalar1=-1.0, scalar2=1.0, op0=ALU.mult, op1=ALU.add), reads=[tk(h, 0)], writes=[tk(h, 0)])

                        def dfn(e, T=T):
                            b3 = T[2].rearrange("p (c s) -> p c s", s=64)
                            return e.tensor_tensor(out=T[3].rearrange("p (c s) -> p c s", s=64), in0=b3[:, :, 63:64].to_broadcast([128, 8, 64]), in1=b3, op=ALU.subtract)
                        S.dve(dfn, reads=[tk(h, 2)], writes=[tk(h, 3)])
                    for h in hs:
                        T = Ts[h]
                        S.act(lambda e, T=T: e.activation(out=T[3], in_=T[3], func=AF.Exp), reads=[tk(h, 3)], writes=[tk(h, 3)])
                        S.act(lambda e, T=T, h=h: e.activation(out=dec[:, h, :], in_=T[2][:, 63::64], func=AF.Exp), reads=[tk(h, 2)], writes=[("dec", h)])
                    for h in hs:
                        T = Ts[h]
                        S.dve(lambda e, T=T, h=h: e.tensor_tensor(out=khatT[:, h, :], in0=T[0], in1=T[3], op=ALU.mult), reads=[tk(h, 0), tk(h, 3)], writes=[("khatT", h)])
                    if not is_ctx:
                        for h in hs:
                            T = Ts[h]
                            S.act(lambda e, T=T: e.activation(out=T[3], in_=T[2], func=AF.Exp, scale=-1.0), reads=[tk(h, 2)], writes=[tk(h, 3)])
                        for h in hs:
                            T = Ts[h]
                            S.pool(lambda e, T=T, h=h: e.tensor_tensor(out=kt[:, h, :], in0=T[0], in1=T[3], op=ALU.mult), reads=[tk(h, 0), tk(h, 3)], writes=[("kt", h)])
                        for h in hs:
                            T = Ts[h]
                            S.act(lambda e, T=T: e.activation(out=T[2], in_=T[2], func=AF.Exp), reads=[tk(h, 2)], writes=[tk(h, 2)])
                        for h in hs:
                            T = Ts[h]
                            S.dve(lambda e, T=T, h=h: e.tensor_tensor(out=qt[:, h, :], in0=T[4], in1=T[2], op=ALU.mult), reads=[tk(h, 4), tk(h, 2)], writes=[("qt", h)])
            for hg in range(2):
                wi_ = load_w(G_BI + hg)
                for sub in range(4):
                    pv = proj_tm(wi_, sub, xb, xkey)
                    S.act(lambda e, pv=pv, sub=sub, hg=hg: e.activation(out=Vt[:, sub, hg * 512:(hg + 1) * 512], in_=PB[pv][:], func=AF.Copy),
                          reads=[("pb", pv)], writes=[("V", sub, hg)])
            for sub in range(4):
                for hg in range(2):
                    def trf(e, sub=sub, hg=hg):
                        for hh in range(4):
                            ins = e.transpose(PT[:, (hg * 4 + hh) * 128:(hg * 4 + hh + 1) * 128], khatT[:, hg * 4 + hh, sub * 128:(sub + 1) * 128], ident[:])
                        return ins
                    S.pe(trf, reads=[("khatT", hg * 4 + hh) for hh in range(4)] + ["ident"], writes=["PT"])
                    S.dve(lambda e, sub=sub, hg=hg: e.tensor_copy(out=khat[:, sub, hg * 512:(hg + 1) * 512], in_=PT[:, hg * 512:(hg + 1) * 512]),
                          reads=["PT"], writes=[("khat", sub, hg)])
            for sub in range(4):
                if not is_ctx:
                    for hg in range(2):
                        def amm(e, sub=sub, hg=hg):
                            for hh in range(4):
                                h = hg * 4 + hh
                                ins = e.matmul(PB[3][:, hh * 128:(hh + 1) * 128], lhsT=kt[:, h, sub * 128:(sub + 1) * 128], rhs=qt[:, h, sub * 128:(sub + 1) * 128], start=True, stop=True)
                            return ins
                        S.pe(amm, reads=[("kt", hg * 4 + hh) for hh in range(4)] + [("qt", hg * 4 + hh) for hh in range(4)], writes=[("pb", 3)])
                        S.dve(lambda e, hg=hg: e.tensor_tensor(out=ATs[:, hg * 4:(hg + 1) * 4, :], in0=PB[3][:].rearrange("p (h t) -> p h t", h=4), in1=cm4[:].rearrange("p (h t) -> p h t", h=4), op=ALU.mult),
                              reads=[("pb", 3), "cm4"], writes=[("ATs", hg)])
                    for hg in range(2):
                        def omm(e, sub=sub, hg=hg):
                            for hh in range(4):
                                h = hg * 4 + hh
                                ins = e.matmul(PB[5 + hg][:, hh * 128:(hh + 1) * 128], lhsT=Vt[:, sub, h * 128:(h + 1) * 128], rhs=ATs[:, h, :], start=(hh == 0), stop=False)
                            return ins
                        S.pe(omm, reads=[("V", sub, hg), ("ATs", hg)], writes=[("ot", hg)])
                for c in range(2):
                    ch = sub * 2 + c
                    r0 = c * 64
                    for hg in range(2):
                        if not is_ctx:
                            def sqm(e, sub=sub, hg=hg, c=c):
                                for hh in range(4):
                                    h = hg * 4 + hh
                                    ins = e.matmul(PB[5 + hg][:, hh * 128 + c * 64:hh * 128 + c * 64 + 64], lhsT=Sbf[:, h, :], rhs=qt[:, h, sub * 128 + c * 64:sub * 128 + c * 64 + 64], start=False, stop=(c == 1 and hh == 3))
                                return ins
                            S.pe(sqm, reads=[("Sbf", hg * 4 + hh) for hh in range(4)] + [("qt", hg * 4 + hh) for hh in range(4)], writes=[("ot", hg)])

                        def umm(e, sub=sub, hg=hg, r0=r0):
                            for hh in range(4):
                                h = hg * 4 + hh
                                ins = e.matmul(PB[4][:, hh * 128:(hh + 1) * 128] if hg == 0 else PT_U[:, hh * 128:(hh + 1) * 128], lhsT=khat[r0:r0 + 64, sub, h * 128:(h + 1) * 128], rhs=Vt[r0:r0 + 64, sub, h * 128:(h + 1) * 128], start=True, stop=True)
                            return ins
                        S.pe(umm, reads=[("khat", sub, hg), ("V", sub, hg)], writes=[("pb", 4 if hg == 0 else 2)])
                        for hh in range(4):
                            h = hg * 4 + hh
                            S.dve(lambda e, h=h, hg=hg, hh=hh, ch=ch: e.scalar_tensor_tensor(out=Sst[:, h, :], in0=Sst[:, h, :], scalar=dec[:, h, ch:ch + 1], in1=(PB[4] if hg == 0 else PT_U)[:, hh * 128:(hh + 1) * 128], op0=ALU.mult, op1=ALU.add),
                                  reads=[("pb", 4 if hg == 0 else 2), ("dec", h), ("S", h)], writes=[("S", h)])
                            if (not is_ctx) or (sub == 3 and c == 1):
                                S.act(lambda e, h=h: e.activation(out=Sbf[:, h, :], in_=Sst[:, h, :], func=AF.Copy), reads=[("S", h)], writes=[("Sbf", h)])
                if not is_ctx:
                    for hg in range(2):
                        S.act(lambda e, hg=hg, sub=sub: e.activation(out=OT[:, hg * 4:(hg + 1) * 4, sub * 128:(sub + 1) * 128], in_=PB[5 + hg][:].rearrange("p (h t) -> p h t", h=4), func=AF.Copy),
                              reads=[("ot", hg)], writes=[("OT", hg * 4 + hh) for hh in range(4)])
            if is_ctx:
                return
            for hg in range(2):
                wg = load_w(G_BG + hg)
                for hh in range(4):
                    h = hg * 4 + hh
                    okeys = [("OT", h)]
                    pg = proj_fm(wg, hh, xb, xkey)
                    S.act(lambda e, pg=pg: e.activation(out=sgt[:], in_=PB[pg][:], func=AF.Silu), reads=[("pb", pg)], writes=["sgt"])
                    S.pool(lambda e, h=h: e.tensor_tensor(out=sqb[:], in0=OT[:, h, :], in1=OT[:, h, :], op=ALU.mult), reads=okeys, writes=["sqb"])
                    pbi = next_pb()
                    S.pe(lambda e, pbi=pbi: e.matmul(PB[pbi][:], lhsT=ones_bf[:], rhs=sqb[:], start=True, stop=True), reads=["sqb", "ones_bf"], writes=[("pb", pbi)])
                    S.dve(lambda e, pbi=pbi: e.tensor_scalar(out=gsb[:], in0=PB[pbi][:], scalar1=1.0 / 128.0, scalar2=RMS_EPS, op0=ALU.mult, op1=ALU.add), reads=[("pb", pbi)], writes=["gsb"])
                    S.act(lambda e: e.activation(out=gsb[:], in_=gsb[:], func=AF.Ln), reads=["gsb"], writes=["gsb"])
                    S.act(lambda e: e.activation(out=gsb[:], in_=gsb[:], func=AF.Exp, scale=-0.5), reads=["gsb"], writes=["gsb"])
                    S.dve(lambda e, h=h: e.tensor_tensor(out=OT[:, h, :], in0=OT[:, h, :], in1=gsb[:], op=ALU.mult), reads=okeys + ["gsb"], writes=okeys)
                    S.dve(lambda e, h=h: e.scalar_tensor_tensor(out=obT[:, h, :], in0=OT[:, h, :], scalar=ngv[:, h:h + 1], in1=sgt[:], op0=ALU.mult, op1=ALU.mult),
                          reads=okeys + ["sgt", "ngv"], writes=[("obT", h)])

        PT_U = PB[2]

        def phase_a_proj(tok0, is_ctx, groups, xb, xkey):
            for g in groups:
                if not is_ctx:
                    wq = load_w(3 * g)
                    for hh in range(4):
                        pq = proj_fm(wq, hh, xb, xkey)
                        S.act(lambda e, pq=pq, g=g, hh=hh: e.activation(out=QT[:, g * 4 + hh, :], in_=PB[pq][:], func=AF.Copy, scale=128.0 ** -0.5), reads=[("pb", pq)], writes=[("QT", g * 4 + hh)])
                wk = load_w(3 * g + 1)
                for hh in range(4):
                    pk = proj_fm(wk, hh, xb, xkey)
                    S.dve(lambda e, pk=pk, hh=hh: e.tensor_copy(out=KTt[:, hh, :], in_=PB[pk][:]), reads=[("pb", pk)], writes=[("KTt", hh)])
                S.dma(lambda e, g=g, tok0=tok0: e.dma_start(out=KTd[g, :, :, tok0:tok0 + NT].rearrange("h p t -> p h t"), in_=KTt), reads=[("KTt", hh) for hh in range(4)], writes=[("KTd", g)])
                wv = load_w(3 * g + 2)
                for sub in range(4):
                    pv = proj_tm(wv, sub, xb, xkey)
                    S.act(lambda e, pv=pv, sub=sub: e.activation(out=Vtl[:, sub, :], in_=PB[pv][:], func=AF.Copy), reads=[("pb", pv)], writes=[("Vtl", sub)])
                S.dma(lambda e, g=g, tok0=tok0: e.dma_start(out=Vd[g, tok0:tok0 + NT, :].rearrange("(s p) d -> p s d", p=128), in_=Vtl), reads=[("Vtl", sub) for sub in range(4)], writes=[("Vd", g)])

        def phase_a_attn(tix):
            tok0 = HALF + tix * NT
            kw_n = [0]
            un_ = [0]
            for h in range(4):
                first = True
                for g, (win, r) in enumerate(GROUPS_A):
                    Hh = win
                    W = Hh + NT
                    nqb = min(128, NT // r)
                    ki = kw_n[0] % 2
                    kw_n[0] += 1
                    S.dma(lambda e, g=g, h=h, ki=ki, W=W, Hh=Hh: e.dma_start(out=Kwin[ki][:, 0:W], in_=KTd[g, h, :, tok0 - Hh:tok0 + NT]), reads=[("KTd", g)], writes=[("Kwin", ki)])
                    kwv = Kwin[ki]
                    gname = "g%d" % g
                    units = [(c, 0) for c in range(r)] if r > 1 else [(0, qb) for qb in range(4)]
                    for (c, qb) in units:
                        pA = c + r * 128 * qb
                        pBq = pA + 128 * r
                        q0 = c + 128 * qb
                        if g == 2:
                            var = min(tix, 4)
                        elif g == 1:
                            var = 0 if tix == 0 else 1
                        else:
                            var = 0 if (tix == 0 and qb == 0) else 1
                        offA = A_OFF[gname + "A"] + (var * 4 + h) * nqb
                        offB = A_OFF[gname + "B"] + h * nqb
                        vi = un_[0] % 3
                        un_[0] += 1
                        rowA = tok0 - Hh + pA
                        S.dma(lambda e, g=g, h=h, vi=vi, rowA=rowA, r=r: e.dma_start(out=VA[vi], in_=Vd[g, rowA:rowA + 127 * r + 1:r, h * 128:(h + 1) * 128]), reads=[("Vd", g)], writes=[("VA", vi)])
                        S.dma(lambda e, g=g, h=h, vi=vi, rowA=rowA, r=r, nqb=nqb: e.dma_start(out=VB[vi][0:nqb, :], in_=Vd[g, rowA + 128 * r:rowA + 128 * r + (nqb - 1) * r + 1:r, h * 128:(h + 1) * 128]), reads=[("Vd", g)], writes=[("VB", vi)])
                        ps = next_pb()

                        def smm(e, ps=ps, kwv=kwv, pA=pA, pBq=pBq, r=r, nqb=nqb, g=g, h=h, q0=q0, offA=offA, offB=offB):
                            qv = QT[:, g * 4 + h, q0:q0 + (nqb - 1) * r + 1:r]
                            e.matmul(PB[ps][:, 0:nqb], lhsT=kwv[:, pA:pA + 127 * r + 1:r], rhs=qv, start=True, stop=False)
                            e.matmul(PB[ps][:, 0:nqb], lhsT=ident[:], rhs=ab[:, offA:offA + nqb], start=False, stop=True)
                            e.matmul(PB[ps][0:nqb, 128:128 + nqb], lhsT=kwv[:, pBq:pBq + (nqb - 1) * r + 1:r], rhs=qv, start=True, stop=False)
                            return e.matmul(PB[ps][0:nqb, 128:128 + nqb], lhsT=ident[0:nqb, 0:nqb], rhs=ab[0:nqb, offB:offB + nqb], start=False, stop=True)
                        S.pe(smm, reads=[("Kwin", ki), ("QT", g * 4 + h), "ab", "ident"], writes=[("pb", ps)])
                        pi = vi

                        def efn(e, ps=ps, pi=pi, nqb=nqb):
                            e.activation(out=PTa[pi][:, 0:nqb], in_=PB[ps][:, 0:nqb], func=AF.Exp)
                            return e.activation(out=PTa[pi][0:nqb, 128:128 + nqb], in_=PB[ps][0:nqb, 128:128 + nqb], func=AF.Exp)
                        S.act(efn, reads=[("pb", ps)], writes=[("PTa", pi)])
                        po = next_pb()

                        def omm2(e, po=po, pi=pi, vi=vi, nqb=nqb):
                            e.matmul(PB[po][:, 0:nqb], lhsT=VA[vi], rhs=PTa[pi][:, 0:nqb], start=True, stop=False)
                            e.matmul(PB[po][:, 0:nqb], lhsT=VB[vi][0:nqb, :], rhs=PTa[pi][0:nqb, 128:128 + nqb], start=False, stop=True)
                            e.matmul(PB[po][:, 128:128 + nqb], lhsT=ones_bf[:], rhs=PTa[pi][:, 0:nqb], start=True, stop=False)
                            return e.matmul(PB[po][:, 128:128 + nqb], lhsT=ones_bf[0:nqb, :], rhs=PTa[pi][0:nqb, 128:128 + nqb], start=False, stop=True)
                        S.pe(omm2, reads=[("PTa", pi), ("VA", vi), ("VB", vi), "ones_bf"], writes=[("pb", po)])
                        qsl = slice(q0, q0 + (nqb - 1) * r + 1, r)
                        if first:
                            S.dve(lambda e, po=po, qsl=qsl, nqb=nqb: e.tensor_copy(out=acc_o[:, qsl], in_=PB[po][:, 0:nqb]), reads=[("pb", po)], writes=["acc_o"])
                            S.dve(lambda e, po=po, qsl=qsl, nqb=nqb: e.tensor_copy(out=acc_l[:, qsl], in_=PB[po][:, 128:128 + nqb]), reads=[("pb", po)], writes=["acc_l"])
                        else:
                            S.dve(lambda e, po=po, qsl=qsl, nqb=nqb: e.tensor_tensor(out=acc_o[:, qsl], in0=acc_o[:, qsl], in1=PB[po][:, 0:nqb], op=ALU.add), reads=[("pb", po)], writes=["acc_o"])
                            S.dve(lambda e, po=po, qsl=qsl, nqb=nqb: e.tensor_tensor(out=acc_l[:, qsl], in0=acc_l[:, qsl], in1=PB[po][:, 128:128 + nqb], op=ALU.add), reads=[("pb", po)], writes=["acc_l"])
                    first = False
                S.dve(lambda e: e.reciprocal(out=recA, in_=acc_l), reads=["acc_l"], writes=["recA"])
                S.dve(lambda e, h=h: e.tensor_tensor(out=oaT[:, h, :], in0=acc_o, in1=recA, op=ALU.mult), reads=["acc_o", "recA"], writes=[("oaT", h)])

        def phase_c(xb, xkey):
            wq = load_w(G_C)
            for hc in range(4):
                pq = proj_fm(wq, hc, xb, xkey)
                S.act(lambda e, pq=pq: e.activation(out=qcT, in_=PB[pq][:], func=AF.Copy, scale=128.0 ** -0.5), reads=[("pb", pq)], writes=["qcT"])
                for mc in range(2):
                    ps = next_pb()
                    S.pe(lambda e, ps=ps, hc=hc, mc=mc: e.matmul(PB[ps][:], lhsT=KcT[:, hc, mc * 128:(mc + 1) * 128], rhs=qcT, start=True, stop=True), reads=["KcT", "qcT"], writes=[("pb", ps)])
                    S.act(lambda e, ps=ps, mc=mc: e.activation(out=PcT[:, mc, :], in_=PB[ps][:], func=AF.Exp), reads=[("pb", ps)], writes=[("PcT", mc)])
                po = next_pb()
                pl = next_pb()

                def cmm(e, po=po, hc=hc):
                    e.matmul(PB[po][:], lhsT=Vc[:, 0, hc * 128:(hc + 1) * 128], rhs=PcT[:, 0, :], start=True, stop=False)
                    return e.matmul(PB[po][:], lhsT=Vc[:, 1, hc * 128:(hc + 1) * 128], rhs=PcT[:, 1, :], start=False, stop=True)
                S.pe(cmm, reads=["Vc", ("PcT", 0), ("PcT", 1)], writes=[("pb", po)])

                def lmm(e, pl=pl):
                    e.matmul(PB[pl][:], lhsT=ones_bf[:], rhs=PcT[:, 0, :], start=True, stop=False)
                    return e.matmul(PB[pl][:], lhsT=ones_bf[:], rhs=PcT[:, 1, :], start=False, stop=True)
                S.pe(lmm, reads=["ones_bf", ("PcT", 0), ("PcT", 1)], writes=[("pb", pl)])
                S.dve(lambda e, pl=pl: e.reciprocal(out=recC, in_=PB[pl][:]), reads=[("pb", pl)], writes=["recC"])
                S.dve(lambda e, po=po, hc=hc: e.tensor_tensor(out=ocT[:, hc, :], in0=PB[po][:], in1=recC, op=ALU.mult), reads=[("pb", po), "recC"], writes=[("ocT", hc)])

        def branch_merge(src, nk, wgroup, gate_group0, xb, xkey, srckeys, first):
            if nk == 8:
                wis = [load_w(wgroup), load_w(wgroup + 1)]
            else:
                wis = [load_w(wgroup)]
            for half in range(2):
                wgt = load_w(gate_group0 + half)
                for blk in range(4):
                    fc = half * 4 + blk
                    pgate = proj_fm(wgt, blk, xb, xkey)
                    S.act(lambda e, pgate=pgate: e.activation(out=gsb[:], in_=PB[pgate][:], func=AF.Sigmoid), reads=[("pb", pgate)], writes=["gsb"])
                    pbi = next_pb()
                    if nk == 8:
                        w3 = wview(wis[half], 8)
                        c0 = blk * 128
                        wkey = ("wt", wis[half])
                    else:
                        w3 = wview(wis[0], 4)
                        c0 = fc * 128
                        wkey = ("wt", wis[0])

                    def mmb(e, pbi=pbi, w3=w3, c0=c0):
                        for k in range(nk):
                            ins = e.matmul(PB[pbi][:], lhsT=w3[:, k, c0:c0 + 128], rhs=src[:, k, :], start=(k == 0), stop=(k == nk - 1))
                        return ins
                    S.pe(mmb, reads=[wkey] + srckeys, writes=[("pb", pbi)])
                    if first:
                        S.dve(lambda e, pbi=pbi, fc=fc: e.tensor_tensor(out=mrg[:, fc, :], in0=PB[pbi][:], in1=gsb[:], op=ALU.mult), reads=[("pb", pbi), "gsb"], writes=[("OT", fc)])
                    else:
                        S.dve(lambda e, pbi=pbi: e.tensor_tensor(out=gsb[:], in0=PB[pbi][:], in1=gsb[:], op=ALU.mult), reads=[("pb", pbi), "gsb"], writes=["gsb"])
                        S.pool(lambda e, fc=fc: e.tensor_tensor(out=mrg[:, fc, :], in0=mrg[:, fc, :], in1=gsb[:], op=ALU.add), reads=["gsb", ("OT", fc)], writes=[("OT", fc)])

        def layer_norm(sub, which):
            xk = ("xres", sub)

            def st_(e, sub=sub):
                e.bn_stats(out=bnst[:, 0, :], in_=xres[:, sub, 0:512])
                return e.bn_stats(out=bnst[:, 1, :], in_=xres[:, sub, 512:1024])
            S.dve(st_, reads=[xk], writes=["bnst"])
            S.dve(lambda e: e.bn_aggr(out=mv[:], in_=bnst[:].rearrange("p a b -> p (a b)")), reads=["bnst"], writes=["mv"])
            S.dve(lambda e: e.tensor_scalar(out=rstd[:], in0=mv[:, 1:2], scalar1=LN_EPS, scalar2=None, op0=ALU.add), reads=["mv"], writes=["rstd"])
            S.act(lambda e: e.activation(out=rstd[:], in_=rstd[:], func=AF.Ln), reads=["rstd"], writes=["rstd"])
            S.act(lambda e: e.activation(out=rstd[:], in_=rstd[:], func=AF.Exp, scale=-0.5), reads=["rstd"], writes=["rstd"])
            S.dve(lambda e, sub=sub: e.tensor_scalar(out=xres[:, sub, :], in0=xres[:, sub, :], scalar1=mv[:, 0:1], scalar2=rstd[:], op0=ALU.subtract, op1=ALU.mult), reads=[xk, "mv", "rstd"], writes=[xk])
            S.dve(lambda e, sub=sub: e.tensor_tensor(out=xres[:, sub, :], in0=xres[:, sub, :], in1=lnrow[:, 0, :], op=ALU.mult), reads=[xk, "lnrow"], writes=[xk])
            S.pool(lambda e, sub=sub: e.tensor_tensor(out=xres[:, sub, :], in0=xres[:, sub, :], in1=lnrow[:, 1, :], op=ALU.add), reads=[xk, "lnrow"], writes=[xk])

        def phase_out(tix):
            for fc in range(8):
                S.pool(lambda e, fc=fc: e.tensor_copy(out=mrgb[:, fc, :], in_=mrg[:, fc, :]), reads=[("OT", fc)], writes=[("mrgb", fc)])
            S.dma(lambda e, tix=tix: e.dma_start(out=xres[:], in_=xo[tix * NT:(tix + 1) * NT, :].rearrange("(s p) d -> p s d", p=128)), writes=[("xres", s_) for s_ in range(4)])
            for half in range(2):
                wo_ = load_w(G_WO + half)
                for sub in range(4):
                    pbi = next_pb()
                    w3 = wview(wo_, 8)

                    def mmo(e, pbi=pbi, sub=sub, w3=w3):
                        for kc in range(8):
                            ins = e.matmul(PB[pbi][:], lhsT=mrgb[:, kc, sub * 128:(sub + 1) * 128], rhs=w3[:, kc, :], start=(kc == 0), stop=(kc == 7))
                        return ins
                    S.pe(mmo, reads=[("wt", wo_)] + [("mrgb", fc) for fc in range(8)], writes=[("pb", pbi)])
                    S.dve(lambda e, pbi=pbi, sub=sub, half=half: e.scalar_tensor_tensor(out=xres[:, sub, half * 512:(half + 1) * 512], in0=xres[:, sub, half * 512:(half + 1) * 512], scalar=DN_ALPHA, in1=PB[pbi][:], op0=ALU.mult, op1=ALU.add),
                          reads=[("pb", pbi), ("xres", sub)], writes=[("xres", sub)])
            for sub in range(4):
                layer_norm(sub, 0)
            for sub in range(4):
                gs = tix * 4 + sub
                for half in range(2):
                    pt_ = next_pb()

                    def trx(e, pt_=pt_, sub=sub, half=half):
                        for k in range(4):
                            kc = half * 4 + k
                            ins = e.transpose(PB[pt_][:, k * 128:(k + 1) * 128], xres[:, sub, kc * 128:(kc + 1) * 128], identf[:])
                        return ins
                    S.pe(trx, reads=[("xres", sub), "identf"], writes=[("pb", pt_)])
                    S.act(lambda e, pt_=pt_, half=half, sub=sub: e.activation(out=x1T4[sub % 2][:, half * 4:(half + 1) * 4, :], in_=PB[pt_][:].rearrange("p (k t) -> p k t", k=4), func=AF.Copy), reads=[("pb", pt_)], writes=[("x1T", 0, half)])
                pl_ = next_pb()

                def rmm(e, pl_=pl_, sub=sub):
                    for kc in range(8):
                        ins = e.matmul(PB[pl_][:, 0:36], lhsT=x1T4[sub % 2][:, kc, :], rhs=wr[:, kc, :], start=(kc == 0), stop=(kc == 7))
                    return ins
                S.pe(rmm, reads=[("x1T", 0, 0), ("x1T", 0, 1), "wr"], writes=[("pb", pl_)])
                S.dve(lambda e, pl_=pl_, gs=gs: e.tensor_tensor(out=lg[:, gs, :], in0=PB[pl_][:, 0:36], in1=brow[:], op=ALU.add), reads=[("pb", pl_), "brow"], writes=["lg"])
            S.dma(lambda e, tix=tix: e.dma_start(out=out[tix * NT:(tix + 1) * NT, :].rearrange("(s p) d -> p s d", p=128), in_=xres[:]),
                  reads=[("xres", s_) for s_ in range(4)], writes=[("out", tix * 4 + s_) for s_ in range(4)])

        xn = [0]

        def load_x(tok0):
            i = xn[0] % 2
            xn[0] += 1
            xb = xTb[i]
            S.dma(lambda e, xb=xb, tok0=tok0: e.dma_start(out=xb[:], in_=xT[:, tok0:tok0 + NT].rearrange("(kc p) t -> p kc t", p=128)),
                  writes=[("xTb", i)], queue="pool", nobar=True)
            return xb, ("xTb", i)

        tiles = [(t, True) for t in range(NTILES - n_ctx, NTILES)] + [(t, False) for t in range(n_own)]
        tok_of = lambda t, c: t * NT + (0 if c else HALF)
        nxt = load_x(tok_of(*tiles[0]))
        ecast_n = [0]

        estg = [sb("estg0", [128, 2048], BF16)]
        estg = [estg[0], estg[0]]

        def expert_casts(n):
            for _ in range(n):
                k = ecast_n[0]
                if k >= 3 * N_EXP:
                    return
                ecast_n[0] += 1
                wi, ex = k % 3, k // 3
                i = k % 2
                src = (weg, weu, wed)[wi]
                S.dma(lambda e, src=src, ex=ex, i=i: e.dma_start(out=estg[i][:], in_=src[ex * 128:(ex + 1) * 128, :]), writes=[("estg", 0)], queue="pool", nobar=True)
                S.dma(lambda e, wi=wi, ex=ex, i=i: e.dma_start(out=webf[ex * 128:(ex + 1) * 128, wi * 2048:(wi + 1) * 2048], in_=estg[i][:]), reads=[("estg", 0)], writes=[("webf", k)], nobar=True)

        pending_out = None
        for n_, (t, is_ctx) in enumerate(tiles):
            xb, xkey = nxt
            if n_ + 1 < len(tiles):
                nxt = load_x(tok_of(*tiles[n_ + 1]))
            pb_pool[0] = 3
            if is_ctx:
                expert_casts(1)
                phase_h(t, True, xb, xkey)
                S.barrier()
                expert_casts(1)
                groups = ([2] if t >= 4 else []) + ([1, 0] if t == 7 else [])
                if groups:
                    phase_a_proj(t * NT, True, groups, xb, xkey)
                expert_casts(1)
                S.barrier()
            else:
                expert_casts(1)
                pb_pool[0] = "H"
                wt_pool[0] = [0, 1]
                S.capture()
                phase_h(t, False, xb, xkey)
                hl = S.end_capture()
                wt_pool[0] = [0, 1, 2]
                S.replay([hl] + ([pending_out] if pending_out else []))
                pending_out = None
                S.barrier()
                pb_pool[0] = 7
                expert_casts(1)
                phase_a_proj(HALF + t * NT, False, [0], xb, xkey)
                expert_casts(1)
                phase_a_proj(HALF + t * NT, False, [1], xb, xkey)
                expert_casts(1)
                phase_a_proj(HALF + t * NT, False, [2], xb, xkey)
                expert_casts(1)
                pb_pool[0] = "L2"
                S.capture()
                branch_merge(obT, 8, G_WB, G_GATE + 2, xb, xkey, [("obT", h) for h in range(8)], True)
                phase_c(xb, xkey)
                branch_merge(ocT, 4, G_WC, G_GATE + 4, xb, xkey, [("ocT", h) for h in range(4)], False)
                l2 = S.end_capture()
                pb_pool[0] = "AT"
                S.capture()
                phase_a_attn(t)
                la = S.end_capture()
                S.replay([la, l2])
                pb_pool[0] = 7
                expert_casts(1)
                branch_merge(oaT, 4, G_WA, G_GATE, xb, xkey, [("oaT", h) for h in range(4)], False)
                expert_casts(1)
                S.barrier()
                pb_pool[0] = "P"
                wt_pool[0] = [2]
                S.capture()
                phase_out(t)
                pending_out = S.end_capture()
                wt_pool[0] = [0, 1, 2]
                expert_casts(2)
        if pending_out:
            S.replay([pending_out])
        S.barrier()
        pb_pool[0] = 7
        expert_casts(3 * N_EXP)

        moe_phase(nc, S, locals())
        S.add("sp", lambda e: None, reads=[("out", s_) for s_ in range(NSUB)])
        S.emit(st)
        print("sched stats", S.stats, flush=True)
    return nc


def _consts():
    cm = np.zeros((128, 1024 + 128 + 97), np.float32)
    rm = np.ones(512, np.float32)
    rm[0::64] = 0.0
    cm[:, 0:512] = rm[None, :]
    s = np.arange(128)[:, None]
    t = np.arange(128)[None, :]
    m = ((s // 64) == (t // 64)) & (s <= t)
    cm[:, 512:1024] = np.tile(m.astype(np.float32), (1, 4))
    cm[:, 1024:1152] = (s < t).astype(np.float32)
    cm[:, 1152:1248] = (128.0 * np.arange(96))[None, :]
    cm[:, 1248] = np.arange(128)
    return cm, np.eye(128, dtype=np.float32)


def _abias(rel_bias, half):
    ab = np.full((128, A_NB), NEG, np.float32)
    for g, (win, r) in enumerate(GROUPS_A):
        nqb = min(128, NT // r)
        gname = "g%d" % g
        jj = np.arange(128)[:, None]
        mm = np.arange(nqb)[None, :]
        uA = 128 + mm - jj
        validA = jj >= mm
        bA = _t5_bucket_np(np.clip(uA, 0, 128) * r)
        jb = np.arange(nqb)[:, None]
        uB = mm - jb
        validB = jb <= mm
        bB = _t5_bucket_np(np.clip(uB, 0, 128) * r)
        nvar = 5 if g == 2 else 2
        for h in range(4):
            tabA = np.where(validA, rel_bias[bA, g * 4 + h], NEG).astype(np.float32)
            tabB = np.where(validB, rel_bias[bB, g * 4 + h], NEG).astype(np.float32)
            for var in range(nvar):
                t_ = tabA.copy()
                if half == 0:
                    if g == 2 and var < 4:
                        t_[0:128 - 32 * var, :] = NEG
                    elif g != 2 and var == 0:
                        t_[:, :] = NEG
                o = A_OFF[gname + "A"] + (var * 4 + h) * nqb
                ab[:, o:o + nqb] = t_
            o = A_OFF[gname + "B"] + h * nqb
            ab[0:nqb, o:o + nqb] = tabB
    return ab


def make_in_maps(inputs):
    x = np.asarray(inputs["x"], np.float32)
    mem = np.asarray(inputs["mem"], np.float32)
    cm, ident = _consts()
    vecs = np.zeros((8, D), np.float32)
    vecs[0:2] = inputs["hgrn_lb_logits"]
    vecs[2] = inputs["hgrn_norm_g"][0]
    vecs[3] = inputs["ln1_g"][0]
    vecs[4] = inputs["ln1_b"][0]
    vecs[5] = inputs["ln2_g"][0]
    vecs[6] = inputs["ln2_b"][0]
    wr = np.concatenate([inputs["w_router_group"][0], inputs["w_router_expert"][0]], axis=1).astype(np.float32)
    br = np.concatenate([inputs["b_router_group"][0], inputs["b_router_expert"][0]])[None, :].astype(np.float32)

    def elay(w, kc):
        e_, k_, n_ = w.shape
        return np.ascontiguousarray(w.reshape(e_, kc, 128, n_).transpose(0, 2, 1, 3).reshape(e_ * 128, kc * n_), dtype=np.float32)
    shared = {
        "w_in": np.ascontiguousarray(inputs["w_in"][0], np.float32),
        "w_bb": np.ascontiguousarray(inputs["w_branch_b"][0], np.float32),
        "w_ba": np.ascontiguousarray(inputs["w_branch_a"][0], np.float32),
        "w_bc": np.ascontiguousarray(inputs["w_branch_c"][0], np.float32),
        "w_o": np.ascontiguousarray(inputs["w_out"][0], np.float32),
        "w_kv": np.ascontiguousarray(inputs["w_mem_kv"][0], np.float32),
        "w_r": np.ascontiguousarray(wr.reshape(8, 128, 36).transpose(1, 0, 2)),
        "b_r": br,
        "weg": elay(np.asarray(inputs["w_exp_gate"][0]), 8),
        "weu": elay(np.asarray(inputs["w_exp_up"][0]), 8),
        "wed": elay(np.asarray(inputs["w_exp_down"][0]), 2),
        "vecs": vecs, "cmask": cm, "ident": ident,
        "pvec": np.ascontiguousarray(vecs[0:3].reshape(3, 8, 128).transpose(2, 0, 1)),
    }
    rel_bias = np.asarray(inputs["rel_bias"], np.float32)
    abs_ = [_abias(rel_bias, 0), _abias(rel_bias, 1)]
    maps = []
    for core in range(8):
        b, half = core // 2, core % 2
        own = x[b, half * HALF:(half + 1) * HALF]
        ctx = x[b, 0:HALF] if half == 1 else np.zeros((HALF, D), np.float32)
        m = dict(shared)
        m["xT"] = np.ascontiguousarray(np.concatenate([ctx, own], axis=0).T)
        m["xo"] = np.ascontiguousarray(own)
        m["memT"] = np.ascontiguousarray(mem[b].T)
        m["abias"] = abs_[half]
        maps.append(m)
    return maps


def kernel(**inputs):
    nc = build_nc("full")
    maps = make_in_maps(inputs)
    res = run_bass_kernel_spmd(nc, maps, core_ids=list(range(8)))
    outp = np.zeros((4, SEQ, D), np.float32)
    for core in range(8):
        b, half = core // 2, core % 2
        outp[b, half * HALF:(half + 1) * HALF] = res.results[core]["out"]
    return outp
```

```python
import math
import numpy as np
from contextlib import ExitStack
import concourse.bass as bass
import concourse.mybir as mybir
from concourse.bass_utils import run_bass_kernel_spmd

F32 = mybir.dt.float32
BF16 = mybir.dt.bfloat16
I32 = mybir.dt.int32
ALU = mybir.AluOpType
AF = mybir.ActivationFunctionType
AX = mybir.AxisListType

SEM_EPOCH = 4000
DMA_RING = 8
DMA_EPOCH = 200


class Op:
    __slots__ = ("stream", "fn", "deps", "is_dma", "signal", "sig", "know", "oid")

    def __init__(self, stream, fn, is_dma):
        self.stream = stream
        self.fn = fn
        self.deps = set()
        self.is_dma = is_dma
        self.signal = is_dma
        self.sig = None
        self.know = None


class Sched:
    def __init__(self, nc, same_engine_sync=True):
        self.nc = nc
        self.ops = []
        self.last_w = {}
        self.readers = {}
        self.same_engine_sync = same_engine_sync
        self.last_op = {}
        self.dmas = []
        self._cap = None

    def capture(self):
        self._cap = []

    def end_capture(self):
        c = self._cap
        self._cap = None
        return c

    def replay(self, lists):
        lists = [l for l in lists if l]
        idx = [0] * len(lists)
        while True:
            best = None
            for i, l in enumerate(lists):
                if idx[i] < len(l):
                    frac = idx[i] / len(l)
                    if best is None or frac < best[0]:
                        best = (frac, i)
            if best is None:
                break
            i = best[1]
            stream, fn, reads, writes, dma, nobar = lists[i][idx[i]]
            idx[i] += 1
            self.add(stream, fn, reads, writes, dma, nobar)

    def barrier(self):
        deps = set(v for v in self.last_op.values())
        deps |= set(self.dmas)
        self.dmas = []
        keep = dict(self.last_op)
        for s_ in ("pe", "act", "dve", "pool", "sp"):
            op = self.add(s_, lambda e: None)
            op.deps |= deps
        self.last_op = keep

    def add(self, stream, fn, reads=(), writes=(), dma=False, nobar=False):
        if self._cap is not None:
            self._cap.append((stream, fn, tuple(reads), tuple(writes), dma, nobar))
            return None
        op = Op(stream, fn, dma)
        op.oid = len(self.ops)
        for k in reads:
            w = self.last_w.get(k)
            if w is not None:
                op.deps.add(w)
        for k in writes:
            w = self.last_w.get(k)
            if w is not None:
                op.deps.add(w)
            for r in self.readers.get(k, ()):
                op.deps.add(r)
        for k in writes:
            self.last_w[k] = op.oid
            self.readers[k] = []
        for k in reads:
            self.readers.setdefault(k, []).append(op.oid)
        op.deps.discard(op.oid)
        self.ops.append(op)
        if dma:
            if not nobar:
                self.dmas.append(op.oid)
        else:
            self.last_op[stream] = op.oid
        return op

    def pe(self, fn, reads=(), writes=()):
        return self.add("pe", fn, reads, writes)

    def act(self, fn, reads=(), writes=()):
        return self.add("act", fn, reads, writes)

    def dve(self, fn, reads=(), writes=()):
        return self.add("dve", fn, reads, writes)

    def pool(self, fn, reads=(), writes=()):
        return self.add("pool", fn, reads, writes)

    def dma(self, fn, reads=(), writes=(), queue="sp", nobar=False):
        return self.add(queue, fn, reads, writes, dma=True, nobar=nobar)

    def emit(self, stack):
        nc = self.nc
        ops = self.ops
        for op in ops:
            for d in op.deps:
                dop = ops[d]
                if dop.is_dma:
                    continue
                if dop.stream == op.stream and not op.is_dma:
                    if dop.stream == "pe" or not self.same_engine_sync:
                        continue
                dop.signal = True
        sems = {}
        cnt = {}
        for op in ops:
            if op.is_dma:
                q = op.stream
                j = cnt.get(("dma", q), 0)
                cnt[("dma", q)] = j + 1
                ring = j % DMA_RING
                n = j // DMA_RING
                ep = n // DMA_EPOCH
                op.sig = ("d_%s_%d_%d" % (q, ring, ep), 16 * (n % DMA_EPOCH + 1))
            elif op.signal:
                s = op.stream
                j = cnt.get(s, 0)
                cnt[s] = j + 1
                ep = j // SEM_EPOCH
                op.sig = ("c_%s_%d" % (s, ep), j % SEM_EPOCH + 1)
        know = {s: {} for s in ("pe", "act", "dve", "pool", "sp")}
        plan = {s: [] for s in know}
        dma_hist = {}
        for op in ops:
            s = op.stream
            K = know[s]
            waits = {}
            for d in sorted(op.deps):
                dop = ops[d]
                if not dop.is_dma and dop.stream == s and not op.is_dma:
                    if s == "pe" or not self.same_engine_sync:
                        continue
                if dop.sig is None:
                    continue
                src, val = dop.sig
                if K.get(src, 0) >= val:
                    continue
                if waits.get(src, 0) < val:
                    waits[src] = val
            if op.is_dma:
                hist = dma_hist.setdefault(s, [])
                if len(hist) >= DMA_RING:
                    pop = ops[hist[-DMA_RING]]
                    src, val = pop.sig
                    if K.get(src, 0) < val and waits.get(src, 0) < val:
                        waits[src] = val
                    op.deps.add(pop.oid)
                hist.append(op.oid)
            for d in op.deps:
                dop = ops[d]
                if dop.sig is None or dop.know is None:
                    continue
                src, val = dop.sig
                if waits.get(src, 0) >= val or K.get(src, 0) >= val:
                    for k2, v2 in dop.know.items():
                        if K.get(k2, 0) < v2:
                            K[k2] = v2
            for src, val in waits.items():
                if K.get(src, 0) < val:
                    K[src] = val
            if op.sig is not None:
                kn = dict(K)
                kn[op.sig[0]] = op.sig[1]
                op.know = kn
                if not op.is_dma and (s == "pe" or not self.same_engine_sync):
                    K[op.sig[0]] = op.sig[1]
            plan[s].append((op, sorted(waits.items())))
        for s in plan:
            for op, waits in plan[s]:
                if op.sig is not None and op.sig[0] not in sems:
                    sems[op.sig[0]] = stack.enter_context(nc.semaphore(op.sig[0]))
        block = stack.enter_context(nc.Block())

        def runner(s):
            def body(eng):
                for op, waits in plan[s]:
                    for src, val in waits:
                        eng.wait_ge(sems[src], val)
                    ins = op.fn(eng)
                    if op.sig is not None and ins is not None:
                        ins.then_inc(sems[op.sig[0]], 16 if op.is_dma else 1)
            return body

        block.tensor(runner("pe"))
        block.scalar(runner("act"))
        block.vector(runner("dve"))
        block.gpsimd(runner("pool"))
        block.sync(runner("sp"))
        self.stats = {s: len(plan[s]) for s in plan}
        self.stats["waits"] = sum(len(w) for s in plan for _, w in plan[s])
        self.stats["sems"] = len(sems)


D = 1024
SEQ = 8192
HALF = 4096
NT = 512
NTILES = HALF // NT
N_IN = 12288
COLS_A = 4608
COLS_B = 4096
G_BQ, G_BF, G_BI, G_BG = 9, 11, 13, 15
G_C = 17
G_GATE = 18
G_WB, G_WA, G_WC, G_WO, G_KV = 24, 26, 27, 28, 30
NGROUPS = 32
DN_ALPHA = 2 ** 0.25
LN_EPS = 1e-5
RMS_EPS = 1e-6
GROUPS_A = ((128, 1), (512, 4), (2048, 16))
NEG = -30000.0


def _t5_bucket_np(dist):
    dist = np.asarray(dist, np.int32)
    max_exact = 16
    d = np.maximum(dist, 1).astype(np.float32)
    large = max_exact + (np.log(d / max_exact) / math.log(2048 / max_exact) * (32 - max_exact)).astype(np.int32)
    large = np.minimum(large, 31)
    return np.where(dist < max_exact, dist, large).astype(np.int32)


def moe_phase(nc, S, L):
    PB, PT, HB, HF, lg, ones_bf, ustr, cb, ident = L["PB"], L["PT"], L["HB"], L["HF"], L["lg"], L["ones_bf"], L["ustr"], L["cb"], L["ident"]
    xres, lnrow, out, xbuf, ybuf, weg, weu, wed, vecs = L["xres"], L["lnrow"], L["out"], L["xbuf"], L["ybuf"], L["weg"], L["weu"], L["wed"], L["vecs"]
    N, NBLK, wt, sb, next_pb, layer_norm = L["NSUB"], L["NBLK"], L["wt"], L["sb"], L["next_pb"], L["layer_norm"]
    msm = sb("msm", [128, 1024])
    d1i = sb("d1i", [128, 32], I32)
    d2i = sb("d2i", [128, 32], I32)
    wix = sb("wix", [128, 96], I32)
    S.barrier()
    S.dma(lambda e: e.dma_start(out=lnrow[:], in_=vecs[5:7, :].partition_broadcast(128)), writes=["lnrow"])
    elm = HF[:, 0:N * 32].rearrange("p (s e) -> p s e", e=32)
    oh1 = HF[:, 1024:1024 + N * 32].rearrange("p (s e) -> p s e", e=32)
    oh2 = HF[:, 2048:2048 + N * 32].rearrange("p (s e) -> p s e", e=32)
    rank = HF[:, 3072:3072 + N * 32].rearrange("p (s e) -> p s e", e=32)
    trr = HF[:, 4096:4096 + N * 32].rearrange("p (s e) -> p s e", e=32)
    sm = lambda i, n=32: msm[:, i * 32:i * 32 + n]
    gmax, gsum, gp, m1, m2, w1, w2, d1, d2 = [sm(i, N) for i in range(9)]
    cnt, pc, pend, pstart, t1, one32 = [sm(i) for i in range(9, 15)]
    g1h = msm[:, 480:480 + N * 4].rearrange("p (s g) -> p s g", g=4)
    te = msm[:, 608:608 + N * 4].rearrange("p (s g) -> p s g", g=4)
    be = msm[:, 736:736 + 96]
    Mb = HB[:, 0:N * 32].rearrange("p (s e) -> p s e", e=32)
    Mcum = HB[:, 1024:1024 + (N + 1) * 32].rearrange("p (s e) -> p s e", e=32)
    K_ = ["moe"]
    gl = lg[:, 0:N, 0:4]
    bc = lambda ap, shape: ap.to_broadcast(shape)
    A3 = lambda ap: ap.rearrange("p (s o) -> p s o", o=1)
    S.dve(lambda e: e.tensor_reduce(out=gmax, in_=gl, axis=AX.X, op=ALU.max), reads=["lg"], writes=K_)
    S.dve(lambda e: e.tensor_tensor(out=g1h, in0=gl, in1=bc(A3(gmax), [128, N, 4]), op=ALU.is_equal), reads=K_, writes=K_)
    S.dve(lambda e: e.tensor_tensor(out=te, in0=gl, in1=bc(A3(gmax), [128, N, 4]), op=ALU.subtract), reads=K_, writes=K_)
    S.act(lambda e: e.activation(out=te, in_=te, func=AF.Exp), reads=K_, writes=K_)
    S.dve(lambda e: e.tensor_reduce(out=gsum, in_=te, axis=AX.X, op=ALU.add), reads=K_, writes=K_)
    S.dve(lambda e: e.reciprocal(out=gp, in_=gsum), reads=K_, writes=K_)
    S.dve(lambda e: e.tensor_scalar(out=g1h, in0=g1h, scalar1=BIG, scalar2=-BIG, op0=ALU.mult, op1=ALU.add), reads=K_, writes=K_)
    S.dve(lambda e: e.tensor_copy(out=elm, in_=lg[:, 0:N, 4:36]), reads=K_, writes=K_)
    S.dve(lambda e: e.tensor_tensor(out=elm.rearrange("p s (g e) -> p (s g) e", g=4), in0=elm.rearrange("p s (g e) -> p (s g) e", g=4),
                                    in1=bc(g1h.rearrange("p s (g o) -> p (s g) o", o=1), [128, N * 4, 8]), op=ALU.add), reads=K_, writes=K_)
    S.dve(lambda e: e.tensor_reduce(out=m1, in_=elm, axis=AX.X, op=ALU.max), reads=K_, writes=K_)
    S.dve(lambda e: e.tensor_tensor(out=oh1, in0=elm, in1=bc(A3(m1), [128, N, 32]), op=ALU.is_equal), reads=K_, writes=K_)
    S.dve(lambda e: e.scalar_tensor_tensor(out=elm, in0=oh1, scalar=-BIG, in1=elm, op0=ALU.mult, op1=ALU.add), reads=K_, writes=K_)
    S.dve(lambda e: e.tensor_reduce(out=m2, in_=elm, axis=AX.X, op=ALU.max), reads=K_, writes=K_)
    S.dve(lambda e: e.tensor_tensor(out=oh2, in0=elm, in1=bc(A3(m2), [128, N, 32]), op=ALU.is_equal), reads=K_, writes=K_)
    S.dve(lambda e: e.tensor_tensor(out=w1, in0=m2, in1=m1, op=ALU.subtract), reads=K_, writes=K_)
    S.act(lambda e: e.activation(out=w1, in_=w1, func=AF.Exp), reads=K_, writes=K_)
    S.dve(lambda e: e.tensor_scalar(out=w1, in0=w1, scalar1=1.0, scalar2=None, op0=ALU.add), reads=K_, writes=K_)
    S.dve(lambda e: e.reciprocal(out=w1, in_=w1), reads=K_, writes=K_)
    S.dve(lambda e: e.tensor_scalar(out=w2, in0=w1, scalar1=-1.0, scalar2=1.0, op0=ALU.mult, op1=ALU.add), reads=K_, writes=K_)
    S.dve(lambda e: e.tensor_tensor(out=w1, in0=w1, in1=gp, op=ALU.mult), reads=K_, writes=K_)
    S.dve(lambda e: e.tensor_tensor(out=w2, in0=w2, in1=gp, op=ALU.mult), reads=K_, writes=K_)
    S.dve(lambda e: e.tensor_tensor(out=Mb, in0=oh1, in1=oh2, op=ALU.add), reads=K_, writes=K_)
    S.dve(lambda e: e.memset(Mcum[:, 0, :], 0.0), reads=K_, writes=K_)
    S.dve(lambda e: e.memset(one32, 1.0), reads=K_, writes=K_)
    for s in range(N):
        S.dve(lambda e, s=s: e.tensor_tensor(out=Mcum[:, s + 1, :], in0=Mcum[:, s, :], in1=Mb[:, s, :], op=ALU.add), reads=K_, writes=K_)
    for s0 in range(0, N, 16):
        pr = next_pb()

        def rk(e, s0=s0, pr=pr):
            for s in range(s0, min(N, s0 + 16)):
                e.matmul(PB[pr][:, (s - s0) * 32:(s - s0 + 1) * 32], lhsT=ustr[:], rhs=Mb[:, s, :], start=True, stop=False)
                ins = e.matmul(PB[pr][:, (s - s0) * 32:(s - s0 + 1) * 32], lhsT=ones_bf[:], rhs=Mcum[:, s, :], start=False, stop=True)
            return ins
        S.pe(rk, reads=K_ + ["ustr", "ones_bf"], writes=[("pb", pr)])
        n_ = min(N, s0 + 16) - s0
        S.dve(lambda e, s0=s0, pr=pr, n_=n_: e.tensor_copy(out=rank[:, s0:s0 + n_, :], in_=PB[pr][:, 0:n_ * 32].rearrange("p (s e) -> p s e", e=32)), reads=[("pb", pr)] + K_, writes=K_)
    pcn = next_pb()
    S.pe(lambda e: e.matmul(PB[pcn][:, 0:32], lhsT=ones_bf[:], rhs=Mcum[:, N, :], start=True, stop=True), reads=K_ + ["ones_bf"], writes=[("pb", pcn)])
    S.dve(lambda e: e.tensor_copy(out=cnt, in_=PB[pcn][:, 0:32]), reads=[("pb", pcn)] + K_, writes=K_)
    cmpc = HB[:, 8192:8192 + 2048].rearrange("p (e k) -> p e k", k=64)
    S.dve(lambda e: e.tensor_tensor(out=cmpc, in0=bc(cnt.rearrange("p (e o) -> p e o", o=1), [128, 32, 64]),
                                    in1=bc(cb[:, 0:64].rearrange("p (o k) -> p o k", o=1), [128, 32, 64]), op=ALU.is_gt), reads=K_ + ["cb"], writes=K_)
    S.dve(lambda e: e.tensor_reduce(out=pc, in_=cmpc, axis=AX.X, op=ALU.add), reads=K_, writes=K_)
    S.dve(lambda e: e.tensor_scalar(out=pc, in0=pc, scalar1=128.0, scalar2=None, op0=ALU.mult), reads=K_, writes=K_)
    S.dve(lambda e: e.tensor_tensor_scan(out=pend, data0=one32, data1=pc, initial=0.0, op0=ALU.mult, op1=ALU.add), reads=K_, writes=K_)
    S.dve(lambda e: e.tensor_tensor(out=pstart, in0=pend, in1=pc, op=ALU.subtract), reads=K_, writes=K_)
    psb = lambda: bc(pstart.rearrange("p (o e) -> p o e", o=1), [128, N, 32])
    S.dve(lambda e: e.tensor_tensor(out=rank, in0=rank, in1=psb(), op=ALU.add), reads=K_, writes=K_)
    S.dve(lambda e: e.tensor_tensor(out=trr, in0=rank, in1=oh1, op=ALU.mult), reads=K_, writes=K_)
    S.dve(lambda e: e.tensor_reduce(out=d1, in_=trr, axis=AX.X, op=ALU.add), reads=K_, writes=K_)
    S.dve(lambda e: e.tensor_tensor(out=trr, in0=rank, in1=oh2, op=ALU.mult), reads=K_, writes=K_)
    S.dve(lambda e: e.tensor_reduce(out=d2, in_=trr, axis=AX.X, op=ALU.add), reads=K_, writes=K_)
    S.dve(lambda e: e.tensor_copy(out=d1i[:, 0:N], in_=d1), reads=K_, writes=K_)
    S.dve(lambda e: e.tensor_copy(out=d2i[:, 0:N], in_=d2), reads=K_, writes=K_)
    cmp3 = HF[:, 0:NBLK * 32].rearrange("p (b e) -> p b e", e=32)
    S.dve(lambda e: e.tensor_tensor(out=cmp3, in0=bc(cb[:, 0:NBLK].rearrange("p (b o) -> p b o", o=1), [128, NBLK, 32]),
                                    in1=bc(pend.rearrange("p (o e) -> p o e", o=1), [128, NBLK, 32]), op=ALU.is_ge), reads=K_ + ["cb"], writes=K_)
    S.dve(lambda e: e.tensor_reduce(out=be[:, 0:NBLK], in_=cmp3, axis=AX.X, op=ALU.add), reads=K_, writes=K_)
    S.dve(lambda e: e.tensor_scalar(out=be[:, 0:NBLK], in0=be[:, 0:NBLK], scalar1=31.0, scalar2=128.0, op0=ALU.min, op1=ALU.mult), reads=K_, writes=K_)
    S.dve(lambda e: e.tensor_scalar(out=be[:, 0:NBLK], in0=be[:, 0:NBLK], scalar1=cb[:, 96:97], scalar2=None, op0=ALU.add), reads=K_ + ["cb"], writes=K_)
    S.dve(lambda e: e.tensor_copy(out=wix[:, 0:NBLK], in_=be[:, 0:NBLK]), reads=K_, writes=K_)

    sc_keys = []
    xbr = [HB[:, 4096:5120], HB[:, 5120:6144]]
    for s in range(N):
        k = s % 4
        S.dma(lambda e, s=s, k=k: e.dma_start(out=xres[:, k, :], in_=out[s * 128:(s + 1) * 128, :]), reads=[("out", s)], writes=[("xres", k)])
        S.act(lambda e, s=s, k=k: e.activation(out=xbr[s % 2], in_=xres[:, k, :], func=AF.Copy), reads=[("xres", k)], writes=[("xbr", s % 2)])
        for di in (d1i, d2i):
            S.dma(lambda e, s=s, di=di: e.indirect_dma_start(out=xbuf[:, :], out_offset=bass.IndirectOffsetOnAxis(ap=di[:, s:s + 1], axis=0), in_=xbr[s % 2], in_offset=None),
                  reads=[("xbr", s % 2), "xbuf"] + K_, writes=[("xbufw", s, id(di))], queue="pool")
            sc_keys.append(("xbufw", s, id(di)))

    webf = L["webf"]
    wb3 = [HB[:, 6144:12288], HB[:, 12288:18432], HF[:, 0:3072].bitcast(BF16)]
    wbb = [[wb3[j][:, i * 2048:(i + 1) * 2048] for i in range(3)] for j in range(3)]
    S.barrier()
    xbk2 = [HB[:, 18432:19456], HB[:, 0:1024]]
    xbT2 = [HB[:, 19456:20480].rearrange("p (k t) -> p k t", k=8), HB[:, 1024:2048].rearrange("p (k t) -> p k t", k=8)]
    hT2 = [HB[:, 20480:20736].rearrange("p (n t) -> p n t", n=2), HB[:, 2048:2304].rearrange("p (n t) -> p n t", n=2)]
    sg22 = [HB[:, 20736:20992], HB[:, 2304:2560]]
    yb2 = [HB[:, 20992:22016], HB[:, 2560:3584]]
    def blk_gather(b):
        j3 = b % 3
        S.dma(lambda e, b=b, j3=j3: e.indirect_dma_start(out=wb3[j3], out_offset=None, in_=webf[:, :], in_offset=bass.IndirectOffsetOnAxis(ap=wix[:, b:b + 1], axis=0)),
              reads=K_ + [("webf", k) for k in range(3 * N_EXP)], writes=[("wbb", j3, 0), ("wbb", j3, 1), ("wbb", j3, 2)], queue="pool")

    def blk_front(b):
        j = b % 2
        j3 = b % 3
        xbk, xbT, hT, sg2 = xbk2[j], xbT2[j], hT2[j], sg22[j]
        S.dma(lambda e, b=b, xbk=xbk: e.dma_start(out=xbk, in_=xbuf[b * 128:(b + 1) * 128, :]), reads=["xbuf"] + sc_keys, writes=[("xbk", j)])

        def trb(e, xbk=xbk):
            for k in range(8):
                ins = e.transpose(PT[:, k * 128:(k + 1) * 128], xbk[:, k * 128:(k + 1) * 128], ident[:])
            return ins
        S.pe(trb, reads=[("xbk", j), "ident"], writes=["PT"])
        S.dve(lambda e, xbT=xbT: e.tensor_copy(out=xbT, in_=PT[:].rearrange("p (k t) -> p k t", k=8)), reads=["PT"], writes=[("xbT", j)])
        pg = next_pb()
        wg3 = wbb[j3][0].rearrange("p (k n) -> p k n", k=8)
        wu3 = wbb[j3][1].rearrange("p (k n) -> p k n", k=8)

        def gum(e, pg=pg, wg3=wg3, wu3=wu3, xbT=xbT):
            for q, w3 in enumerate((wg3, wu3)):
                for n_ in range(2):
                    for kc in range(8):
                        ins = e.matmul(PB[pg][:, (q * 2 + n_) * 128:(q * 2 + n_ + 1) * 128], lhsT=w3[:, kc, n_ * 128:(n_ + 1) * 128], rhs=xbT[:, kc, :], start=(kc == 0), stop=(kc == 7))
            return ins
        S.pe(gum, reads=[("wbb", j3, 0), ("wbb", j3, 1), ("xbT", j)], writes=[("pb", pg)])
        S.act(lambda e, pg=pg, sg2=sg2: e.activation(out=sg2, in_=PB[pg][:, 0:256], func=AF.Silu), reads=[("pb", pg)], writes=[("sg2", j)])
        S.dve(lambda e, pg=pg, sg2=sg2, hT=hT: e.tensor_tensor(out=hT, in0=sg2.rearrange("p (n t) -> p n t", n=2), in1=PB[pg][:, 256:512].rearrange("p (n t) -> p n t", n=2), op=ALU.mult), reads=[("pb", pg), ("sg2", j)], writes=[("hT", j)])

    def blk_back(b):
        j = b % 2
        hT, yb = hT2[j], yb2[j]
        j3 = b % 3
        wd3 = wbb[j3][2].rearrange("p (k n) -> p k n", k=2)
        for half in range(2):
            py = next_pb()

            def ym(e, py=py, half=half, wd3=wd3, hT=hT):
                e.matmul(PB[py][:], lhsT=hT[:, 0, :], rhs=wd3[:, 0, half * 512:(half + 1) * 512], start=True, stop=False)
                return e.matmul(PB[py][:], lhsT=hT[:, 1, :], rhs=wd3[:, 1, half * 512:(half + 1) * 512], start=False, stop=True)
            S.pe(ym, reads=[("hT", j), ("wbb", j3, 2)], writes=[("pb", py)])
            if half == 0:
                S.act(lambda e, py=py, yb=yb: e.activation(out=yb[:, 0:512], in_=PB[py][:], func=AF.Copy), reads=[("pb", py)], writes=[("yb", j, 0)])
            else:
                S.dve(lambda e, py=py, yb=yb: e.tensor_copy(out=yb[:, 512:1024], in_=PB[py][:]), reads=[("pb", py)], writes=[("yb", j, 1)])
        S.dma(lambda e, b=b, yb=yb: e.dma_start(out=ybuf[b * 128:(b + 1) * 128, :], in_=yb), reads=[("yb", j, 0), ("yb", j, 1)], writes=[("ybufw", b)])

    blk_gather(0)
    if NBLK > 1:
        blk_gather(1)
    blk_front(0)
    for b in range(NBLK):
        if b + 2 < NBLK:
            blk_gather(b + 2)
        if b + 1 < NBLK:
            blk_front(b + 1)
        blk_back(b)
    yb_keys = [("ybufw", b) for b in range(NBLK)]
    S.barrier()

    r12 = [HB[:, 0:1024], HB[:, 1024:2048], HB[:, 2048:3072], HB[:, 3072:4096]]

    def cmb_fetch(s):
        k = s % 4
        S.dma(lambda e, s=s, k=k: e.dma_start(out=xres[:, k, :], in_=out[s * 128:(s + 1) * 128, :]), reads=[("out", s)], writes=[("xres", k)])
        for q, di in enumerate((d1i, d2i)):
            S.dma(lambda e, s=s, di=di, q=q: e.indirect_dma_start(out=r12[(s % 2) * 2 + q], out_offset=None, in_=ybuf[:, :], in_offset=bass.IndirectOffsetOnAxis(ap=di[:, s:s + 1], axis=0)),
                  reads=yb_keys + K_, writes=[("r12", (s % 2) * 2 + q)], queue="pool")

    def cmb_compute(s):
        k = s % 4
        ya = HF[:, (s % 2) * 1024:(s % 2 + 1) * 1024]
        S.dve(lambda e, s=s, ya=ya: e.tensor_scalar(out=ya, in0=r12[(s % 2) * 2], scalar1=w1[:, s:s + 1], scalar2=None, op0=ALU.mult), reads=[("r12", (s % 2) * 2)] + K_, writes=[("ya", s % 2)])
        S.dve(lambda e, s=s, ya=ya: e.scalar_tensor_tensor(out=ya, in0=r12[(s % 2) * 2 + 1], scalar=w2[:, s:s + 1], in1=ya, op0=ALU.mult, op1=ALU.add), reads=[("r12", (s % 2) * 2 + 1), ("ya", s % 2)] + K_, writes=[("ya", s % 2)])
        S.dve(lambda e, k=k, ya=ya: e.scalar_tensor_tensor(out=xres[:, k, :], in0=xres[:, k, :], scalar=DN_ALPHA, in1=ya, op0=ALU.mult, op1=ALU.add), reads=[("xres", k), ("ya", s % 2)], writes=[("xres", k)])
        layer_norm(k, 1)
        S.dma(lambda e, s=s, k=k: e.dma_start(out=out[s * 128:(s + 1) * 128, :], in_=xres[:, k, :]), reads=[("xres", k)], writes=[("out", s)])

    for s in range(min(2, N)):
        cmb_fetch(s)
    for s in range(N):
        cmb_compute(s)
        if s + 2 < N:
            cmb_fetch(s + 2)


A_OFF = {"g2A": 0, "g1A": 640, "g0A": 1664, "g2B": 2688, "g1B": 2816, "g0B": 3328}
A_NB = 3840
N_EXP = 32
BIG = 1.0e4


def build_nc(stage="full"):
    quick = stage == "quick"
    n_ctx = 4 if quick else NTILES
    n_own = 2 if quick else NTILES
    NSUB = n_own * 4
    NBLK = (2 * 128 * NSUB + N_EXP * 127 + 127) // 128
    NSLOT = NBLK * 128

    nc = bass.Bass("TRN2", target_bir_lowering=False)
    din = lambda name, shape, dt=F32: nc.dram_tensor(name, list(shape), dt, kind="ExternalInput").ap()
    xT = din("xT", [D, 2 * HALF])
    xo = din("xo", [HALF, D])
    memT = din("memT", [D, 256])
    w_in = din("w_in", [D, N_IN])
    w_bb = din("w_bb", [D, D])
    w_ba = din("w_ba", [512, D])
    w_bc = din("w_bc", [512, D])
    w_o = din("w_o", [D, D])
    w_kv = din("w_kv", [D, D])
    w_r = din("w_r", [128, 8, 36])
    b_r = din("b_r", [1, 36])
    weg = din("weg", [N_EXP * 128, 2048])
    weu = din("weu", [N_EXP * 128, 2048])
    wed = din("wed", [N_EXP * 128, 2048])
    vecs = din("vecs", [8, D])
    pvec = din("pvec", [128, 3, 8])
    cmask = din("cmask", [128, 1024 + 128 + 97])
    ident_in = din("ident", [128, 128])
    abias = din("abias", [128, A_NB])
    out = nc.dram_tensor("out", [HALF, D], F32, kind="ExternalOutput").ap()
    wbf = nc.dram_tensor("wbf", [NGROUPS, 128, 4096], BF16).ap()
    KTd = nc.dram_tensor("KTd", [3, 4, 128, 2 * HALF], BF16).ap()
    Vd = nc.dram_tensor("Vd", [3, 2 * HALF, 512], BF16).ap()
    xbuf = nc.dram_tensor("xbuf", [NSLOT, D], BF16).ap()
    ybuf = nc.dram_tensor("ybuf", [NSLOT, D], BF16).ap()
    webf = nc.dram_tensor("webf", [N_EXP * 128, 6144], BF16).ap()

    with ExitStack() as st:
        def sb(name, shape, dt=F32):
            return st.enter_context(nc.sbuf_tensor("s_" + name, list(shape), dt))

        def psum(name, shape, dt=F32):
            return st.enter_context(nc.psum_tensor("p_" + name, list(shape), dt))

        S = Sched(nc)
        ident = sb("ident", [128, 128], BF16)
        identf = sb("identf", [128, 128])
        rmask = sb("rmask", [128, 512])
        cm4 = sb("cm4", [128, 512], BF16)
        ustr = sb("ustr", [128, 128], BF16)
        cb = sb("cb", [128, 97])
        ones_bf = sb("ones_bf", [128, 128], BF16)
        lbv = sb("lbv", [128, 8])
        omlv = sb("omlv", [128, 8])
        lgt = sb("lgt", [128, 2, 8])
        ngv = sb("ngv", [128, 8])
        lnrow = sb("lnrow", [128, 2, D])
        ab = sb("ab", [128, A_NB], BF16)
        wr = sb("wr", [128, 8, 36])
        brow = sb("brow", [128, 36])
        S.dma(lambda e: e.dma_start(out=ident[:], in_=ident_in), writes=["ident"], queue="pool")
        S.dma(lambda e: e.dma_start(out=identf[:], in_=ident_in), writes=["identf"])
        S.dma(lambda e: e.dma_start(out=rmask[:], in_=cmask[:, 0:512]), writes=["rmask"])
        S.dma(lambda e: e.dma_start(out=cm4[:], in_=cmask[:, 512:1024]), writes=["cm4"], queue="pool")
        S.dma(lambda e: e.dma_start(out=ustr[:], in_=cmask[:, 1024:1152]), writes=["ustr"], queue="pool")
        S.dma(lambda e: e.dma_start(out=cb[:], in_=cmask[:, 1152:1249]), writes=["cb"])
        S.dma(lambda e: e.dma_start(out=ab[:], in_=abias), writes=["ab"], queue="pool")
        S.dma(lambda e: e.dma_start(out=wr[:], in_=w_r), writes=["wr"])
        S.dma(lambda e: e.dma_start(out=brow[:], in_=b_r.partition_broadcast(128)), writes=["brow"])
        S.pool(lambda e: e.memset(ones_bf[:], 1.0), writes=["ones_bf"])
        S.dma(lambda e: e.dma_start(out=lgt[:], in_=pvec[:, 0:2, :]), writes=["lgt"])
        S.dma(lambda e: e.dma_start(out=ngv[:], in_=pvec[:, 2, :]), writes=["ngv"])
        S.dma(lambda e: e.dma_start(out=lnrow[:], in_=vecs[3:5, :].partition_broadcast(128)), writes=["lnrow"])
        S.dve(lambda e: e.tensor_tensor(out=lbv[:], in0=lgt[:, 0, :], in1=lgt[:, 1, :], op=ALU.subtract), reads=["lgt"], writes=["lbv"])
        S.act(lambda e: e.activation(out=lbv[:], in_=lbv[:], func=AF.Sigmoid), reads=["lbv"], writes=["lbv"])
        S.dve(lambda e: e.tensor_scalar(out=omlv[:], in0=lbv[:], scalar1=-1.0, scalar2=1.0, op0=ALU.mult, op1=ALU.add), reads=["lbv"], writes=["omlv"])

        xTb = [sb("xTb0", [128, 8, NT], BF16), sb("xTb1", [128, 8, NT], BF16)]
        wt = [sb("wt%d" % i, [128, 4096], BF16) for i in range(3)]
        srcs = []
        for j in range(24):
            srcs.append((w_in[:, j * 512:(j + 1) * 512].rearrange("(kc p) n -> p kc n", p=128), 8))
        srcs.append((w_bb[:, 0:512].rearrange("(kc p) n -> p kc n", p=128), 8))
        srcs.append((w_bb[:, 512:1024].rearrange("(kc p) n -> p kc n", p=128), 8))
        srcs.append((w_ba.rearrange("(kc p) n -> p kc n", p=128), 4))
        srcs.append((w_bc.rearrange("(kc p) n -> p kc n", p=128), 4))
        srcs.append((w_o[:, 0:512].rearrange("(kc p) n -> p kc n", p=128), 8))
        srcs.append((w_o[:, 512:1024].rearrange("(kc p) n -> p kc n", p=128), 8))
        srcs.append((w_kv[:, 0:512].rearrange("(kc p) n -> p kc n", p=128), 8))
        srcs.append((w_kv[:, 512:1024].rearrange("(kc p) n -> p kc n", p=128), 8))
        first = [30, 31, 11, 12, 13, 14, 9, 10, 15, 16, 20, 21, 24, 25, 28, 29]
        order = first + [g for g in range(NGROUPS) if g not in first]
        for n, g in enumerate(order):
            src, kcs = srcs[g]
            sg_ = wt[n % 3]
            S.dma(lambda e, src=src, sg_=sg_, kcs=kcs: e.dma_start(out=sg_[:].rearrange("p (kc n) -> p kc n", kc=kcs), in_=src),
                  writes=[("wt", n % 3)], queue="pool")
            S.dma(lambda e, g=g, sg_=sg_: e.dma_start(out=wbf[g], in_=sg_[:]), reads=[("wt", n % 3)], writes=[("wbf", g)])

        wt_n = [0]
        PB = [psum("pb%d" % i, [128, 512]) for i in range(7)]
        PT = psum("ptr", [128, 1024], BF16)
        HB = sb("HB", [128, 22528], BF16)
        HF = sb("HF", [128, 5120])
        khatT = HB[:, 0:4096].rearrange("p (h t) -> p h t", h=8)
        kt = HB[:, 4096:8192].rearrange("p (h t) -> p h t", h=8)
        qt = HB[:, 8192:12288].rearrange("p (h t) -> p h t", h=8)
        khat = HB[:, 12288:16384].rearrange("p (s d) -> p s d", s=4)
        Vt = HB[:, 16384:20480].rearrange("p (s d) -> p s d", s=4)
        ATs = HB[:, 20480:21504].rearrange("p (h t) -> p h t", h=8)
        tmp = [[HF[:, (i * 5 + j) * 512:(i * 5 + j + 1) * 512] for j in range(5)] for i in range(2)]
        QT = HB[:, 0:6144].rearrange("p (g t) -> p g t", g=12)
        Kwin = [HB[:, 6144:8704], HB[:, 8704:11264]]
        VA = [HB[:, 11264:11392], HB[:, 11392:11520], HB[:, 22016:22144]]
        VB = [HB[:, 11520:11648], HB[:, 11648:11776], HB[:, 22144:22272]]
        PTa = [HB[:, 11776:12032], HB[:, 12032:12288], HB[:, 22272:22528]]
        oaT = HB[:, 12288:14336].rearrange("p (h t) -> p h t", h=4)
        ocT = HB[:, 14336:16384].rearrange("p (h t) -> p h t", h=4)
        qcT = HB[:, 16384:16896]
        PcT = HB[:, 16896:17920].rearrange("p (m t) -> p m t", m=2)
        KTt = HB[:, 17920:19968].rearrange("p (h t) -> p h t", h=4)
        Vtl = HB[:, 19968:22016].rearrange("p (s d) -> p s d", s=4)
        acc_o = HF[:, 0:512]
        acc_l = HF[:, 512:1024]
        recA = HF[:, 1024:1536]
        x1Ts = sb("x1Ts", [128, 8, 128])
        x1T4 = [x1Ts, x1Ts]
        Sst = sb("Sst", [128, 8, 128])
        Sbf = sb("Sbf", [128, 8, 128], BF16)
        dec = sb("dec", [128, 8, 8])
        OT = sb("OT", [128, 8, NT])
        mrg = OT
        sgt = sb("sgt", [128, NT], BF16)
        obT = sb("obT", [128, 8, NT], BF16)
        mrgb = sb("mrgb", [128, 8, NT], BF16)
        gsb = sb("gsb", [128, NT])
        xres = sb("xres", [128, 4, D])
        bnst = sb("bnst", [128, 2, 6])
        mv = sb("mv", [128, 2])
        rstd = sb("rstd", [128, 1])
        sqb = sb("sqb", [128, NT], BF16)
        KcT = sb("KcT", [128, 4, 256], BF16)
        Vc = sb("Vc", [128, 2, 512], BF16)
        lg = sb("lg", [128, 32, 36])

        S.pool(lambda e: e.memset(gsb[:], 0.0), writes=["gsb"])
        S.dma(lambda e: e.dma_start(out=xbuf.rearrange("(b p) d -> p b d", p=128), in_=gsb[:].bitcast(BF16).rearrange("p (o d) -> p o d", o=1).to_broadcast([128, NBLK, 1024])), reads=["gsb"], writes=["xbuf"], nobar=True)
        S.pool(lambda e: e.memset(Sst[:], 0.0), writes=[("S", h) for h in range(8)])
        S.pool(lambda e: e.memset(Sbf[:], 0.0), writes=[("Sbf", h) for h in range(8)])

        wt_pool = [[0, 1, 2]]

        def load_w(g):
            i = wt_pool[0][wt_n[0] % len(wt_pool[0])]
            wt_n[0] += 1
            S.dma(lambda e, g=g, i=i: e.dma_start(out=wt[i][:], in_=wbf[g]), reads=[("wbf", g)], writes=[("wt", i)], nobar=True)
            return i

        def wview(i, kcs):
            return wt[i][:].rearrange("p (kc n) -> p kc n", kc=kcs)

        pb_n = [0]

        pb_pool = [3]
        POOLS = {3: [0, 1, 2], 7: [0, 1, 2, 3, 4, 5, 6], "H": [0, 2], "P": [1]}

        def next_pb():
            pl = POOLS[pb_pool[0]]
            pbi = pl[pb_n[0] % len(pl)]
            pb_n[0] += 1
            return pbi

        def proj_fm(wi, blk, xb, xkey, ncols=NT):
            pbi = next_pb()
            w3 = wview(wi, 8)

            def mm(e, pbi=pbi, blk=blk, xb=xb, w3=w3):
                for kc in range(8):
                    ins = e.matmul(PB[pbi][:, 0:ncols], lhsT=w3[:, kc, blk * 128:(blk + 1) * 128], rhs=xb[:, kc, 0:ncols], start=(kc == 0), stop=(kc == 7))
                return ins
            S.pe(mm, reads=[("wt", wi), xkey], writes=[("pb", pbi)])
            return pbi

        def proj_tm(wi, sub, xb, xkey):
            pbi = next_pb()
            w3 = wview(wi, 8)

            def mm(e, pbi=pbi, sub=sub, xb=xb, w3=w3):
                for kc in range(8):
                    ins = e.matmul(PB[pbi][:], lhsT=xb[:, kc, sub * 128:(sub + 1) * 128], rhs=w3[:, kc, :], start=(kc == 0), stop=(kc == 7))
                return ins
            S.pe(mm, reads=[("wt", wi), xkey], writes=[("pb", pbi)])
            return pbi

        memb = HB[:, 0:2048].rearrange("p (k m) -> p k m", k=8)
        S.dma(lambda e: e.dma_start(out=memb, in_=memT.rearrange("(kc p) m -> p kc m", p=128)), writes=["memb"], queue="pool")
        wk_ = load_w(G_KV)
        for hc in range(4):
            pk = proj_fm(wk_, hc, memb, "memb", ncols=256)
            S.act(lambda e, pk=pk, hc=hc: e.activation(out=KcT[:, hc, :], in_=PB[pk][:, 0:256], func=AF.Copy), reads=[("pb", pk)], writes=["KcT"])
        wv_ = load_w(G_KV + 1)
        for ms in range(2):
            pv = proj_tm(wv_, ms, memb, "memb")
            S.act(lambda e, pv=pv, ms=ms: e.activation(out=Vc[:, ms, :], in_=PB[pv][:], func=AF.Copy), reads=[("pb", pv)], writes=["Vc"])
        S.barrier()

        def phase_h(tix, is_ctx, xb, xkey):
            for hg in range(2):
                wf = load_w(G_BF + hg)
                wq = load_w(G_BQ + hg) if not is_ctx else None
                for pr_ in range(2):
                    hs = [hg * 4 + pr_ * 2, hg * 4 + pr_ * 2 + 1]
                    Ts = {h: tmp[h % 2] for h in hs}
                    tk = lambda h, j: ("tmp", h % 2, j)
                    for h in hs:
                        T = Ts[h]
                        pf = proj_fm(wf, h % 4, xb, xkey)
                        S.act(lambda e, T=T, pf=pf: e.activation(out=T[0], in_=PB[pf][:], func=AF.Sigmoid), reads=[("pb", pf)], writes=[tk(h, 0)])
                        if not is_ctx:
                            pq = proj_fm(wq, h % 4, xb, xkey)
                            S.act(lambda e, T=T, pq=pq: e.activation(out=T[4], in_=PB[pq][:], func=AF.Silu), reads=[("pb", pq)], writes=[tk(h, 4)])
                    for h in hs:
                        T = Ts[h]
                        S.dve(lambda e, T=T, h=h: e.tensor_scalar(out=T[0], in0=T[0], scalar1=omlv[:, h:h + 1], scalar2=lbv[:, h:h + 1], op0=ALU.mult, op1=ALU.add),
                              reads=[tk(h, 0), "lbv", "omlv"], writes=[tk(h, 0)])
                    for h in hs:
                        T = Ts[h]
                        S.act(lambda e, T=T: e.activation(out=T[1], in_=T[0], func=AF.Ln), reads=[tk(h, 0)], writes=[tk(h, 1)])
                    for h in hs:
                        T = Ts[h]
                        S.dve(lambda e, T=T: e.tensor_tensor_scan(out=T[2], data0=rmask[:], data1=T[1], initial=0.0, op0=ALU.mult, op1=ALU.add),
                              reads=[tk(h, 1), "rmask"], writes=[tk(h, 2)])
                        S.pool(lambda e, T=T: e.tensor_scalar(out=T[0], in0=T[0], scalar1=-1.0, scalar2=1.0, op0=ALU.mult, op1=ALU.add), reads=[tk(h, 0)], writes=[tk(h, 0)])

                        def dfn(e, T=T):
                            b3 = T[2].rearrange("p (c s) -> p c s", s=64)
                            return e.tensor_tensor(out=T[3].rearrange("p (c s) -> p c s", s=64), in0=b3[:, :, 63:64].to_broadcast([128, 8, 64]), in1=b3, op=ALU.subtract)
                        S.dve(dfn, reads=[tk(h, 2)], writes=[tk(h, 3)])
                    for h in hs:
                        T = Ts[h]
                        S.act(lambda e, T=T: e.activation(out=T[3], in_=T[3], func=AF.Exp), reads=[tk(h, 3)], writes=[tk(h, 3)])
                        S.act(lambda e, T=T, h=h: e.activation(out=dec[:, h, :], in_=T[2][:, 63::64], func=AF.Exp), reads=[tk(h, 2)], writes=[("dec", h)])
                    for h in hs:
                        T = Ts[h]
                        S.dve(lambda e, T=T, h=h: e.tensor_tensor(out=khatT[:, h, :], in0=T[0], in1=T[3], op=ALU.mult), reads=[tk(h, 0), tk(h, 3)], writes=[("khatT", h)])
                    if not is_ctx:
                        for h in hs:
                            T = Ts[h]
                            S.act(lambda e, T=T: e.activation(out=T[3], in_=T[2], func=AF.Exp, scale=-1.0), reads=[tk(h, 2)], writes=[tk(h, 3)])
                        for h in hs:
                            T = Ts[h]
                            S.pool(lambda e, T=T, h=h: e.tensor_tensor(out=kt[:, h, :], in0=T[0], in1=T[3], op=ALU.mult), reads=[tk(h, 0), tk(h, 3)], writes=[("kt", h)])
                        for h in hs:
                            T = Ts[h]
                            S.act(lambda e, T=T: e.activation(out=T[2], in_=T[2], func=AF.Exp), reads=[tk(h, 2)], writes=[tk(h, 2)])
                        for h in hs:
                            T = Ts[h]
                            S.dve(lambda e, T=T, h=h: e.tensor_tensor(out=qt[:, h, :], in0=T[4], in1=T[2], op=ALU.mult), reads=[tk(h, 4), tk(h, 2)], writes=[("qt", h)])
            for hg in range(2):
                wi_ = load_w(G_BI + hg)
                for sub in range(4):
                    pv = proj_tm(wi_, sub, xb, xkey)
                    S.act(lambda e, pv=pv, sub=sub, hg=hg: e.activation(out=Vt[:, sub, hg * 512:(hg + 1) * 512], in_=PB[pv][:], func=AF.Copy),
                          reads=[("pb", pv)], writes=[("V", sub, hg)])
            for sub in range(4):
                for hg in range(2):
                    def trf(e, sub=sub, hg=hg):
                        for hh in range(4):
                            ins = e.transpose(PT[:, (hg * 4 + hh) * 128:(hg * 4 + hh + 1) * 128], khatT[:, hg * 4 + hh, sub * 128:(sub + 1) * 128], ident[:])
                        return ins
                    S.pe(trf, reads=[("khatT", hg * 4 + hh) for hh in range(4)] + ["ident"], writes=["PT"])
                    S.dve(lambda e, sub=sub, hg=hg: e.tensor_copy(out=khat[:, sub, hg * 512:(hg + 1) * 512], in_=PT[:, hg * 512:(hg + 1) * 512]),
                          reads=["PT"], writes=[("khat", sub, hg)])
            for sub in range(4):
                if not is_ctx:
                    for hg in range(2):
                        def amm(e, sub=sub, hg=hg):
                            for hh in range(4):
                                h = hg * 4 + hh
                                ins = e.matmul(PB[3][:, hh * 128:(hh + 1) * 128], lhsT=kt[:, h, sub * 128:(sub + 1) * 128], rhs=qt[:, h, sub * 128:(sub + 1) * 128], start=True, stop=True)
                            return ins
                        S.pe(amm, reads=[("kt", hg * 4 + hh) for hh in range(4)] + [("qt", hg * 4 + hh) for hh in range(4)], writes=[("pb", 3)])
                        S.dve(lambda e, hg=hg: e.tensor_tensor(out=ATs[:, hg * 4:(hg + 1) * 4, :], in0=PB[3][:].rearrange("p (h t) -> p h t", h=4), in1=cm4[:].rearrange("p (h t) -> p h t", h=4), op=ALU.mult),
                              reads=[("pb", 3), "cm4"], writes=[("ATs", hg)])
                    for hg in range(2):
                        def omm(e, sub=sub, hg=hg):
                            for hh in range(4):
                                h = hg * 4 + hh
                                ins = e.matmul(PB[5 + hg][:, hh * 128:(hh + 1) * 128], lhsT=Vt[:, sub, h * 128:(h + 1) * 128], rhs=ATs[:, h, :], start=(hh == 0), stop=False)
                            return ins
                        S.pe(omm, reads=[("V", sub, hg), ("ATs", hg)], writes=[("ot", hg)])
                for c in range(2):
                    ch = sub * 2 + c
                    r0 = c * 64
                    for hg in range(2):
                        if not is_ctx:
                            def sqm(e, sub=sub, hg=hg, c=c):
                                for hh in range(4):
                                    h = hg * 4 + hh
                                    ins = e.matmul(PB[5 + hg][:, hh * 128 + c * 64:hh * 128 + c * 64 + 64], lhsT=Sbf[:, h, :], rhs=qt[:, h, sub * 128 + c * 64:sub * 128 + c * 64 + 64], start=False, stop=(c == 1 and hh == 3))
                                return ins
                            S.pe(sqm, reads=[("Sbf", hg * 4 + hh) for hh in range(4)] + [("qt", hg * 4 + hh) for hh in range(4)], writes=[("ot", hg)])

                        def umm(e, sub=sub, hg=hg, r0=r0):
                            for hh in range(4):
                                h = hg * 4 + hh
                                ins = e.matmul(PB[4][:, hh * 128:(hh + 1) * 128] if hg == 0 else PT_U[:, hh * 128:(hh + 1) * 128], lhsT=khat[r0:r0 + 64, sub, h * 128:(h + 1) * 128], rhs=Vt[r0:r0 + 64, sub, h * 128:(h + 1) * 128], start=True, stop=True)
                            return ins
                        S.pe(umm, reads=[("khat", sub, hg), ("V", sub, hg)], writes=[("pb", 4 if hg == 0 else 2)])
                        for hh in range(4):
                            h = hg * 4 + hh
                            S.dve(lambda e, h=h, hg=hg, hh=hh, ch=ch: e.scalar_tensor_tensor(out=Sst[:, h, :], in0=Sst[:, h, :], scalar=dec[:, h, ch:ch + 1], in1=(PB[4] if hg == 0 else PT_U)[:, hh * 128:(hh + 1) * 128], op0=ALU.mult, op1=ALU.add),
                                  reads=[("pb", 4 if hg == 0 else 2), ("dec", h), ("S", h)], writes=[("S", h)])
                            if (not is_ctx) or (sub == 3 and c == 1):
                                S.act(lambda e, h=h: e.activation(out=Sbf[:, h, :], in_=Sst[:, h, :], func=AF.Copy), reads=[("S", h)], writes=[("Sbf", h)])
                if not is_ctx:
                    for hg in range(2):
                        S.act(lambda e, hg=hg, sub=sub: e.activation(out=OT[:, hg * 4:(hg + 1) * 4, sub * 128:(sub + 1) * 128], in_=PB[5 + hg][:].rearrange("p (h t) -> p h t", h=4), func=AF.Copy),
                              reads=[("ot", hg)], writes=[("OT", hg * 4 + hh) for hh in range(4)])
            if is_ctx:
                return
            for hg in range(2):
                wg = load_w(G_BG + hg)
                for hh in range(4):
                    h = hg * 4 + hh
                    okeys = [("OT", h)]
                    pg = proj_fm(wg, hh, xb, xkey)
                    S.act(lambda e, pg=pg: e.activation(out=sgt[:], in_=PB[pg][:], func=AF.Silu), reads=[("pb", pg)], writes=["sgt"])
                    S.pool(lambda e, h=h: e.tensor_tensor(out=sqb[:], in0=OT[:, h, :], in1=OT[:, h, :], op=ALU.mult), reads=okeys, writes=["sqb"])
                    pbi = next_pb()
                    S.pe(lambda e, pbi=pbi: e.matmul(PB[pbi][:], lhsT=ones_bf[:], rhs=sqb[:], start=True, stop=True), reads=["sqb", "ones_bf"], writes=[("pb", pbi)])
                    S.dve(lambda e, pbi=pbi: e.tensor_scalar(out=gsb[:], in0=PB[pbi][:], scalar1=1.0 / 128.0, scalar2=RMS_EPS, op0=ALU.mult, op1=ALU.add), reads=[("pb", pbi)], writes=["gsb"])
                    S.act(lambda e: e.activation(out=gsb[:], in_=gsb[:], func=AF.Ln), reads=["gsb"], writes=["gsb"])
                    S.act(lambda e: e.activation(out=gsb[:], in_=gsb[:], func=AF.Exp, scale=-0.5), reads=["gsb"], writes=["gsb"])
                    S.dve(lambda e, h=h: e.tensor_tensor(out=OT[:, h, :], in0=OT[:, h, :], in1=gsb[:], op=ALU.mult), reads=okeys + ["gsb"], writes=okeys)
                    S.dve(lambda e, h=h: e.scalar_tensor_tensor(out=obT[:, h, :], in0=OT[:, h, :], scalar=ngv[:, h:h + 1], in1=sgt[:], op0=ALU.mult, op1=ALU.mult),
                          reads=okeys + ["sgt", "ngv"], writes=[("obT", h)])

        PT_U = PB[2]

        def phase_a_proj(tok0, is_ctx, groups, xb, xkey):
            for g in groups:
                if not is_ctx:
                    wq = load_w(3 * g)
                    for hh in range(4):
                        pq = proj_fm(wq, hh, xb, xkey)
                        S.act(lambda e, pq=pq, g=g, hh=hh: e.activation(out=QT[:, g * 4 + hh, :], in_=PB[pq][:], func=AF.Copy, scale=128.0 ** -0.5), reads=[("pb", pq)], writes=[("QT", g * 4 + hh)])
                wk = load_w(3 * g + 1)
                for hh in range(4):
                    pk = proj_fm(wk, hh, xb, xkey)
                    S.dve(lambda e, pk=pk, hh=hh: e.tensor_copy(out=KTt[:, hh, :], in_=PB[pk][:]), reads=[("pb", pk)], writes=[("KTt", hh)])
                S.dma(lambda e, g=g, tok0=tok0: e.dma_start(out=KTd[g, :, :, tok0:tok0 + NT].rearrange("h p t -> p h t"), in_=KTt), reads=[("KTt", hh) for hh in range(4)], writes=[("KTd", g)])
                wv = load_w(3 * g + 2)
                for sub in range(4):
                    pv = proj_tm(wv, sub, xb, xkey)
                    S.act(lambda e, pv=pv, sub=sub: e.activation(out=Vtl[:, sub, :], in_=PB[pv][:], func=AF.Copy), reads=[("pb", pv)], writes=[("Vtl", sub)])
                S.dma(lambda e, g=g, tok0=tok0: e.dma_start(out=Vd[g, tok0:tok0 + NT, :].rearrange("(s p) d -> p s d", p=128), in_=Vtl), reads=[("Vtl", sub) for sub in range(4)], writes=[("Vd", g)])

        def phase_a_attn(tix):
            tok0 = HALF + tix * NT
            kw_n = [0]
            un_ = [0]
            for h in range(4):
                first = True
                for g, (win, r) in enumerate(GROUPS_A):
                    Hh = win
                    W = Hh + NT
                    nqb = min(128, NT // r)
                    ki = kw_n[0] % 2
                    kw_n[0] += 1
                    S.dma(lambda e, g=g, h=h, ki=ki, W=W, Hh=Hh: e.dma_start(out=Kwin[ki][:, 0:W], in_=KTd[g, h, :, tok0 - Hh:tok0 + NT]), reads=[("KTd", g)], writes=[("Kwin", ki)])
                    kwv = Kwin[ki]
                    gname = "g%d" % g
                    units = [(c, 0) for c in range(r)] if r > 1 else [(0, qb) for qb in range(4)]
                    for (c, qb) in units:
                        pA = c + r * 128 * qb
                        pBq = pA + 128 * r
                        q0 = c + 128 * qb
                        if g == 2:
                            var = min(tix, 4)
                        elif g == 1:
                            var = 0 if tix == 0 else 1
                        else:
                            var = 0 if (tix == 0 and qb == 0) else 1
                        offA = A_OFF[gname + "A"] + (var * 4 + h) * nqb
                        offB = A_OFF[gname + "B"] + h * nqb
                        vi = un_[0] % 3
                        un_[0] += 1
                        rowA = tok0 - Hh + pA
                        S.dma(lambda e, g=g, h=h, vi=vi, rowA=rowA, r=r: e.dma_start(out=VA[vi], in_=Vd[g, rowA:rowA + 127 * r + 1:r, h * 128:(h + 1) * 128]), reads=[("Vd", g)], writes=[("VA", vi)])
                        S.dma(lambda e, g=g, h=h, vi=vi, rowA=rowA, r=r, nqb=nqb: e.dma_start(out=VB[vi][0:nqb, :], in_=Vd[g, rowA + 128 * r:rowA + 128 * r + (nqb - 1) * r + 1:r, h * 128:(h + 1) * 128]), reads=[("Vd", g)], writes=[("VB", vi)])
                        ps = next_pb()

                        def smm(e, ps=ps, kwv=kwv, pA=pA, pBq=pBq, r=r, nqb=nqb, g=g, h=h, q0=q0, offA=offA, offB=offB):
                            qv = QT[:, g * 4 + h, q0:q0 + (nqb - 1) * r + 1:r]
                            e.matmul(PB[ps][:, 0:nqb], lhsT=kwv[:, pA:pA + 127 * r + 1:r], rhs=qv, start=True, stop=False)
                            e.matmul(PB[ps][:, 0:nqb], lhsT=ident[:], rhs=ab[:, offA:offA + nqb], start=False, stop=True)
                            e.matmul(PB[ps][0:nqb, 128:128 + nqb], lhsT=kwv[:, pBq:pBq + (nqb - 1) * r + 1:r], rhs=qv, start=True, stop=False)
                            return e.matmul(PB[ps][0:nqb, 128:128 + nqb], lhsT=ident[0:nqb, 0:nqb], rhs=ab[0:nqb, offB:offB + nqb], start=False, stop=True)
                        S.pe(smm, reads=[("Kwin", ki), ("QT", g * 4 + h), "ab", "ident"], writes=[("pb", ps)])
                        pi = vi

                        def efn(e, ps=ps, pi=pi, nqb=nqb):
                            e.activation(out=PTa[pi][:, 0:nqb], in_=PB[ps][:, 0:nqb], func=AF.Exp)
                            return e.activation(out=PTa[pi][0:nqb, 128:128 + nqb], in_=PB[ps][0:nqb, 128:128 + nqb], func=AF.Exp)
                        S.act(efn, reads=[("pb", ps)], writes=[("PTa", pi)])
                        po = next_pb()

                        def omm2(e, po=po, pi=pi, vi=vi, nqb=nqb):
                            e.matmul(PB[po][:, 0:nqb], lhsT=VA[vi], rhs=PTa[pi][:, 0:nqb], start=True, stop=False)
                            e.matmul(PB[po][:, 0:nqb], lhsT=VB[vi][0:nqb, :], rhs=PTa[pi][0:nqb, 128:128 + nqb], start=False, stop=True)
                            e.matmul(PB[po][:, 128:128 + nqb], lhsT=ones_bf[:], rhs=PTa[pi][:, 0:nqb], start=True, stop=False)
                            return e.matmul(PB[po][:, 128:128 + nqb], lhsT=ones_bf[0:nqb, :], rhs=PTa[pi][0:nqb, 128:128 + nqb], start=False, stop=True)
                        S.pe(omm2, reads=[("PTa", pi), ("VA", vi), ("VB", vi), "ones_bf"], writes=[("pb", po)])
                        qsl = slice(q0, q0 + (nqb - 1) * r + 1, r)
                        if first:
                            S.dve(lambda e, po=po, qsl=qsl, nqb=nqb: e.tensor_copy(out=acc_o[:, qsl], in_=PB[po][:, 0:nqb]), reads=[("pb", po)], writes=["acc_o"])
                            S.dve(lambda e, po=po, qsl=qsl, nqb=nqb: e.tensor_copy(out=acc_l[:, qsl], in_=PB[po][:, 128:128 + nqb]), reads=[("pb", po)], writes=["acc_l"])
                        else:
                            S.dve(lambda e, po=po, qsl=qsl, nqb=nqb: e.tensor_tensor(out=acc_o[:, qsl], in0=acc_o[:, qsl], in1=PB[po][:, 0:nqb], op=ALU.add), reads=[("pb", po)], writes=["acc_o"])
                            S.dve(lambda e, po=po, qsl=qsl, nqb=nqb: e.tensor_tensor(out=acc_l[:, qsl], in0=acc_l[:, qsl], in1=PB[po][:, 128:128 + nqb], op=ALU.add), reads=[("pb", po)], writes=["acc_l"])
                    first = False
                S.dve(lambda e: e.reciprocal(out=recA, in_=acc_l), reads=["acc_l"], writes=["recA"])
                S.dve(lambda e, h=h: e.tensor_tensor(out=oaT[:, h, :], in0=acc_o, in1=recA, op=ALU.mult), reads=["acc_o", "recA"], writes=[("oaT", h)])

        def phase_c(xb, xkey):
            wq = load_w(G_C)
            for hc in range(4):
                pq = proj_fm(wq, hc, xb, xkey)
                S.act(lambda e, pq=pq: e.activation(out=qcT, in_=PB[pq][:], func=AF.Copy, scale=128.0 ** -0.5), reads=[("pb", pq)], writes=["qcT"])
                for mc in range(2):
                    ps = next_pb()
                    S.pe(lambda e, ps=ps, hc=hc, mc=mc: e.matmul(PB[ps][:], lhsT=KcT[:, hc, mc * 128:(mc + 1) * 128], rhs=qcT, start=True, stop=True), reads=["KcT", "qcT"], writes=[("pb", ps)])
                    S.act(lambda e, ps=ps, mc=mc: e.activation(out=PcT[:, mc, :], in_=PB[ps][:], func=AF.Exp), reads=[("pb", ps)], writes=[("PcT", mc)])
                po = next_pb()
                pl = next_pb()

                def cmm(e, po=po, hc=hc):
                    e.matmul(PB[po][:], lhsT=Vc[:, 0, hc * 128:(hc + 1) * 128], rhs=PcT[:, 0, :], start=True, stop=False)
                    return e.matmul(PB[po][:], lhsT=Vc[:, 1, hc * 128:(hc + 1) * 128], rhs=PcT[:, 1, :], start=False, stop=True)
                S.pe(cmm, reads=["Vc", ("PcT", 0), ("PcT", 1)], writes=[("pb", po)])

                def lmm(e, pl=pl):
                    e.matmul(PB[pl][:], lhsT=ones_bf[:], rhs=PcT[:, 0, :], start=True, stop=False)
                    return e.matmul(PB[pl][:], lhsT=ones_bf[:], rhs=PcT[:, 1, :], start=False, stop=True)
                S.pe(lmm, reads=["ones_bf", ("PcT", 0), ("PcT", 1)], writes=[("pb", pl)])
                S.dve(lambda e, pl=pl: e.reciprocal(out=recA, in_=PB[pl][:]), reads=[("pb", pl)], writes=["recA"])
                S.dve(lambda e, po=po, hc=hc: e.tensor_tensor(out=ocT[:, hc, :], in0=PB[po][:], in1=recA, op=ALU.mult), reads=[("pb", po), "recA"], writes=[("ocT", hc)])

        def branch_merge(src, nk, wgroup, gate_group0, xb, xkey, srckeys, first):
            if nk == 8:
                wis = [load_w(wgroup), load_w(wgroup + 1)]
            else:
                wis = [load_w(wgroup)]
            for half in range(2):
                wgt = load_w(gate_group0 + half)
                for blk in range(4):
                    fc = half * 4 + blk
                    pgate = proj_fm(wgt, blk, xb, xkey)
                    S.act(lambda e, pgate=pgate: e.activation(out=gsb[:], in_=PB[pgate][:], func=AF.Sigmoid), reads=[("pb", pgate)], writes=["gsb"])
                    pbi = next_pb()
                    if nk == 8:
                        w3 = wview(wis[half], 8)
                        c0 = blk * 128
                        wkey = ("wt", wis[half])
                    else:
                        w3 = wview(wis[0], 4)
                        c0 = fc * 128
                        wkey = ("wt", wis[0])

                    def mmb(e, pbi=pbi, w3=w3, c0=c0):
                        for k in range(nk):
                            ins = e.matmul(PB[pbi][:], lhsT=w3[:, k, c0:c0 + 128], rhs=src[:, k, :], start=(k == 0), stop=(k == nk - 1))
                        return ins
                    S.pe(mmb, reads=[wkey] + srckeys, writes=[("pb", pbi)])
                    if first:
                        S.dve(lambda e, pbi=pbi, fc=fc: e.tensor_tensor(out=mrg[:, fc, :], in0=PB[pbi][:], in1=gsb[:], op=ALU.mult), reads=[("pb", pbi), "gsb"], writes=[("OT", fc)])
                    else:
                        S.dve(lambda e, pbi=pbi: e.tensor_tensor(out=gsb[:], in0=PB[pbi][:], in1=gsb[:], op=ALU.mult), reads=[("pb", pbi), "gsb"], writes=["gsb"])
                        S.pool(lambda e, fc=fc: e.tensor_tensor(out=mrg[:, fc, :], in0=mrg[:, fc, :], in1=gsb[:], op=ALU.add), reads=["gsb", ("OT", fc)], writes=[("OT", fc)])

        def layer_norm(sub, which):
            xk = ("xres", sub)

            def st_(e, sub=sub):
                e.bn_stats(out=bnst[:, 0, :], in_=xres[:, sub, 0:512])
                return e.bn_stats(out=bnst[:, 1, :], in_=xres[:, sub, 512:1024])
            S.dve(st_, reads=[xk], writes=["bnst"])
            S.dve(lambda e: e.bn_aggr(out=mv[:], in_=bnst[:].rearrange("p a b -> p (a b)")), reads=["bnst"], writes=["mv"])
            S.dve(lambda e: e.tensor_scalar(out=rstd[:], in0=mv[:, 1:2], scalar1=LN_EPS, scalar2=None, op0=ALU.add), reads=["mv"], writes=["rstd"])
            S.act(lambda e: e.activation(out=rstd[:], in_=rstd[:], func=AF.Ln), reads=["rstd"], writes=["rstd"])
            S.act(lambda e: e.activation(out=rstd[:], in_=rstd[:], func=AF.Exp, scale=-0.5), reads=["rstd"], writes=["rstd"])
            S.dve(lambda e, sub=sub: e.tensor_scalar(out=xres[:, sub, :], in0=xres[:, sub, :], scalar1=mv[:, 0:1], scalar2=rstd[:], op0=ALU.subtract, op1=ALU.mult), reads=[xk, "mv", "rstd"], writes=[xk])
            S.dve(lambda e, sub=sub: e.tensor_tensor(out=xres[:, sub, :], in0=xres[:, sub, :], in1=lnrow[:, 0, :], op=ALU.mult), reads=[xk, "lnrow"], writes=[xk])
            S.pool(lambda e, sub=sub: e.tensor_tensor(out=xres[:, sub, :], in0=xres[:, sub, :], in1=lnrow[:, 1, :], op=ALU.add), reads=[xk, "lnrow"], writes=[xk])

        def phase_out(tix):
            for fc in range(8):
                S.pool(lambda e, fc=fc: e.tensor_copy(out=mrgb[:, fc, :], in_=mrg[:, fc, :]), reads=[("OT", fc)], writes=[("mrgb", fc)])
            S.dma(lambda e, tix=tix: e.dma_start(out=xres[:], in_=xo[tix * NT:(tix + 1) * NT, :].rearrange("(s p) d -> p s d", p=128)), writes=[("xres", s_) for s_ in range(4)])
            for half in range(2):
                wo_ = load_w(G_WO + half)
                for sub in range(4):
                    pbi = next_pb()
                    w3 = wview(wo_, 8)

                    def mmo(e, pbi=pbi, sub=sub, w3=w3):
                        for kc in range(8):
                            ins = e.matmul(PB[pbi][:], lhsT=mrgb[:, kc, sub * 128:(sub + 1) * 128], rhs=w3[:, kc, :], start=(kc == 0), stop=(kc == 7))
                        return ins
                    S.pe(mmo, reads=[("wt", wo_)] + [("mrgb", fc) for fc in range(8)], writes=[("pb", pbi)])
                    S.dve(lambda e, pbi=pbi, sub=sub, half=half: e.scalar_tensor_tensor(out=xres[:, sub, half * 512:(half + 1) * 512], in0=xres[:, sub, half * 512:(half + 1) * 512], scalar=DN_ALPHA, in1=PB[pbi][:], op0=ALU.mult, op1=ALU.add),
                          reads=[("pb", pbi), ("xres", sub)], writes=[("xres", sub)])
            for sub in range(4):
                layer_norm(sub, 0)
            for sub in range(4):
                gs = tix * 4 + sub
                for half in range(2):
                    pt_ = next_pb()

                    def trx(e, pt_=pt_, sub=sub, half=half):
                        for k in range(4):
                            kc = half * 4 + k
                            ins = e.transpose(PB[pt_][:, k * 128:(k + 1) * 128], xres[:, sub, kc * 128:(kc + 1) * 128], identf[:])
                        return ins
                    S.pe(trx, reads=[("xres", sub), "identf"], writes=[("pb", pt_)])
                    S.act(lambda e, pt_=pt_, half=half, sub=sub: e.activation(out=x1T4[sub % 2][:, half * 4:(half + 1) * 4, :], in_=PB[pt_][:].rearrange("p (k t) -> p k t", k=4), func=AF.Copy), reads=[("pb", pt_)], writes=[("x1T", 0, half)])
                pl_ = next_pb()

                def rmm(e, pl_=pl_, sub=sub):
                    for kc in range(8):
                        ins = e.matmul(PB[pl_][:, 0:36], lhsT=x1T4[sub % 2][:, kc, :], rhs=wr[:, kc, :], start=(kc == 0), stop=(kc == 7))
                    return ins
                S.pe(rmm, reads=[("x1T", 0, 0), ("x1T", 0, 1), "wr"], writes=[("pb", pl_)])
                S.dve(lambda e, pl_=pl_, gs=gs: e.tensor_tensor(out=lg[:, gs, :], in0=PB[pl_][:, 0:36], in1=brow[:], op=ALU.add), reads=[("pb", pl_), "brow"], writes=["lg"])
            S.dma(lambda e, tix=tix: e.dma_start(out=out[tix * NT:(tix + 1) * NT, :].rearrange("(s p) d -> p s d", p=128), in_=xres[:]),
                  reads=[("xres", s_) for s_ in range(4)], writes=[("out", tix * 4 + s_) for s_ in range(4)])

        xn = [0]

        def load_x(tok0):
            i = xn[0] % 2
            xn[0] += 1
            xb = xTb[i]
            S.dma(lambda e, xb=xb, tok0=tok0: e.dma_start(out=xb[:], in_=xT[:, tok0:tok0 + NT].rearrange("(kc p) t -> p kc t", p=128)),
                  writes=[("xTb", i)], queue="pool", nobar=True)
            return xb, ("xTb", i)

        tiles = [(t, True) for t in range(NTILES - n_ctx, NTILES)] + [(t, False) for t in range(n_own)]
        tok_of = lambda t, c: t * NT + (0 if c else HALF)
        nxt = load_x(tok_of(*tiles[0]))
        ecast_n = [0]

        estg = [sb("estg0", [128, 2048], BF16)]
        estg = [estg[0], estg[0]]

        def expert_casts(n):
            for _ in range(n):
                k = ecast_n[0]
                if k >= 3 * N_EXP:
                    return
                ecast_n[0] += 1
                wi, ex = k % 3, k // 3
                i = k % 2
                src = (weg, weu, wed)[wi]
                S.dma(lambda e, src=src, ex=ex, i=i: e.dma_start(out=estg[i][:], in_=src[ex * 128:(ex + 1) * 128, :]), writes=[("estg", 0)], queue="pool", nobar=True)
                S.dma(lambda e, wi=wi, ex=ex, i=i: e.dma_start(out=webf[ex * 128:(ex + 1) * 128, wi * 2048:(wi + 1) * 2048], in_=estg[i][:]), reads=[("estg", 0)], writes=[("webf", k)], nobar=True)

        pending_out = None
        for n_, (t, is_ctx) in enumerate(tiles):
            xb, xkey = nxt
            if n_ + 1 < len(tiles):
                nxt = load_x(tok_of(*tiles[n_ + 1]))
            pb_pool[0] = 3
            if is_ctx:
                expert_casts(1)
                phase_h(t, True, xb, xkey)
                S.barrier()
                expert_casts(1)
                groups = ([2] if t >= 4 else []) + ([1, 0] if t == 7 else [])
                if groups:
                    phase_a_proj(t * NT, True, groups, xb, xkey)
                expert_casts(1)
                S.barrier()
            else:
                expert_casts(1)
                pb_pool[0] = "H"
                wt_pool[0] = [0, 1]
                S.capture()
                phase_h(t, False, xb, xkey)
                hl = S.end_capture()
                wt_pool[0] = [0, 1, 2]
                S.replay([hl] + ([pending_out] if pending_out else []))
                pending_out = None
                S.barrier()
                pb_pool[0] = 7
                expert_casts(1)
                branch_merge(obT, 8, G_WB, G_GATE + 2, xb, xkey, [("obT", h) for h in range(8)], True)
                expert_casts(1)
                phase_c(xb, xkey)
                expert_casts(1)
                branch_merge(ocT, 4, G_WC, G_GATE + 4, xb, xkey, [("ocT", h) for h in range(4)], False)
                expert_casts(1)
                phase_a_proj(HALF + t * NT, False, [0, 1, 2], xb, xkey)
                expert_casts(1)
                phase_a_attn(t)
                expert_casts(1)
                branch_merge(oaT, 4, G_WA, G_GATE, xb, xkey, [("oaT", h) for h in range(4)], False)
                expert_casts(1)
                S.barrier()
                pb_pool[0] = "P"
                wt_pool[0] = [2]
                S.capture()
                phase_out(t)
                pending_out = S.end_capture()
                wt_pool[0] = [0, 1, 2]
                expert_casts(1)
        if pending_out:
            S.replay([pending_out])
        S.barrier()
        pb_pool[0] = 7
        expert_casts(3 * N_EXP)

        moe_phase(nc, S, locals())
        S.add("sp", lambda e: None, reads=[("out", s_) for s_ in range(NSUB)])
        S.emit(st)
        print("sched stats", S.stats, flush=True)
    return nc


def _consts():
    cm = np.zeros((128, 1024 + 128 + 97), np.float32)
    rm = np.ones(512, np.float32)
    rm[0::64] = 0.0
    cm[:, 0:512] = rm[None, :]
    s = np.arange(128)[:, None]
    t = np.arange(128)[None, :]
    m = ((s // 64) == (t // 64)) & (s <= t)
    cm[:, 512:1024] = np.tile(m.astype(np.float32), (1, 4))
    cm[:, 1024:1152] = (s < t).astype(np.float32)
    cm[:, 1152:1248] = (128.0 * np.arange(96))[None, :]
    cm[:, 1248] = np.arange(128)
    return cm, np.eye(128, dtype=np.float32)


def _abias(rel_bias, half):
    ab = np.full((128, A_NB), NEG, np.float32)
    for g, (win, r) in enumerate(GROUPS_A):
        nqb = min(128, NT // r)
        gname = "g%d" % g
        jj = np.arange(128)[:, None]
        mm = np.arange(nqb)[None, :]
        uA = 128 + mm - jj
        validA = jj >= mm
        bA = _t5_bucket_np(np.clip(uA, 0, 128) * r)
        jb = np.arange(nqb)[:, None]
        uB = mm - jb
        validB = jb <= mm
        bB = _t5_bucket_np(np.clip(uB, 0, 128) * r)
        nvar = 5 if g == 2 else 2
        for h in range(4):
            tabA = np.where(validA, rel_bias[bA, g * 4 + h], NEG).astype(np.float32)
            tabB = np.where(validB, rel_bias[bB, g * 4 + h], NEG).astype(np.float32)
            for var in range(nvar):
                t_ = tabA.copy()
                if half == 0:
                    if g == 2 and var < 4:
                        t_[0:128 - 32 * var, :] = NEG
                    elif g != 2 and var == 0:
                        t_[:, :] = NEG
                o = A_OFF[gname + "A"] + (var * 4 + h) * nqb
                ab[:, o:o + nqb] = t_
            o = A_OFF[gname + "B"] + h * nqb
            ab[0:nqb, o:o + nqb] = tabB
    return ab


def make_in_maps(inputs):
    x = np.asarray(inputs["x"], np.float32)
    mem = np.asarray(inputs["mem"], np.float32)
    cm, ident = _consts()
    vecs = np.zeros((8, D), np.float32)
    vecs[0:2] = inputs["hgrn_lb_logits"]
    vecs[2] = inputs["hgrn_norm_g"][0]
    vecs[3] = inputs["ln1_g"][0]
    vecs[4] = inputs["ln1_b"][0]
    vecs[5] = inputs["ln2_g"][0]
    vecs[6] = inputs["ln2_b"][0]
    wr = np.concatenate([inputs["w_router_group"][0], inputs["w_router_expert"][0]], axis=1).astype(np.float32)
    br = np.concatenate([inputs["b_router_group"][0], inputs["b_router_expert"][0]])[None, :].astype(np.float32)

    def elay(w, kc):
        e_, k_, n_ = w.shape
        return np.ascontiguousarray(w.reshape(e_, kc, 128, n_).transpose(0, 2, 1, 3).reshape(e_ * 128, kc * n_), dtype=np.float32)
    shared = {
        "w_in": np.ascontiguousarray(inputs["w_in"][0], np.float32),
        "w_bb": np.ascontiguousarray(inputs["w_branch_b"][0], np.float32),
        "w_ba": np.ascontiguousarray(inputs["w_branch_a"][0], np.float32),
        "w_bc": np.ascontiguousarray(inputs["w_branch_c"][0], np.float32),
        "w_o": np.ascontiguousarray(inputs["w_out"][0], np.float32),
        "w_kv": np.ascontiguousarray(inputs["w_mem_kv"][0], np.float32),
        "w_r": np.ascontiguousarray(wr.reshape(8, 128, 36).transpose(1, 0, 2)),
        "b_r": br,
        "weg": elay(np.asarray(inputs["w_exp_gate"][0]), 8),
        "weu": elay(np.asarray(inputs["w_exp_up"][0]), 8),
        "wed": elay(np.asarray(inputs["w_exp_down"][0]), 2),
        "vecs": vecs, "cmask": cm, "ident": ident,
        "pvec": np.ascontiguousarray(vecs[0:3].reshape(3, 8, 128).transpose(2, 0, 1)),
    }
    rel_bias = np.asarray(inputs["rel_bias"], np.float32)
    abs_ = [_abias(rel_bias, 0), _abias(rel_bias, 1)]
    maps = []
    for core in range(8):
        b, half = core // 2, core % 2
        own = x[b, half * HALF:(half + 1) * HALF]
        ctx = x[b, 0:HALF] if half == 1 else np.zeros((HALF, D), np.float32)
        m = dict(shared)
        m["xT"] = np.ascontiguousarray(np.concatenate([ctx, own], axis=0).T)
        m["xo"] = np.ascontiguousarray(own)
        m["memT"] = np.ascontiguousarray(mem[b].T)
        m["abias"] = abs_[half]
        maps.append(m)
    return maps


def kernel(**inputs):
    nc = build_nc("full")
    maps = make_in_maps(inputs)
    res = run_bass_kernel_spmd(nc, maps, core_ids=list(range(8)))
    outp = np.zeros((4, SEQ, D), np.float32)
    for core in range(8):
        b, half = core // 2, core % 2
        outp[b, half * HALF:(half + 1) * HALF] = res.results[core]["out"]
    return outp
```

```python
import math
import numpy as np
from contextlib import ExitStack
import concourse.bass as bass
import concourse.mybir as mybir
from concourse.bass_utils import run_bass_kernel_spmd

F32 = mybir.dt.float32
BF16 = mybir.dt.bfloat16
I32 = mybir.dt.int32
ALU = mybir.AluOpType
AF = mybir.ActivationFunctionType
AX = mybir.AxisListType

SEM_EPOCH = 4000
DMA_RING = 8
DMA_EPOCH = 200


class Op:
    __slots__ = ("stream", "fn", "deps", "is_dma", "signal", "sig", "know", "oid")

    def __init__(self, stream, fn, is_dma):
        self.stream = stream
        self.fn = fn
        self.deps = set()
        self.is_dma = is_dma
        self.signal = is_dma
        self.sig = None
        self.know = None


class Sched:
    def __init__(self, nc, same_engine_sync=True):
        self.nc = nc
        self.ops = []
        self.last_w = {}
        self.readers = {}
        self.same_engine_sync = same_engine_sync
        self.last_op = {}
        self.dmas = []
        self._cap = None

    def capture(self):
        self._cap = []

    def end_capture(self):
        c = self._cap
        self._cap = None
        return c

    def replay(self, lists):
        lists = [l for l in lists if l]
        idx = [0] * len(lists)
        while True:
            best = None
            for i, l in enumerate(lists):
                if idx[i] < len(l):
                    frac = idx[i] / len(l)
                    if best is None or frac < best[0]:
                        best = (frac, i)
            if best is None:
                break
            i = best[1]
            stream, fn, reads, writes, dma, nobar = lists[i][idx[i]]
            idx[i] += 1
            self.add(stream, fn, reads, writes, dma, nobar)

    def barrier(self):
        deps = set(v for v in self.last_op.values())
        deps |= set(self.dmas)
        self.dmas = []
        keep = dict(self.last_op)
        for s_ in ("pe", "act", "dve", "pool", "sp"):
            op = self.add(s_, lambda e: None)
            op.deps |= deps
        self.last_op = keep

    def add(self, stream, fn, reads=(), writes=(), dma=False, nobar=False):
        if self._cap is not None:
            self._cap.append((stream, fn, tuple(reads), tuple(writes), dma, nobar))
            return None
        op = Op(stream, fn, dma)
        op.oid = len(self.ops)
        for k in reads:
            w = self.last_w.get(k)
            if w is not None:
                op.deps.add(w)
        for k in writes:
            w = self.last_w.get(k)
            if w is not None:
                op.deps.add(w)
            for r in self.readers.get(k, ()):
                op.deps.add(r)
        for k in writes:
            self.last_w[k] = op.oid
            self.readers[k] = []
        for k in reads:
            self.readers.setdefault(k, []).append(op.oid)
        op.deps.discard(op.oid)
        self.ops.append(op)
        if dma:
            if not nobar:
                self.dmas.append(op.oid)
        else:
            self.last_op[stream] = op.oid
        return op

    def pe(self, fn, reads=(), writes=()):
        return self.add("pe", fn, reads, writes)

    def act(self, fn, reads=(), writes=()):
        return self.add("act", fn, reads, writes)

    def dve(self, fn, reads=(), writes=()):
        return self.add("dve", fn, reads, writes)

    def pool(self, fn, reads=(), writes=()):
        return self.add("pool", fn, reads, writes)

    def dma(self, fn, reads=(), writes=(), queue="sp", nobar=False):
        return self.add(queue, fn, reads, writes, dma=True, nobar=nobar)

    def emit(self, stack):
        nc = self.nc
        ops = self.ops
        for op in ops:
            for d in op.deps:
                dop = ops[d]
                if dop.is_dma:
                    continue
                if dop.stream == op.stream and not op.is_dma:
                    if dop.stream == "pe" or not self.same_engine_sync:
                        continue
                dop.signal = True
        sems = {}
        cnt = {}
        for op in ops:
            if op.is_dma:
                q = op.stream
                j = cnt.get(("dma", q), 0)
                cnt[("dma", q)] = j + 1
                ring = j % DMA_RING
                n = j // DMA_RING
                ep = n // DMA_EPOCH
                op.sig = ("d_%s_%d_%d" % (q, ring, ep), 16 * (n % DMA_EPOCH + 1))
            elif op.signal:
                s = op.stream
                j = cnt.get(s, 0)
                cnt[s] = j + 1
                ep = j // SEM_EPOCH
                op.sig = ("c_%s_%d" % (s, ep), j % SEM_EPOCH + 1)
        know = {s: {} for s in ("pe", "act", "dve", "pool", "sp")}
        plan = {s: [] for s in know}
        dma_hist = {}
        for op in ops:
            s = op.stream
            K = know[s]
            waits = {}
            for d in sorted(op.deps):
                dop = ops[d]
                if not dop.is_dma and dop.stream == s and not op.is_dma:
                    if s == "pe" or not self.same_engine_sync:
                        continue
                if dop.sig is None:
                    continue
                src, val = dop.sig
                if K.get(src, 0) >= val:
                    continue
                if waits.get(src, 0) < val:
                    waits[src] = val
            if op.is_dma:
                hist = dma_hist.setdefault(s, [])
                if len(hist) >= DMA_RING:
                    pop = ops[hist[-DMA_RING]]
                    src, val = pop.sig
                    if K.get(src, 0) < val and waits.get(src, 0) < val:
                        waits[src] = val
                    op.deps.add(pop.oid)
                hist.append(op.oid)
            for d in op.deps:
                dop = ops[d]
                if dop.sig is None or dop.know is None:
                    continue
                src, val = dop.sig
                if waits.get(src, 0) >= val or K.get(src, 0) >= val:
                    for k2, v2 in dop.know.items():
                        if K.get(k2, 0) < v2:
                            K[k2] = v2
            for src, val in waits.items():
                if K.get(src, 0) < val:
                    K[src] = val
            if op.sig is not None:
                kn = dict(K)
                kn[op.sig[0]] = op.sig[1]
                op.know = kn
                if not op.is_dma and (s == "pe" or not self.same_engine_sync):
                    K[op.sig[0]] = op.sig[1]
            plan[s].append((op, sorted(waits.items())))
        for s in plan:
            for op, waits in plan[s]:
                if op.sig is not None and op.sig[0] not in sems:
                    sems[op.sig[0]] = stack.enter_context(nc.semaphore(op.sig[0]))
        block = stack.enter_context(nc.Block())

        def runner(s):
            def body(eng):
                for op, waits in plan[s]:
                    for src, val in waits:
                        eng.wait_ge(sems[src], val)
                    ins = op.fn(eng)
                    if op.sig is not None and ins is not None:
                        ins.then_inc(sems[op.sig[0]], 16 if op.is_dma else 1)
            return body

        block.tensor(runner("pe"))
        block.scalar(runner("act"))
        block.vector(runner("dve"))
        block.gpsimd(runner("pool"))
        block.sync(runner("sp"))
        self.stats = {s: len(plan[s]) for s in plan}
        self.stats["waits"] = sum(len(w) for s in plan for _, w in plan[s])
        self.stats["sems"] = len(sems)


D = 1024
SEQ = 8192
HALF = 4096
NT = 512
NTILES = HALF // NT
N_IN = 12288
COLS_A = 4608
COLS_B = 4096
G_BQ, G_BF, G_BI, G_BG = 9, 11, 13, 15
G_C = 17
G_GATE = 18
G_WB, G_WA, G_WC, G_WO, G_KV = 24, 26, 27, 28, 30
NGROUPS = 32
DN_ALPHA = 2 ** 0.25
LN_EPS = 1e-5
RMS_EPS = 1e-6
GROUPS_A = ((128, 1), (512, 4), (2048, 16))
NEG = -30000.0


def _t5_bucket_np(dist):
    dist = np.asarray(dist, np.int32)
    max_exact = 16
    d = np.maximum(dist, 1).astype(np.float32)
    large = max_exact + (np.log(d / max_exact) / math.log(2048 / max_exact) * (32 - max_exact)).astype(np.int32)
    large = np.minimum(large, 31)
    return np.where(dist < max_exact, dist, large).astype(np.int32)


def moe_phase(nc, S, L):
    PB, PT, HB, HF, lg, ones_bf, ustr, cb, ident = L["PB"], L["PT"], L["HB"], L["HF"], L["lg"], L["ones_bf"], L["ustr"], L["cb"], L["ident"]
    xres, lnrow, out, xbuf, ybuf, weg, weu, wed, vecs = L["xres"], L["lnrow"], L["out"], L["xbuf"], L["ybuf"], L["weg"], L["weu"], L["wed"], L["vecs"]
    N, NBLK, wt, sb, next_pb, layer_norm = L["NSUB"], L["NBLK"], L["wt"], L["sb"], L["next_pb"], L["layer_norm"]
    msm = sb("msm", [128, 1024])
    d1i = sb("d1i", [128, 32], I32)
    d2i = sb("d2i", [128, 32], I32)
    wix = sb("wix", [128, 96], I32)
    S.barrier()
    S.dma(lambda e: e.dma_start(out=lnrow[:], in_=vecs[5:7, :].partition_broadcast(128)), writes=["lnrow"])
    elm = HF[:, 0:N * 32].rearrange("p (s e) -> p s e", e=32)
    oh1 = HF[:, 1024:1024 + N * 32].rearrange("p (s e) -> p s e", e=32)
    oh2 = HF[:, 2048:2048 + N * 32].rearrange("p (s e) -> p s e", e=32)
    rank = HF[:, 3072:3072 + N * 32].rearrange("p (s e) -> p s e", e=32)
    trr = HF[:, 4096:4096 + N * 32].rearrange("p (s e) -> p s e", e=32)
    sm = lambda i, n=32: msm[:, i * 32:i * 32 + n]
    gmax, gsum, gp, m1, m2, w1, w2, d1, d2 = [sm(i, N) for i in range(9)]
    cnt, pc, pend, pstart, t1, one32 = [sm(i) for i in range(9, 15)]
    g1h = msm[:, 480:480 + N * 4].rearrange("p (s g) -> p s g", g=4)
    te = msm[:, 608:608 + N * 4].rearrange("p (s g) -> p s g", g=4)
    be = msm[:, 736:736 + 96]
    Mb = HB[:, 0:N * 32].rearrange("p (s e) -> p s e", e=32)
    Mcum = HB[:, 1024:1024 + (N + 1) * 32].rearrange("p (s e) -> p s e", e=32)
    K_ = ["moe"]
    gl = lg[:, 0:N, 0:4]
    bc = lambda ap, shape: ap.to_broadcast(shape)
    A3 = lambda ap: ap.rearrange("p (s o) -> p s o", o=1)
    S.dve(lambda e: e.tensor_reduce(out=gmax, in_=gl, axis=AX.X, op=ALU.max), reads=["lg"], writes=K_)
    S.dve(lambda e: e.tensor_tensor(out=g1h, in0=gl, in1=bc(A3(gmax), [128, N, 4]), op=ALU.is_equal), reads=K_, writes=K_)
    S.dve(lambda e: e.tensor_tensor(out=te, in0=gl, in1=bc(A3(gmax), [128, N, 4]), op=ALU.subtract), reads=K_, writes=K_)
    S.act(lambda e: e.activation(out=te, in_=te, func=AF.Exp), reads=K_, writes=K_)
    S.dve(lambda e: e.tensor_reduce(out=gsum, in_=te, axis=AX.X, op=ALU.add), reads=K_, writes=K_)
    S.dve(lambda e: e.reciprocal(out=gp, in_=gsum), reads=K_, writes=K_)
    S.dve(lambda e: e.tensor_scalar(out=g1h, in0=g1h, scalar1=BIG, scalar2=-BIG, op0=ALU.mult, op1=ALU.add), reads=K_, writes=K_)
    S.dve(lambda e: e.tensor_copy(out=elm, in_=lg[:, 0:N, 4:36]), reads=K_, writes=K_)
    S.dve(lambda e: e.tensor_tensor(out=elm.rearrange("p s (g e) -> p (s g) e", g=4), in0=elm.rearrange("p s (g e) -> p (s g) e", g=4),
                                    in1=bc(g1h.rearrange("p s (g o) -> p (s g) o", o=1), [128, N * 4, 8]), op=ALU.add), reads=K_, writes=K_)
    S.dve(lambda e: e.tensor_reduce(out=m1, in_=elm, axis=AX.X, op=ALU.max), reads=K_, writes=K_)
    S.dve(lambda e: e.tensor_tensor(out=oh1, in0=elm, in1=bc(A3(m1), [128, N, 32]), op=ALU.is_equal), reads=K_, writes=K_)
    S.dve(lambda e: e.scalar_tensor_tensor(out=elm, in0=oh1, scalar=-BIG, in1=elm, op0=ALU.mult, op1=ALU.add), reads=K_, writes=K_)
    S.dve(lambda e: e.tensor_reduce(out=m2, in_=elm, axis=AX.X, op=ALU.max), reads=K_, writes=K_)
    S.dve(lambda e: e.tensor_tensor(out=oh2, in0=elm, in1=bc(A3(m2), [128, N, 32]), op=ALU.is_equal), reads=K_, writes=K_)
    S.dve(lambda e: e.tensor_tensor(out=w1, in0=m2, in1=m1, op=ALU.subtract), reads=K_, writes=K_)
    S.act(lambda e: e.activation(out=w1, in_=w1, func=AF.Exp), reads=K_, writes=K_)
    S.dve(lambda e: e.tensor_scalar(out=w1, in0=w1, scalar1=1.0, scalar2=None, op0=ALU.add), reads=K_, writes=K_)
    S.dve(lambda e: e.reciprocal(out=w1, in_=w1), reads=K_, writes=K_)
    S.dve(lambda e: e.tensor_scalar(out=w2, in0=w1, scalar1=-1.0, scalar2=1.0, op0=ALU.mult, op1=ALU.add), reads=K_, writes=K_)
    S.dve(lambda e: e.tensor_tensor(out=w1, in0=w1, in1=gp, op=ALU.mult), reads=K_, writes=K_)
    S.dve(lambda e: e.tensor_tensor(out=w2, in0=w2, in1=gp, op=ALU.mult), reads=K_, writes=K_)
    S.dve(lambda e: e.tensor_tensor(out=Mb, in0=oh1, in1=oh2, op=ALU.add), reads=K_, writes=K_)
    S.dve(lambda e: e.memset(Mcum[:, 0, :], 0.0), reads=K_, writes=K_)
    S.dve(lambda e: e.memset(one32, 1.0), reads=K_, writes=K_)
    for s in range(N):
        S.dve(lambda e, s=s: e.tensor_tensor(out=Mcum[:, s + 1, :], in0=Mcum[:, s, :], in1=Mb[:, s, :], op=ALU.add), reads=K_, writes=K_)
    for s0 in range(0, N, 16):
        pr = next_pb()

        def rk(e, s0=s0, pr=pr):
            for s in range(s0, min(N, s0 + 16)):
                e.matmul(PB[pr][:, (s - s0) * 32:(s - s0 + 1) * 32], lhsT=ustr[:], rhs=Mb[:, s, :], start=True, stop=False)
                ins = e.matmul(PB[pr][:, (s - s0) * 32:(s - s0 + 1) * 32], lhsT=ones_bf[:], rhs=Mcum[:, s, :], start=False, stop=True)
            return ins
        S.pe(rk, reads=K_ + ["ustr", "ones_bf"], writes=[("pb", pr)])
        n_ = min(N, s0 + 16) - s0
        S.dve(lambda e, s0=s0, pr=pr, n_=n_: e.tensor_copy(out=rank[:, s0:s0 + n_, :], in_=PB[pr][:, 0:n_ * 32].rearrange("p (s e) -> p s e", e=32)), reads=[("pb", pr)] + K_, writes=K_)
    pcn = next_pb()
    S.pe(lambda e: e.matmul(PB[pcn][:, 0:32], lhsT=ones_bf[:], rhs=Mcum[:, N, :], start=True, stop=True), reads=K_ + ["ones_bf"], writes=[("pb", pcn)])
    S.dve(lambda e: e.tensor_copy(out=cnt, in_=PB[pcn][:, 0:32]), reads=[("pb", pcn)] + K_, writes=K_)
    cmpc = HB[:, 8192:8192 + 2048].rearrange("p (e k) -> p e k", k=64)
    S.dve(lambda e: e.tensor_tensor(out=cmpc, in0=bc(cnt.rearrange("p (e o) -> p e o", o=1), [128, 32, 64]),
                                    in1=bc(cb[:, 0:64].rearrange("p (o k) -> p o k", o=1), [128, 32, 64]), op=ALU.is_gt), reads=K_ + ["cb"], writes=K_)
    S.dve(lambda e: e.tensor_reduce(out=pc, in_=cmpc, axis=AX.X, op=ALU.add), reads=K_, writes=K_)
    S.dve(lambda e: e.tensor_scalar(out=pc, in0=pc, scalar1=128.0, scalar2=None, op0=ALU.mult), reads=K_, writes=K_)
    S.dve(lambda e: e.tensor_tensor_scan(out=pend, data0=one32, data1=pc, initial=0.0, op0=ALU.mult, op1=ALU.add), reads=K_, writes=K_)
    S.dve(lambda e: e.tensor_tensor(out=pstart, in0=pend, in1=pc, op=ALU.subtract), reads=K_, writes=K_)
    psb = lambda: bc(pstart.rearrange("p (o e) -> p o e", o=1), [128, N, 32])
    S.dve(lambda e: e.tensor_tensor(out=rank, in0=rank, in1=psb(), op=ALU.add), reads=K_, writes=K_)
    S.dve(lambda e: e.tensor_tensor(out=trr, in0=rank, in1=oh1, op=ALU.mult), reads=K_, writes=K_)
    S.dve(lambda e: e.tensor_reduce(out=d1, in_=trr, axis=AX.X, op=ALU.add), reads=K_, writes=K_)
    S.dve(lambda e: e.tensor_tensor(out=trr, in0=rank, in1=oh2, op=ALU.mult), reads=K_, writes=K_)
    S.dve(lambda e: e.tensor_reduce(out=d2, in_=trr, axis=AX.X, op=ALU.add), reads=K_, writes=K_)
    S.dve(lambda e: e.tensor_copy(out=d1i[:, 0:N], in_=d1), reads=K_, writes=K_)
    S.dve(lambda e: e.tensor_copy(out=d2i[:, 0:N], in_=d2), reads=K_, writes=K_)
    cmp3 = HF[:, 0:NBLK * 32].rearrange("p (b e) -> p b e", e=32)
    S.dve(lambda e: e.tensor_tensor(out=cmp3, in0=bc(cb[:, 0:NBLK].rearrange("p (b o) -> p b o", o=1), [128, NBLK, 32]),
                                    in1=bc(pend.rearrange("p (o e) -> p o e", o=1), [128, NBLK, 32]), op=ALU.is_ge), reads=K_ + ["cb"], writes=K_)
    S.dve(lambda e: e.tensor_reduce(out=be[:, 0:NBLK], in_=cmp3, axis=AX.X, op=ALU.add), reads=K_, writes=K_)
    S.dve(lambda e: e.tensor_scalar(out=be[:, 0:NBLK], in0=be[:, 0:NBLK], scalar1=31.0, scalar2=128.0, op0=ALU.min, op1=ALU.mult), reads=K_, writes=K_)
    S.dve(lambda e: e.tensor_scalar(out=be[:, 0:NBLK], in0=be[:, 0:NBLK], scalar1=cb[:, 96:97], scalar2=None, op0=ALU.add), reads=K_ + ["cb"], writes=K_)
    S.dve(lambda e: e.tensor_copy(out=wix[:, 0:NBLK], in_=be[:, 0:NBLK]), reads=K_, writes=K_)

    sc_keys = []
    xbr = [HB[:, 4096:5120], HB[:, 5120:6144]]
    for s in range(N):
        k = s % 4
        S.dma(lambda e, s=s, k=k: e.dma_start(out=xres[:, k, :], in_=out[s * 128:(s + 1) * 128, :]), reads=[("out", s)], writes=[("xres", k)])
        S.act(lambda e, s=s, k=k: e.activation(out=xbr[s % 2], in_=xres[:, k, :], func=AF.Copy), reads=[("xres", k)], writes=[("xbr", s % 2)])
        for di in (d1i, d2i):
            S.dma(lambda e, s=s, di=di: e.indirect_dma_start(out=xbuf[:, :], out_offset=bass.IndirectOffsetOnAxis(ap=di[:, s:s + 1], axis=0), in_=xbr[s % 2], in_offset=None),
                  reads=[("xbr", s % 2), "xbuf"] + K_, writes=[("xbufw", s, id(di))], queue="pool")
            sc_keys.append(("xbufw", s, id(di)))

    webf = L["webf"]
    wb3 = [HB[:, 6144:12288], HB[:, 12288:18432], HF[:, 0:3072].bitcast(BF16)]
    wbb = [[wb3[j][:, i * 2048:(i + 1) * 2048] for i in range(3)] for j in range(3)]
    S.barrier()
    xbk2 = [HB[:, 18432:19456], HB[:, 0:1024]]
    xbT2 = [HB[:, 19456:20480].rearrange("p (k t) -> p k t", k=8), HB[:, 1024:2048].rearrange("p (k t) -> p k t", k=8)]
    hT2 = [HB[:, 20480:20736].rearrange("p (n t) -> p n t", n=2), HB[:, 2048:2304].rearrange("p (n t) -> p n t", n=2)]
    sg22 = [HB[:, 20736:20992], HB[:, 2304:2560]]
    yb2 = [HB[:, 20992:22016], HB[:, 2560:3584]]
    def blk_gather(b):
        j3 = b % 3
        S.dma(lambda e, b=b, j3=j3: e.indirect_dma_start(out=wb3[j3], out_offset=None, in_=webf[:, :], in_offset=bass.IndirectOffsetOnAxis(ap=wix[:, b:b + 1], axis=0)),
              reads=K_ + [("webf", k) for k in range(3 * N_EXP)], writes=[("wbb", j3, 0), ("wbb", j3, 1), ("wbb", j3, 2)], queue="pool")

    def blk_front(b):
        j = b % 2
        j3 = b % 3
        xbk, xbT, hT, sg2 = xbk2[j], xbT2[j], hT2[j], sg22[j]
        S.dma(lambda e, b=b, xbk=xbk: e.dma_start(out=xbk, in_=xbuf[b * 128:(b + 1) * 128, :]), reads=["xbuf"] + sc_keys, writes=[("xbk", j)])

        def trb(e, xbk=xbk):
            for k in range(8):
                ins = e.transpose(PT[:, k * 128:(k + 1) * 128], xbk[:, k * 128:(k + 1) * 128], ident[:])
            return ins
        S.pe(trb, reads=[("xbk", j), "ident"], writes=["PT"])
        S.dve(lambda e, xbT=xbT: e.tensor_copy(out=xbT, in_=PT[:].rearrange("p (k t) -> p k t", k=8)), reads=["PT"], writes=[("xbT", j)])
        pg = next_pb()
        wg3 = wbb[j3][0].rearrange("p (k n) -> p k n", k=8)
        wu3 = wbb[j3][1].rearrange("p (k n) -> p k n", k=8)

        def gum(e, pg=pg, wg3=wg3, wu3=wu3, xbT=xbT):
            for q, w3 in enumerate((wg3, wu3)):
                for n_ in range(2):
                    for kc in range(8):
                        ins = e.matmul(PB[pg][:, (q * 2 + n_) * 128:(q * 2 + n_ + 1) * 128], lhsT=w3[:, kc, n_ * 128:(n_ + 1) * 128], rhs=xbT[:, kc, :], start=(kc == 0), stop=(kc == 7))
            return ins
        S.pe(gum, reads=[("wbb", j3, 0), ("wbb", j3, 1), ("xbT", j)], writes=[("pb", pg)])
        S.act(lambda e, pg=pg, sg2=sg2: e.activation(out=sg2, in_=PB[pg][:, 0:256], func=AF.Silu), reads=[("pb", pg)], writes=[("sg2", j)])
        S.dve(lambda e, pg=pg, sg2=sg2, hT=hT: e.tensor_tensor(out=hT, in0=sg2.rearrange("p (n t) -> p n t", n=2), in1=PB[pg][:, 256:512].rearrange("p (n t) -> p n t", n=2), op=ALU.mult), reads=[("pb", pg), ("sg2", j)], writes=[("hT", j)])

    def blk_back(b):
        j = b % 2
        hT, yb = hT2[j], yb2[j]
        j3 = b % 3
        wd3 = wbb[j3][2].rearrange("p (k n) -> p k n", k=2)
        for half in range(2):
            py = next_pb()

            def ym(e, py=py, half=half, wd3=wd3, hT=hT):
                e.matmul(PB[py][:], lhsT=hT[:, 0, :], rhs=wd3[:, 0, half * 512:(half + 1) * 512], start=True, stop=False)
                return e.matmul(PB[py][:], lhsT=hT[:, 1, :], rhs=wd3[:, 1, half * 512:(half + 1) * 512], start=False, stop=True)
            S.pe(ym, reads=[("hT", j), ("wbb", j3, 2)], writes=[("pb", py)])
            if half == 0:
                S.act(lambda e, py=py, yb=yb: e.activation(out=yb[:, 0:512], in_=PB[py][:], func=AF.Copy), reads=[("pb", py)], writes=[("yb", j, 0)])
            else:
                S.dve(lambda e, py=py, yb=yb: e.tensor_copy(out=yb[:, 512:1024], in_=PB[py][:]), reads=[("pb", py)], writes=[("yb", j, 1)])
        S.dma(lambda e, b=b, yb=yb: e.dma_start(out=ybuf[b * 128:(b + 1) * 128, :], in_=yb), reads=[("yb", j, 0), ("yb", j, 1)], writes=[("ybufw", b)])

    blk_gather(0)
    if NBLK > 1:
        blk_gather(1)
    blk_front(0)
    for b in range(NBLK):
        if b + 2 < NBLK:
            blk_gather(b + 2)
        if b + 1 < NBLK:
            blk_front(b + 1)
        blk_back(b)
    yb_keys = [("ybufw", b) for b in range(NBLK)]
    S.barrier()

    r12 = [HB[:, 0:1024], HB[:, 1024:2048], HB[:, 2048:3072], HB[:, 3072:4096]]

    def cmb_fetch(s):
        k = s % 4
        S.dma(lambda e, s=s, k=k: e.dma_start(out=xres[:, k, :], in_=out[s * 128:(s + 1) * 128, :]), reads=[("out", s)], writes=[("xres", k)])
        for q, di in enumerate((d1i, d2i)):
            S.dma(lambda e, s=s, di=di, q=q: e.indirect_dma_start(out=r12[(s % 2) * 2 + q], out_offset=None, in_=ybuf[:, :], in_offset=bass.IndirectOffsetOnAxis(ap=di[:, s:s + 1], axis=0)),
                  reads=yb_keys + K_, writes=[("r12", (s % 2) * 2 + q)], queue="pool")

    def cmb_compute(s):
        k = s % 4
        ya = HF[:, (s % 2) * 1024:(s % 2 + 1) * 1024]
        S.dve(lambda e, s=s, ya=ya: e.tensor_scalar(out=ya, in0=r12[(s % 2) * 2], scalar1=w1[:, s:s + 1], scalar2=None, op0=ALU.mult), reads=[("r12", (s % 2) * 2)] + K_, writes=[("ya", s % 2)])
        S.dve(lambda e, s=s, ya=ya: e.scalar_tensor_tensor(out=ya, in0=r12[(s % 2) * 2 + 1], scalar=w2[:, s:s + 1], in1=ya, op0=ALU.mult, op1=ALU.add), reads=[("r12", (s % 2) * 2 + 1), ("ya", s % 2)] + K_, writes=[("ya", s % 2)])
        S.dve(lambda e, k=k, ya=ya: e.scalar_tensor_tensor(out=xres[:, k, :], in0=xres[:, k, :], scalar=DN_ALPHA, in1=ya, op0=ALU.mult, op1=ALU.add), reads=[("xres", k), ("ya", s % 2)], writes=[("xres", k)])
        layer_norm(k, 1)
        S.dma(lambda e, s=s, k=k: e.dma_start(out=out[s * 128:(s + 1) * 128, :], in_=xres[:, k, :]), reads=[("xres", k)], writes=[("out", s)])

    for s in range(min(2, N)):
        cmb_fetch(s)
    for s in range(N):
        cmb_compute(s)
        if s + 2 < N:
            cmb_fetch(s + 2)


A_OFF = {"g2A": 0, "g1A": 640, "g0A": 1664, "g2B": 2688, "g1B": 2816, "g0B": 3328}
A_NB = 3840
N_EXP = 32
BIG = 1.0e4


def build_nc(stage="full"):
    quick = stage == "quick"
    n_ctx = 4 if quick else NTILES
    n_own = 2 if quick else NTILES
    NSUB = n_own * 4
    NBLK = (2 * 128 * NSUB + N_EXP * 127 + 127) // 128
    NSLOT = NBLK * 128

    nc = bass.Bass("TRN2", target_bir_lowering=False)
    din = lambda name, shape, dt=F32: nc.dram_tensor(name, list(shape), dt, kind="ExternalInput").ap()
    xT = din("xT", [D, 2 * HALF])
    xo = din("xo", [HALF, D])
    memT = din("memT", [D, 256])
    w_in = din("w_in", [D, N_IN])
    w_bb = din("w_bb", [D, D])
    w_ba = din("w_ba", [512, D])
    w_bc = din("w_bc", [512, D])
    w_o = din("w_o", [D, D])
    w_kv = din("w_kv", [D, D])
    w_r = din("w_r", [128, 8, 36])
    b_r = din("b_r", [1, 36])
    weg = din("weg", [N_EXP * 128, 2048])
    weu = din("weu", [N_EXP * 128, 2048])
    wed = din("wed", [N_EXP * 128, 2048])
    vecs = din("vecs", [8, D])
    pvec = din("pvec", [128, 3, 8])
    cmask = din("cmask", [128, 1024 + 128 + 97])
    ident_in = din("ident", [128, 128])
    abias = din("abias", [128, A_NB])
    out = nc.dram_tensor("out", [HALF, D], F32, kind="ExternalOutput").ap()
    wbf = nc.dram_tensor("wbf", [NGROUPS, 128, 4096], BF16).ap()
    KTd = nc.dram_tensor("KTd", [3, 4, 128, 2 * HALF], BF16).ap()
    Vd = nc.dram_tensor("Vd", [3, 2 * HALF, 512], BF16).ap()
    xbuf = nc.dram_tensor("xbuf", [NSLOT, D], BF16).ap()
    ybuf = nc.dram_tensor("ybuf", [NSLOT, D], BF16).ap()
    webf = nc.dram_tensor("webf", [N_EXP * 128, 6144], BF16).ap()

    with ExitStack() as st:
        def sb(name, shape, dt=F32):
            return st.enter_context(nc.sbuf_tensor("s_" + name, list(shape), dt))

        def psum(name, shape, dt=F32):
            return st.enter_context(nc.psum_tensor("p_" + name, list(shape), dt))

        S = Sched(nc)
        ident = sb("ident", [128, 128], BF16)
        identf = sb("identf", [128, 128])
        rmask = sb("rmask", [128, 512])
        cm4 = sb("cm4", [128, 512], BF16)
        ustr = sb("ustr", [128, 128], BF16)
        cb = sb("cb", [128, 97])
        ones_bf = sb("ones_bf", [128, 128], BF16)
        lbv = sb("lbv", [128, 8])
        omlv = sb("omlv", [128, 8])
        lgt = sb("lgt", [128, 2, 8])
        ngv = sb("ngv", [128, 8])
        lnrow = sb("lnrow", [128, 2, D])
        ab = sb("ab", [128, A_NB], BF16)
        wr = sb("wr", [128, 8, 36])
        brow = sb("brow", [128, 36])
        S.dma(lambda e: e.dma_start(out=ident[:], in_=ident_in), writes=["ident"], queue="pool")
        S.dma(lambda e: e.dma_start(out=identf[:], in_=ident_in), writes=["identf"])
        S.dma(lambda e: e.dma_start(out=rmask[:], in_=cmask[:, 0:512]), writes=["rmask"])
        S.dma(lambda e: e.dma_start(out=cm4[:], in_=cmask[:, 512:1024]), writes=["cm4"], queue="pool")
        S.dma(lambda e: e.dma_start(out=ustr[:], in_=cmask[:, 1024:1152]), writes=["ustr"], queue="pool")
        S.dma(lambda e: e.dma_start(out=cb[:], in_=cmask[:, 1152:1249]), writes=["cb"])
        S.dma(lambda e: e.dma_start(out=ab[:], in_=abias), writes=["ab"], queue="pool")
        S.dma(lambda e: e.dma_start(out=wr[:], in_=w_r), writes=["wr"])
        S.dma(lambda e: e.dma_start(out=brow[:], in_=b_r.partition_broadcast(128)), writes=["brow"])
        S.pool(lambda e: e.memset(ones_bf[:], 1.0), writes=["ones_bf"])
        S.dma(lambda e: e.dma_start(out=lgt[:], in_=pvec[:, 0:2, :]), writes=["lgt"])
        S.dma(lambda e: e.dma_start(out=ngv[:], in_=pvec[:, 2, :]), writes=["ngv"])
        S.dma(lambda e: e.dma_start(out=lnrow[:], in_=vecs[3:5, :].partition_broadcast(128)), writes=["lnrow"])
        S.dve(lambda e: e.tensor_tensor(out=lbv[:], in0=lgt[:, 0, :], in1=lgt[:, 1, :], op=ALU.subtract), reads=["lgt"], writes=["lbv"])
        S.act(lambda e: e.activation(out=lbv[:], in_=lbv[:], func=AF.Sigmoid), reads=["lbv"], writes=["lbv"])
        S.dve(lambda e: e.tensor_scalar(out=omlv[:], in0=lbv[:], scalar1=-1.0, scalar2=1.0, op0=ALU.mult, op1=ALU.add), reads=["lbv"], writes=["omlv"])

        xTb = [sb("xTb0", [128, 8, NT], BF16), sb("xTb1", [128, 8, NT], BF16)]
        wt = [sb("wt%d" % i, [128, 4096], BF16) for i in range(3)]
        srcs = []
        for j in range(24):
            srcs.append((w_in[:, j * 512:(j + 1) * 512].rearrange("(kc p) n -> p kc n", p=128), 8))
        srcs.append((w_bb[:, 0:512].rearrange("(kc p) n -> p kc n", p=128), 8))
        srcs.append((w_bb[:, 512:1024].rearrange("(kc p) n -> p kc n", p=128), 8))
        srcs.append((w_ba.rearrange("(kc p) n -> p kc n", p=128), 4))
        srcs.append((w_bc.rearrange("(kc p) n -> p kc n", p=128), 4))
        srcs.append((w_o[:, 0:512].rearrange("(kc p) n -> p kc n", p=128), 8))
        srcs.append((w_o[:, 512:1024].rearrange("(kc p) n -> p kc n", p=128), 8))
        srcs.append((w_kv[:, 0:512].rearrange("(kc p) n -> p kc n", p=128), 8))
        srcs.append((w_kv[:, 512:1024].rearrange("(kc p) n -> p kc n", p=128), 8))
        first = [30, 31, 11, 12, 13, 14, 9, 10, 15, 16, 20, 21, 24, 25, 28, 29]
        order = first + [g for g in range(NGROUPS) if g not in first]
        for n, g in enumerate(order):
            src, kcs = srcs[g]
            sg_ = wt[n % 3]
            S.dma(lambda e, src=src, sg_=sg_, kcs=kcs: e.dma_start(out=sg_[:].rearrange("p (kc n) -> p kc n", kc=kcs), in_=src),
                  writes=[("wt", n % 3)], queue="pool")
            S.dma(lambda e, g=g, sg_=sg_: e.dma_start(out=wbf[g], in_=sg_[:]), reads=[("wt", n % 3)], writes=[("wbf", g)])

        wt_n = [0]
        PB = [psum("pb%d" % i, [128, 512]) for i in range(7)]
        PT = psum("ptr", [128, 1024], BF16)
        HB = sb("HB", [128, 22528], BF16)
        HF = sb("HF", [128, 5120])
        khatT = HB[:, 0:4096].rearrange("p (h t) -> p h t", h=8)
        kt = HB[:, 4096:8192].rearrange("p (h t) -> p h t", h=8)
        qt = HB[:, 8192:12288].rearrange("p (h t) -> p h t", h=8)
        khat = HB[:, 12288:16384].rearrange("p (s d) -> p s d", s=4)
        Vt = HB[:, 16384:20480].rearrange("p (s d) -> p s d", s=4)
        ATs = HB[:, 20480:21504].rearrange("p (h t) -> p h t", h=8)
        tmp = [[HF[:, (i * 5 + j) * 512:(i * 5 + j + 1) * 512] for j in range(5)] for i in range(2)]
        QT = HB[:, 0:6144].rearrange("p (g t) -> p g t", g=12)
        Kwin = [HB[:, 6144:8704], HB[:, 8704:11264]]
        VA = [HB[:, 11264:11392], HB[:, 11392:11520], HB[:, 22016:22144]]
        VB = [HB[:, 11520:11648], HB[:, 11648:11776], HB[:, 22144:22272]]
        PTa = [HB[:, 11776:12032], HB[:, 12032:12288], HB[:, 22272:22528]]
        oaT = HB[:, 12288:14336].rearrange("p (h t) -> p h t", h=4)
        ocT = HB[:, 14336:16384].rearrange("p (h t) -> p h t", h=4)
        qcT = HB[:, 16384:16896]
        PcT = HB[:, 16896:17920].rearrange("p (m t) -> p m t", m=2)
        KTt = HB[:, 17920:19968].rearrange("p (h t) -> p h t", h=4)
        Vtl = HB[:, 19968:22016].rearrange("p (s d) -> p s d", s=4)
        acc_o = HF[:, 0:512]
        acc_l = HF[:, 512:1024]
        recA = HF[:, 1024:1536]
        x1Ts = sb("x1Ts", [128, 8, 128])
        x1T4 = [x1Ts, x1Ts]
        Sst = sb("Sst", [128, 8, 128])
        Sbf = sb("Sbf", [128, 8, 128], BF16)
        dec = sb("dec", [128, 8, 8])
        OT = sb("OT", [128, 8, NT])
        mrg = OT
        sgt = sb("sgt", [128, NT], BF16)
        obT = sb("obT", [128, 8, NT], BF16)
        mrgb = sb("mrgb", [128, 8, NT], BF16)
        gsb = sb("gsb", [128, NT])
        xres = sb("xres", [128, 4, D])
        bnst = sb("bnst", [128, 2, 6])
        mv = sb("mv", [128, 2])
        rstd = sb("rstd", [128, 1])
        sqb = sb("sqb", [128, NT], BF16)
        KcT = sb("KcT", [128, 4, 256], BF16)
        Vc = sb("Vc", [128, 2, 512], BF16)
        lg = sb("lg", [128, 32, 36])

        S.pool(lambda e: e.memset(gsb[:], 0.0), writes=["gsb"])
        S.dma(lambda e: e.dma_start(out=xbuf.rearrange("(b p) d -> p b d", p=128), in_=gsb[:].bitcast(BF16).rearrange("p (o d) -> p o d", o=1).to_broadcast([128, NBLK, 1024])), reads=["gsb"], writes=["xbuf"], nobar=True)
        S.pool(lambda e: e.memset(Sst[:], 0.0), writes=[("S", h) for h in range(8)])
        S.pool(lambda e: e.memset(Sbf[:], 0.0), writes=[("Sbf", h) for h in range(8)])

        wt_pool = [[0, 1, 2]]

        def load_w(g):
            i = wt_pool[0][wt_n[0] % len(wt_pool[0])]
            wt_n[0] += 1
            S.dma(lambda e, g=g, i=i: e.dma_start(out=wt[i][:], in_=wbf[g]), reads=[("wbf", g)], writes=[("wt", i)], nobar=True)
            return i

        def wview(i, kcs):
            return wt[i][:].rearrange("p (kc n) -> p kc n", kc=kcs)

        pb_n = [0]

        pb_pool = [3]
        POOLS = {3: [0, 1, 2], 7: [0, 1, 2, 3, 4, 5, 6], "H": [0, 2], "P": [1]}

        def next_pb():
            pl = POOLS[pb_pool[0]]
            pbi = pl[pb_n[0] % len(pl)]
            pb_n[0] += 1
            return pbi

        def proj_fm(wi, blk, xb, xkey, ncols=NT):
            pbi = next_pb()
            w3 = wview(wi, 8)

            def mm(e, pbi=pbi, blk=blk, xb=xb, w3=w3):
                for kc in range(8):
                    ins = e.matmul(PB[pbi][:, 0:ncols], lhsT=w3[:, kc, blk * 128:(blk + 1) * 128], rhs=xb[:, kc, 0:ncols], start=(kc == 0), stop=(kc == 7))
                return ins
            S.pe(mm, reads=[("wt", wi), xkey], writes=[("pb", pbi)])
            return pbi

        def proj_tm(wi, sub, xb, xkey):
            pbi = next_pb()
            w3 = wview(wi, 8)

            def mm(e, pbi=pbi, sub=sub, xb=xb, w3=w3):
                for kc in range(8):
                    ins = e.matmul(PB[pbi][:], lhsT=xb[:, kc, sub * 128:(sub + 1) * 128], rhs=w3[:, kc, :], start=(kc == 0), stop=(kc == 7))
                return ins
            S.pe(mm, reads=[("wt", wi), xkey], writes=[("pb", pbi)])
            return pbi

        memb = HB[:, 0:2048].rearrange("p (k m) -> p k m", k=8)
        S.dma(lambda e: e.dma_start(out=memb, in_=memT.rearrange("(kc p) m -> p kc m", p=128)), writes=["memb"], queue="pool")
        wk_ = load_w(G_KV)
        for hc in range(4):
            pk = proj_fm(wk_, hc, memb, "memb", ncols=256)
            S.act(lambda e, pk=pk, hc=hc: e.activation(out=KcT[:, hc, :], in_=PB[pk][:, 0:256], func=AF.Copy), reads=[("pb", pk)], writes=["KcT"])
        wv_ = load_w(G_KV + 1)
        for ms in range(2):
            pv = proj_tm(wv_, ms, memb, "memb")
            S.act(lambda e, pv=pv, ms=ms: e.activation(out=Vc[:, ms, :], in_=PB[pv][:], func=AF.Copy), reads=[("pb", pv)], writes=["Vc"])
        S.barrier()

        def phase_h(tix, is_ctx, xb, xkey):
            for hg in range(2):
                wf = load_w(G_BF + hg)
                wq = load_w(G_BQ + hg) if not is_ctx else None
                for pr_ in range(2):
                    hs = [hg * 4 + pr_ * 2, hg * 4 + pr_ * 2 + 1]
                    Ts = {h: tmp[h % 2] for h in hs}
                    tk = lambda h, j: ("tmp", h % 2, j)
                    for h in hs:
                        T = Ts[h]
                        pf = proj_fm(wf, h % 4, xb, xkey)
                        S.act(lambda e, T=T, pf=pf: e.activation(out=T[0], in_=PB[pf][:], func=AF.Sigmoid), reads=[("pb", pf)], writes=[tk(h, 0)])
                        if not is_ctx:
                            pq = proj_fm(wq, h % 4, xb, xkey)
                            S.act(lambda e, T=T, pq=pq: e.activation(out=T[4], in_=PB[pq][:], func=AF.Silu), reads=[("pb", pq)], writes=[tk(h, 4)])
                    for h in hs:
                        T = Ts[h]
                        S.dve(lambda e, T=T, h=h: e.tensor_scalar(out=T[0], in0=T[0], scalar1=omlv[:, h:h + 1], scalar2=lbv[:, h:h + 1], op0=ALU.mult, op1=ALU.add),
                              reads=[tk(h, 0), "lbv", "omlv"], writes=[tk(h, 0)])
                    for h in hs:
                        T = Ts[h]
                        S.act(lambda e, T=T: e.activation(out=T[1], in_=T[0], func=AF.Ln), reads=[tk(h, 0)], writes=[tk(h, 1)])
                    for h in hs:
                        T = Ts[h]
                        S.dve(lambda e, T=T: e.tensor_tensor_scan(out=T[2], data0=rmask[:], data1=T[1], initial=0.0, op0=ALU.mult, op1=ALU.add),
                              reads=[tk(h, 1), "rmask"], writes=[tk(h, 2)])
                        S.pool(lambda e, T=T: e.tensor_scalar(out=T[0], in0=T[0], scalar1=-1.0, scalar2=1.0, op0=ALU.mult, op1=ALU.add), reads=[tk(h, 0)], writes=[tk(h, 0)])

                        def dfn(e, T=T):
                            b3 = T[2].rearrange("p (c s) -> p c s", s=64)
                            return e.tensor_tensor(out=T[3].rearrange("p (c s) -> p c s", s=64), in0=b3[:, :, 63:64].to_broadcast([128, 8, 64]), in1=b3, op=ALU.subtract)
                        S.dve(dfn, reads=[tk(h, 2)], writes=[tk(h, 3)])
                    for h in hs:
                        T = Ts[h]
                        S.act(lambda e, T=T: e.activation(out=T[3], in_=T[3], func=AF.Exp), reads=[tk(h, 3)], writes=[tk(h, 3)])
                        S.act(lambda e, T=T, h=h: e.activation(out=dec[:, h, :], in_=T[2][:, 63::64], func=AF.Exp), reads=[tk(h, 2)], writes=[("dec", h)])
                    for h in hs:
                        T = Ts[h]
                        S.dve(lambda e, T=T, h=h: e.tensor_tensor(out=khatT[:, h, :], in0=T[0], in1=T[3], op=ALU.mult), reads=[tk(h, 0), tk(h, 3)], writes=[("khatT", h)])
                    if not is_ctx:
                        for h in hs:
                            T = Ts[h]
                            S.act(lambda e, T=T: e.activation(out=T[3], in_=T[2], func=AF.Exp, scale=-1.0), reads=[tk(h, 2)], writes=[tk(h, 3)])
                        for h in hs:
                            T = Ts[h]
                            S.pool(lambda e, T=T, h=h: e.tensor_tensor(out=kt[:, h, :], in0=T[0], in1=T[3], op=ALU.mult), reads=[tk(h, 0), tk(h, 3)], writes=[("kt", h)])
                        for h in hs:
                            T = Ts[h]
                            S.act(lambda e, T=T: e.activation(out=T[2], in_=T[2], func=AF.Exp), reads=[tk(h, 2)], writes=[tk(h, 2)])
                        for h in hs:
                            T = Ts[h]
                            S.dve(lambda e, T=T, h=h: e.tensor_tensor(out=qt[:, h, :], in0=T[4], in1=T[2], op=ALU.mult), reads=[tk(h, 4), tk(h, 2)], writes=[("qt", h)])
            for hg in range(2):
                wi_ = load_w(G_BI + hg)
                for sub in range(4):
                    pv = proj_tm(wi_, sub, xb, xkey)
                    S.act(lambda e, pv=pv, sub=sub, hg=hg: e.activation(out=Vt[:, sub, hg * 512:(hg + 1) * 512], in_=PB[pv][:], func=AF.Copy),
                          reads=[("pb", pv)], writes=[("V", sub, hg)])
            for sub in range(4):
                for hg in range(2):
                    def trf(e, sub=sub, hg=hg):
                        for hh in range(4):
                            ins = e.transpose(PT[:, (hg * 4 + hh) * 128:(hg * 4 + hh + 1) * 128], khatT[:, hg * 4 + hh, sub * 128:(sub + 1) * 128], ident[:])
                        return ins
                    S.pe(trf, reads=[("khatT", hg * 4 + hh) for hh in range(4)] + ["ident"], writes=["PT"])
                    S.dve(lambda e, sub=sub, hg=hg: e.tensor_copy(out=khat[:, sub, hg * 512:(hg + 1) * 512], in_=PT[:, hg * 512:(hg + 1) * 512]),
                          reads=["PT"], writes=[("khat", sub, hg)])
            for sub in range(4):
                if not is_ctx:
                    for hg in range(2):
                        def amm(e, sub=sub, hg=hg):
                            for hh in range(4):
                                h = hg * 4 + hh
                                ins = e.matmul(PB[3][:, hh * 128:(hh + 1) * 128], lhsT=kt[:, h, sub * 128:(sub + 1) * 128], rhs=qt[:, h, sub * 128:(sub + 1) * 128], start=True, stop=True)
                            return ins
                        S.pe(amm, reads=[("kt", hg * 4 + hh) for hh in range(4)] + [("qt", hg * 4 + hh) for hh in range(4)], writes=[("pb", 3)])
                        S.dve(lambda e, hg=hg: e.tensor_tensor(out=ATs[:, hg * 4:(hg + 1) * 4, :], in0=PB[3][:].rearrange("p (h t) -> p h t", h=4), in1=cm4[:].rearrange("p (h t) -> p h t", h=4), op=ALU.mult),
                              reads=[("pb", 3), "cm4"], writes=[("ATs", hg)])
                    for hg in range(2):
                        def omm(e, sub=sub, hg=hg):
                            for hh in range(4):
                                h = hg * 4 + hh
                                ins = e.matmul(PB[5 + hg][:, hh * 128:(hh + 1) * 128], lhsT=Vt[:, sub, h * 128:(h + 1) * 128], rhs=ATs[:, h, :], start=(hh == 0), stop=False)
                            return ins
                        S.pe(omm, reads=[("V", sub, hg), ("ATs", hg)], writes=[("ot", hg)])
                for c in range(2):
                    ch = sub * 2 + c
                    r0 = c * 64
                    for hg in range(2):
                        if not is_ctx:
                            def sqm(e, sub=sub, hg=hg, c=c):
                                for hh in range(4):
                                    h = hg * 4 + hh
                                    ins = e.matmul(PB[5 + hg][:, hh * 128 + c * 64:hh * 128 + c * 64 + 64], lhsT=Sbf[:, h, :], rhs=qt[:, h, sub * 128 + c * 64:sub * 128 + c * 64 + 64], start=False, stop=(c == 1 and hh == 3))
                                return ins
                            S.pe(sqm, reads=[("Sbf", hg * 4 + hh) for hh in range(4)] + [("qt", hg * 4 + hh) for hh in range(4)], writes=[("ot", hg)])

                        def umm(e, sub=sub, hg=hg, r0=r0):
                            for hh in range(4):
                                h = hg * 4 + hh
                                ins = e.matmul(PB[4][:, hh * 128:(hh + 1) * 128] if hg == 0 else PT_U[:, hh * 128:(hh + 1) * 128], lhsT=khat[r0:r0 + 64, sub, h * 128:(h + 1) * 128], rhs=Vt[r0:r0 + 64, sub, h * 128:(h + 1) * 128], start=True, stop=True)
                            return ins
                        S.pe(umm, reads=[("khat", sub, hg), ("V", sub, hg)], writes=[("pb", 4 if hg == 0 else 2)])
                        for hh in range(4):
                            h = hg * 4 + hh
                            S.dve(lambda e, h=h, hg=hg, hh=hh, ch=ch: e.scalar_tensor_tensor(out=Sst[:, h, :], in0=Sst[:, h, :], scalar=dec[:, h, ch:ch + 1], in1=(PB[4] if hg == 0 else PT_U)[:, hh * 128:(hh + 1) * 128], op0=ALU.mult, op1=ALU.add),
                                  reads=[("pb", 4 if hg == 0 else 2), ("dec", h), ("S", h)], writes=[("S", h)])
                            if (not is_ctx) or (sub == 3 and c == 1):
                                S.act(lambda e, h=h: e.activation(out=Sbf[:, h, :], in_=Sst[:, h, :], func=AF.Copy), reads=[("S", h)], writes=[("Sbf", h)])
                if not is_ctx:
                    for hg in range(2):
                        S.act(lambda e, hg=hg, sub=sub: e.activation(out=OT[:, hg * 4:(hg + 1) * 4, sub * 128:(sub + 1) * 128], in_=PB[5 + hg][:].rearrange("p (h t) -> p h t", h=4), func=AF.Copy),
                              reads=[("ot", hg)], writes=[("OT", hg * 4 + hh) for hh in range(4)])
            if is_ctx:
                return
            for hg in range(2):
                wg = load_w(G_BG + hg)
                for hh in range(4):
                    h = hg * 4 + hh
                    okeys = [("OT", h)]
                    pg = proj_fm(wg, hh, xb, xkey)
                    S.act(lambda e, pg=pg: e.activation(out=sgt[:], in_=PB[pg][:], func=AF.Silu), reads=[("pb", pg)], writes=["sgt"])
                    S.pool(lambda e, h=h: e.tensor_tensor(out=sqb[:], in0=OT[:, h, :], in1=OT[:, h, :], op=ALU.mult), reads=okeys, writes=["sqb"])
                    pbi = next_pb()
                    S.pe(lambda e, pbi=pbi: e.matmul(PB[pbi][:], lhsT=ones_bf[:], rhs=sqb[:], start=True, stop=True), reads=["sqb", "ones_bf"], writes=[("pb", pbi)])
                    S.dve(lambda e, pbi=pbi: e.tensor_scalar(out=gsb[:], in0=PB[pbi][:], scalar1=1.0 / 128.0, scalar2=RMS_EPS, op0=ALU.mult, op1=ALU.add), reads=[("pb", pbi)], writes=["gsb"])
                    S.act(lambda e: e.activation(out=gsb[:], in_=gsb[:], func=AF.Ln), reads=["gsb"], writes=["gsb"])
                    S.act(lambda e: e.activation(out=gsb[:], in_=gsb[:], func=AF.Exp, scale=-0.5), reads=["gsb"], writes=["gsb"])
                    S.dve(lambda e, h=h: e.tensor_tensor(out=OT[:, h, :], in0=OT[:, h, :], in1=gsb[:], op=ALU.mult), reads=okeys + ["gsb"], writes=okeys)
                    S.dve(lambda e, h=h: e.scalar_tensor_tensor(out=obT[:, h, :], in0=OT[:, h, :], scalar=ngv[:, h:h + 1], in1=sgt[:], op0=ALU.mult, op1=ALU.mult),
                          reads=okeys + ["sgt", "ngv"], writes=[("obT", h)])

        PT_U = PB[2]

        def phase_a_proj(tok0, is_ctx, groups, xb, xkey):
            for g in groups:
                if not is_ctx:
                    wq = load_w(3 * g)
                    for hh in range(4):
                        pq = proj_fm(wq, hh, xb, xkey)
                        S.act(lambda e, pq=pq, g=g, hh=hh: e.activation(out=QT[:, g * 4 + hh, :], in_=PB[pq][:], func=AF.Copy, scale=128.0 ** -0.5), reads=[("pb", pq)], writes=[("QT", g * 4 + hh)])
                wk = load_w(3 * g + 1)
                for hh in range(4):
                    pk = proj_fm(wk, hh, xb, xkey)
                    S.dve(lambda e, pk=pk, hh=hh: e.tensor_copy(out=KTt[:, hh, :], in_=PB[pk][:]), reads=[("pb", pk)], writes=[("KTt", hh)])
                S.dma(lambda e, g=g, tok0=tok0: e.dma_start(out=KTd[g, :, :, tok0:tok0 + NT].rearrange("h p t -> p h t"), in_=KTt), reads=[("KTt", hh) for hh in range(4)], writes=[("KTd", g)])
                wv = load_w(3 * g + 2)
                for sub in range(4):
                    pv = proj_tm(wv, sub, xb, xkey)
                    S.act(lambda e, pv=pv, sub=sub: e.activation(out=Vtl[:, sub, :], in_=PB[pv][:], func=AF.Copy), reads=[("pb", pv)], writes=[("Vtl", sub)])
                S.dma(lambda e, g=g, tok0=tok0: e.dma_start(out=Vd[g, tok0:tok0 + NT, :].rearrange("(s p) d -> p s d", p=128), in_=Vtl), reads=[("Vtl", sub) for sub in range(4)], writes=[("Vd", g)])

        def phase_a_attn(tix):
            tok0 = HALF + tix * NT
            kw_n = [0]
            un_ = [0]
            for h in range(4):
                first = True
                for g, (win, r) in enumerate(GROUPS_A):
                    Hh = win
                    W = Hh + NT
                    nqb = min(128, NT // r)
                    ki = kw_n[0] % 2
                    kw_n[0] += 1
                    S.dma(lambda e, g=g, h=h, ki=ki, W=W, Hh=Hh: e.dma_start(out=Kwin[ki][:, 0:W], in_=KTd[g, h, :, tok0 - Hh:tok0 + NT]), reads=[("KTd", g)], writes=[("Kwin", ki)])
                    kwv = Kwin[ki]
                    gname = "g%d" % g
                    units = [(c, 0) for c in range(r)] if r > 1 else [(0, qb) for qb in range(4)]
                    for (c, qb) in units:
                        pA = c + r * 128 * qb
                        pBq = pA + 128 * r
                        q0 = c + 128 * qb
                        if g == 2:
                            var = min(tix, 4)
                        elif g == 1:
                            var = 0 if tix == 0 else 1
                        else:
                            var = 0 if (tix == 0 and qb == 0) else 1
                        offA = A_OFF[gname + "A"] + (var * 4 + h) * nqb
                        offB = A_OFF[gname + "B"] + h * nqb
                        vi = un_[0] % 3
                        un_[0] += 1
                        rowA = tok0 - Hh + pA
                        S.dma(lambda e, g=g, h=h, vi=vi, rowA=rowA, r=r: e.dma_start(out=VA[vi], in_=Vd[g, rowA:rowA + 127 * r + 1:r, h * 128:(h + 1) * 128]), reads=[("Vd", g)], writes=[("VA", vi)])
                        S.dma(lambda e, g=g, h=h, vi=vi, rowA=rowA, r=r, nqb=nqb: e.dma_start(out=VB[vi][0:nqb, :], in_=Vd[g, rowA + 128 * r:rowA + 128 * r + (nqb - 1) * r + 1:r, h * 128:(h + 1) * 128]), reads=[("Vd", g)], writes=[("VB", vi)])
                        ps = next_pb()

                        def smm(e, ps=ps, kwv=kwv, pA=pA, pBq=pBq, r=r, nqb=nqb, g=g, h=h, q0=q0, offA=offA, offB=offB):
                            qv = QT[:, g * 4 + h, q0:q0 + (nqb - 1) * r + 1:r]
                            e.matmul(PB[ps][:, 0:nqb], lhsT=kwv[:, pA:pA + 127 * r + 1:r], rhs=qv, start=True, stop=False)
                            e.matmul(PB[ps][:, 0:nqb], lhsT=ident[:], rhs=ab[:, offA:offA + nqb], start=False, stop=True)
                            e.matmul(PB[ps][0:nqb, 128:128 + nqb], lhsT=kwv[:, pBq:pBq + (nqb - 1) * r + 1:r], rhs=qv, start=True, stop=False)
                            return e.matmul(PB[ps][0:nqb, 128:128 + nqb], lhsT=ident[0:nqb, 0:nqb], rhs=ab[0:nqb, offB:offB + nqb], start=False, stop=True)
                        S.pe(smm, reads=[("Kwin", ki), ("QT", g * 4 + h), "ab", "ident"], writes=[("pb", ps)])
                        pi = vi

                        def efn(e, ps=ps, pi=pi, nqb=nqb):
                            e.activation(out=PTa[pi][:, 0:nqb], in_=PB[ps][:, 0:nqb], func=AF.Exp)
                            return e.activation(out=PTa[pi][0:nqb, 128:128 + nqb], in_=PB[ps][0:nqb, 128:128 + nqb], func=AF.Exp)
                        S.act(efn, reads=[("pb", ps)], writes=[("PTa", pi)])
                        po = next_pb()

                        def omm2(e, po=po, pi=pi, vi=vi, nqb=nqb):
                            e.matmul(PB[po][:, 0:nqb], lhsT=VA[vi], rhs=PTa[pi][:, 0:nqb], start=True, stop=False)
                            e.matmul(PB[po][:, 0:nqb], lhsT=VB[vi][0:nqb, :], rhs=PTa[pi][0:nqb, 128:128 + nqb], start=False, stop=True)
                            e.matmul(PB[po][:, 128:128 + nqb], lhsT=ones_bf[:], rhs=PTa[pi][:, 0:nqb], start=True, stop=False)
                            return e.matmul(PB[po][:, 128:128 + nqb], lhsT=ones_bf[0:nqb, :], rhs=PTa[pi][0:nqb, 128:128 + nqb], start=False, stop=True)
                        S.pe(omm2, reads=[("PTa", pi), ("VA", vi), ("VB", vi), "ones_bf"], writes=[("pb", po)])
                        qsl = slice(q0, q0 + (nqb - 1) * r + 1, r)
                        if first:
                            S.dve(lambda e, po=po, qsl=qsl, nqb=nqb: e.tensor_copy(out=acc_o[:, qsl], in_=PB[po][:, 0:nqb]), reads=[("pb", po)], writes=["acc_o"])
                            S.dve(lambda e, po=po, qsl=qsl, nqb=nqb: e.tensor_copy(out=acc_l[:, qsl], in_=PB[po][:, 128:128 + nqb]), reads=[("pb", po)], writes=["acc_l"])
                        else:
                            S.dve(lambda e, po=po, qsl=qsl, nqb=nqb: e.tensor_tensor(out=acc_o[:, qsl], in0=acc_o[:, qsl], in1=PB[po][:, 0:nqb], op=ALU.add), reads=[("pb", po)], writes=["acc_o"])
                            S.dve(lambda e, po=po, qsl=qsl, nqb=nqb: e.tensor_tensor(out=acc_l[:, qsl], in0=acc_l[:, qsl], in1=PB[po][:, 128:128 + nqb], op=ALU.add), reads=[("pb", po)], writes=["acc_l"])
                    first = False
                S.dve(lambda e: e.reciprocal(out=recA, in_=acc_l), reads=["acc_l"], writes=["recA"])
                S.dve(lambda e, h=h: e.tensor_tensor(out=oaT[:, h, :], in0=acc_o, in1=recA, op=ALU.mult), reads=["acc_o", "recA"], writes=[("oaT", h)])

        def phase_c(xb, xkey):
            wq = load_w(G_C)
            for hc in range(4):
                pq = proj_fm(wq, hc, xb, xkey)
                S.act(lambda e, pq=pq: e.activation(out=qcT, in_=PB[pq][:], func=AF.Copy, scale=128.0 ** -0.5), reads=[("pb", pq)], writes=["qcT"])
                for mc in range(2):
                    ps = next_pb()
                    S.pe(lambda e, ps=ps, hc=hc, mc=mc: e.matmul(PB[ps][:], lhsT=KcT[:, hc, mc * 128:(mc + 1) * 128], rhs=qcT, start=True, stop=True), reads=["KcT", "qcT"], writes=[("pb", ps)])
                    S.act(lambda e, ps=ps, mc=mc: e.activation(out=PcT[:, mc, :], in_=PB[ps][:], func=AF.Exp), reads=[("pb", ps)], writes=[("PcT", mc)])
                po = next_pb()
                pl = next_pb()

                def cmm(e, po=po, hc=hc):
                    e.matmul(PB[po][:], lhsT=Vc[:, 0, hc * 128:(hc + 1) * 128], rhs=PcT[:, 0, :], start=True, stop=False)
                    return e.matmul(PB[po][:], lhsT=Vc[:, 1, hc * 128:(hc + 1) * 128], rhs=PcT[:, 1, :], start=False, stop=True)
                S.pe(cmm, reads=["Vc", ("PcT", 0), ("PcT", 1)], writes=[("pb", po)])

                def lmm(e, pl=pl):
                    e.matmul(PB[pl][:], lhsT=ones_bf[:], rhs=PcT[:, 0, :], start=True, stop=False)
                    return e.matmul(PB[pl][:], lhsT=ones_bf[:], rhs=PcT[:, 1, :], start=False, stop=True)
                S.pe(lmm, reads=["ones_bf", ("PcT", 0), ("PcT", 1)], writes=[("pb", pl)])
                S.dve(lambda e, pl=pl: e.reciprocal(out=recA, in_=PB[pl][:]), reads=[("pb", pl)], writes=["recA"])
                S.dve(lambda e, po=po, hc=hc: e.tensor_tensor(out=ocT[:, hc, :], in0=PB[po][:], in1=recA, op=ALU.mult), reads=[("pb", po), "recA"], writes=[("ocT", hc)])

        def branch_merge(src, nk, wgroup, gate_group0, xb, xkey, srckeys, first, pre=None):
            if pre is not None:
                wis = pre[0:2]
            elif nk == 8:
                wis = [load_w(wgroup), load_w(wgroup + 1)]
            else:
                wis = [load_w(wgroup)]
            for half in range(2):
                wgt = pre[2] if (pre is not None and half == 0) else load_w(gate_group0 + half)
                for blk in range(4):
                    fc = half * 4 + blk
                    pgate = proj_fm(wgt, blk, xb, xkey)
                    S.act(lambda e, pgate=pgate: e.activation(out=gsb[:], in_=PB[pgate][:], func=AF.Sigmoid), reads=[("pb", pgate)], writes=["gsb"])
                    pbi = next_pb()
                    if nk == 8:
                        w3 = wview(wis[half], 8)
                        c0 = blk * 128
                        wkey = ("wt", wis[half])
                    else:
                        w3 = wview(wis[0], 4)
                        c0 = fc * 128
                        wkey = ("wt", wis[0])

                    def mmb(e, pbi=pbi, w3=w3, c0=c0):
                        for k in range(nk):
                            ins = e.matmul(PB[pbi][:], lhsT=w3[:, k, c0:c0 + 128], rhs=src[:, k, :], start=(k == 0), stop=(k == nk - 1))
                        return ins
                    S.pe(mmb, reads=[wkey] + srckeys, writes=[("pb", pbi)])
                    if first:
                        S.dve(lambda e, pbi=pbi, fc=fc: e.tensor_tensor(out=mrg[:, fc, :], in0=PB[pbi][:], in1=gsb[:], op=ALU.mult), reads=[("pb", pbi), "gsb"], writes=[("OT", fc)])
                    else:
                        S.dve(lambda e, pbi=pbi: e.tensor_tensor(out=gsb[:], in0=PB[pbi][:], in1=gsb[:], op=ALU.mult), reads=[("pb", pbi), "gsb"], writes=["gsb"])
                        S.pool(lambda e, fc=fc: e.tensor_tensor(out=mrg[:, fc, :], in0=mrg[:, fc, :], in1=gsb[:], op=ALU.add), reads=["gsb", ("OT", fc)], writes=[("OT", fc)])

        def layer_norm(sub, which):
            xk = ("xres", sub)

            def st_(e, sub=sub):
                e.bn_stats(out=bnst[:, 0, :], in_=xres[:, sub, 0:512])
                return e.bn_stats(out=bnst[:, 1, :], in_=xres[:, sub, 512:1024])
            S.dve(st_, reads=[xk], writes=["bnst"])
            S.dve(lambda e: e.bn_aggr(out=mv[:], in_=bnst[:].rearrange("p a b -> p (a b)")), reads=["bnst"], writes=["mv"])
            S.dve(lambda e: e.tensor_scalar(out=rstd[:], in0=mv[:, 1:2], scalar1=LN_EPS, scalar2=None, op0=ALU.add), reads=["mv"], writes=["rstd"])
            S.act(lambda e: e.activation(out=rstd[:], in_=rstd[:], func=AF.Ln), reads=["rstd"], writes=["rstd"])
            S.act(lambda e: e.activation(out=rstd[:], in_=rstd[:], func=AF.Exp, scale=-0.5), reads=["rstd"], writes=["rstd"])
            S.dve(lambda e, sub=sub: e.tensor_scalar(out=xres[:, sub, :], in0=xres[:, sub, :], scalar1=mv[:, 0:1], scalar2=rstd[:], op0=ALU.subtract, op1=ALU.mult), reads=[xk, "mv", "rstd"], writes=[xk])
            S.dve(lambda e, sub=sub: e.tensor_tensor(out=xres[:, sub, :], in0=xres[:, sub, :], in1=lnrow[:, 0, :], op=ALU.mult), reads=[xk, "lnrow"], writes=[xk])
            S.pool(lambda e, sub=sub: e.tensor_tensor(out=xres[:, sub, :], in0=xres[:, sub, :], in1=lnrow[:, 1, :], op=ALU.add), reads=[xk, "lnrow"], writes=[xk])

        def phase_out(tix):
            for fc in range(8):
                S.pool(lambda e, fc=fc: e.tensor_copy(out=mrgb[:, fc, :], in_=mrg[:, fc, :]), reads=[("OT", fc)], writes=[("mrgb", fc)])
            S.dma(lambda e, tix=tix: e.dma_start(out=xres[:], in_=xo[tix * NT:(tix + 1) * NT, :].rearrange("(s p) d -> p s d", p=128)), writes=[("xres", s_) for s_ in range(4)])
            for half in range(2):
                wo_ = load_w(G_WO + half)
                for sub in range(4):
                    pbi = next_pb()
                    w3 = wview(wo_, 8)

                    def mmo(e, pbi=pbi, sub=sub, w3=w3):
                        for kc in range(8):
                            ins = e.matmul(PB[pbi][:], lhsT=mrgb[:, kc, sub * 128:(sub + 1) * 128], rhs=w3[:, kc, :], start=(kc == 0), stop=(kc == 7))
                        return ins
                    S.pe(mmo, reads=[("wt", wo_)] + [("mrgb", fc) for fc in range(8)], writes=[("pb", pbi)])
                    S.dve(lambda e, pbi=pbi, sub=sub, half=half: e.scalar_tensor_tensor(out=xres[:, sub, half * 512:(half + 1) * 512], in0=xres[:, sub, half * 512:(half + 1) * 512], scalar=DN_ALPHA, in1=PB[pbi][:], op0=ALU.mult, op1=ALU.add),
                          reads=[("pb", pbi), ("xres", sub)], writes=[("xres", sub)])
            for sub in range(4):
                layer_norm(sub, 0)
            for sub in range(4):
                gs = tix * 4 + sub
                for half in range(2):
                    pt_ = next_pb()

                    def trx(e, pt_=pt_, sub=sub, half=half):
                        for k in range(4):
                            kc = half * 4 + k
                            ins = e.transpose(PB[pt_][:, k * 128:(k + 1) * 128], xres[:, sub, kc * 128:(kc + 1) * 128], identf[:])
                        return ins
                    S.pe(trx, reads=[("xres", sub), "identf"], writes=[("pb", pt_)])
                    S.act(lambda e, pt_=pt_, half=half, sub=sub: e.activation(out=x1T4[sub % 2][:, half * 4:(half + 1) * 4, :], in_=PB[pt_][:].rearrange("p (k t) -> p k t", k=4), func=AF.Copy), reads=[("pb", pt_)], writes=[("x1T", 0, half)])
                pl_ = next_pb()

                def rmm(e, pl_=pl_, sub=sub):
                    for kc in range(8):
                        ins = e.matmul(PB[pl_][:, 0:36], lhsT=x1T4[sub % 2][:, kc, :], rhs=wr[:, kc, :], start=(kc == 0), stop=(kc == 7))
                    return ins
                S.pe(rmm, reads=[("x1T", 0, 0), ("x1T", 0, 1), "wr"], writes=[("pb", pl_)])
                S.dve(lambda e, pl_=pl_, gs=gs: e.tensor_tensor(out=lg[:, gs, :], in0=PB[pl_][:, 0:36], in1=brow[:], op=ALU.add), reads=[("pb", pl_), "brow"], writes=["lg"])
            S.dma(lambda e, tix=tix: e.dma_start(out=out[tix * NT:(tix + 1) * NT, :].rearrange("(s p) d -> p s d", p=128), in_=xres[:]),
                  reads=[("xres", s_) for s_ in range(4)], writes=[("out", tix * 4 + s_) for s_ in range(4)])

        xn = [0]

        def load_x(tok0):
            i = xn[0] % 2
            xn[0] += 1
            xb = xTb[i]
            S.dma(lambda e, xb=xb, tok0=tok0: e.dma_start(out=xb[:], in_=xT[:, tok0:tok0 + NT].rearrange("(kc p) t -> p kc t", p=128)),
                  writes=[("xTb", i)], queue="pool", nobar=True)
            return xb, ("xTb", i)

        tiles = [(t, True) for t in range(NTILES - n_ctx, NTILES)] + [(t, False) for t in range(n_own)]
        tok_of = lambda t, c: t * NT + (0 if c else HALF)
        nxt = load_x(tok_of(*tiles[0]))
        ecast_n = [0]

        estg = [sb("estg0", [128, 2048], BF16)]
        estg = [estg[0], estg[0]]

        def expert_casts(n):
            for _ in range(n):
                k = ecast_n[0]
                if k >= 3 * N_EXP:
                    return
                ecast_n[0] += 1
                wi, ex = k % 3, k // 3
                i = k % 2
                src = (weg, weu, wed)[wi]
                S.dma(lambda e, src=src, ex=ex, i=i: e.dma_start(out=estg[i][:], in_=src[ex * 128:(ex + 1) * 128, :]), writes=[("estg", 0)], queue="pool", nobar=True)
                S.dma(lambda e, wi=wi, ex=ex, i=i: e.dma_start(out=webf[ex * 128:(ex + 1) * 128, wi * 2048:(wi + 1) * 2048], in_=estg[i][:]), reads=[("estg", 0)], writes=[("webf", k)], nobar=True)

        pending_out = None
        for n_, (t, is_ctx) in enumerate(tiles):
            xb, xkey = nxt
            if n_ + 1 < len(tiles):
                nxt = load_x(tok_of(*tiles[n_ + 1]))
            pb_pool[0] = 3
            if is_ctx:
                expert_casts(1)
                phase_h(t, True, xb, xkey)
                S.barrier()
                expert_casts(1)
                groups = ([2] if t >= 4 else []) + ([1, 0] if t == 7 else [])
                if groups:
                    phase_a_proj(t * NT, True, groups, xb, xkey)
                expert_casts(1)
                S.barrier()
            else:
                expert_casts(1)
                pb_pool[0] = "H"
                wt_pool[0] = [0, 1]
                S.capture()
                phase_h(t, False, xb, xkey)
                hl = S.end_capture()
                wt_pool[0] = [0, 1, 2]
                S.replay([hl] + ([pending_out] if pending_out else []))
                pending_out = None
                pre = [load_w(G_WB), load_w(G_WB + 1), load_w(G_GATE + 2)]
                S.barrier()
                pb_pool[0] = 7
                expert_casts(1)
                branch_merge(obT, 8, G_WB, G_GATE + 2, xb, xkey, [("obT", h) for h in range(8)], True, pre=pre)
                expert_casts(1)
                phase_c(xb, xkey)
                expert_casts(1)
                branch_merge(ocT, 4, G_WC, G_GATE + 4, xb, xkey, [("ocT", h) for h in range(4)], False)
                expert_casts(1)
                phase_a_proj(HALF + t * NT, False, [0, 1, 2], xb, xkey)
                expert_casts(1)
                phase_a_attn(t)
                expert_casts(1)
                branch_merge(oaT, 4, G_WA, G_GATE, xb, xkey, [("oaT", h) for h in range(4)], False)
                expert_casts(1)
                S.barrier()
                pb_pool[0] = "P"
                wt_pool[0] = [2]
                S.capture()
                phase_out(t)
                pending_out = S.end_capture()
                wt_pool[0] = [0, 1, 2]
                expert_casts(1)
        if pending_out:
            S.replay([pending_out])
        S.barrier()
        pb_pool[0] = 7
        expert_casts(3 * N_EXP)

        moe_phase(nc, S, locals())
        S.add("sp", lambda e: None, reads=[("out", s_) for s_ in range(NSUB)])
        S.emit(st)
        print("sched stats", S.stats, flush=True)
    return nc


def _consts():
    cm = np.zeros((128, 1024 + 128 + 97), np.float32)
    rm = np.ones(512, np.float32)
    rm[0::64] = 0.0
    cm[:, 0:512] = rm[None, :]
    s = np.arange(128)[:, None]
    t = np.arange(128)[None, :]
    m = ((s // 64) == (t // 64)) & (s <= t)
    cm[:, 512:1024] = np.tile(m.astype(np.float32), (1, 4))
    cm[:, 1024:1152] = (s < t).astype(np.float32)
    cm[:, 1152:1248] = (128.0 * np.arange(96))[None, :]
    cm[:, 1248] = np.arange(128)
    return cm, np.eye(128, dtype=np.float32)


def _abias(rel_bias, half):
    ab = np.full((128, A_NB), NEG, np.float32)
    for g, (win, r) in enumerate(GROUPS_A):
        nqb = min(128, NT // r)
        gname = "g%d" % g
        jj = np.arange(128)[:, None]
        mm = np.arange(nqb)[None, :]
        uA = 128 + mm - jj
        validA = jj >= mm
        bA = _t5_bucket_np(np.clip(uA, 0, 128) * r)
        jb = np.arange(nqb)[:, None]
        uB = mm - jb
        validB = jb <= mm
        bB = _t5_bucket_np(np.clip(uB, 0, 128) * r)
        nvar = 5 if g == 2 else 2
        for h in range(4):
            tabA = np.where(validA, rel_bias[bA, g * 4 + h], NEG).astype(np.float32)
            tabB = np.where(validB, rel_bias[bB, g * 4 + h], NEG).astype(np.float32)
            for var in range(nvar):
                t_ = tabA.copy()
                if half == 0:
                    if g == 2 and var < 4:
                        t_[0:128 - 32 * var, :] = NEG
                    elif g != 2 and var == 0:
                        t_[:, :] = NEG
                o = A_OFF[gname + "A"] + (var * 4 + h) * nqb
                ab[:, o:o + nqb] = t_
            o = A_OFF[gname + "B"] + h * nqb
            ab[0:nqb, o:o + nqb] = tabB
    return ab


def make_in_maps(inputs):
    x = np.asarray(inputs["x"], np.float32)
    mem = np.asarray(inputs["mem"], np.float32)
    cm, ident = _consts()
    vecs = np.zeros((8, D), np.float32)
    vecs[0:2] = inputs["hgrn_lb_logits"]
    vecs[2] = inputs["hgrn_norm_g"][0]
    vecs[3] = inputs["ln1_g"][0]
    vecs[4] = inputs["ln1_b"][0]
    vecs[5] = inputs["ln2_g"][0]
    vecs[6] = inputs["ln2_b"][0]
    wr = np.concatenate([inputs["w_router_group"][0], inputs["w_router_expert"][0]], axis=1).astype(np.float32)
    br = np.concatenate([inputs["b_router_group"][0], inputs["b_router_expert"][0]])[None, :].astype(np.float32)

    def elay(w, kc):
        e_, k_, n_ = w.shape
        return np.ascontiguousarray(w.reshape(e_, kc, 128, n_).transpose(0, 2, 1, 3).reshape(e_ * 128, kc * n_), dtype=np.float32)
    shared = {
        "w_in": np.ascontiguousarray(inputs["w_in"][0], np.float32),
        "w_bb": np.ascontiguousarray(inputs["w_branch_b"][0], np.float32),
        "w_ba": np.ascontiguousarray(inputs["w_branch_a"][0], np.float32),
        "w_bc": np.ascontiguousarray(inputs["w_branch_c"][0], np.float32),
        "w_o": np.ascontiguousarray(inputs["w_out"][0], np.float32),
        "w_kv": np.ascontiguousarray(inputs["w_mem_kv"][0], np.float32),
        "w_r": np.ascontiguousarray(wr.reshape(8, 128, 36).transpose(1, 0, 2)),
        "b_r": br,
        "weg": elay(np.asarray(inputs["w_exp_gate"][0]), 8),
        "weu": elay(np.asarray(inputs["w_exp_up"][0]), 8),
        "wed": elay(np.asarray(inputs["w_exp_down"][0]), 2),
        "vecs": vecs, "cmask": cm, "ident": ident,
        "pvec": np.ascontiguousarray(vecs[0:3].reshape(3, 8, 128).transpose(2, 0, 1)),
    }
    rel_bias = np.asarray(inputs["rel_bias"], np.float32)
    abs_ = [_abias(rel_bias, 0), _abias(rel_bias, 1)]
    maps = []
    for core in range(8):
        b, half = core // 2, core % 2
        own = x[b, half * HALF:(half + 1) * HALF]
        ctx = x[b, 0:HALF] if half == 1 else np.zeros((HALF, D), np.float32)
        m = dict(shared)
        m["xT"] = np.ascontiguousarray(np.concatenate([ctx, own], axis=0).T)
        m["xo"] = np.ascontiguousarray(own)
        m["memT"] = np.ascontiguousarray(mem[b].T)
        m["abias"] = abs_[half]
        maps.append(m)
    return maps


def kernel(**inputs):
    nc = build_nc("full")
    maps = make_in_maps(inputs)
    res = run_bass_kernel_spmd(nc, maps, core_ids=list(range(8)))
    outp = np.zeros((4, SEQ, D), np.float32)
    for core in range(8):
        b, half = core // 2, core % 2
        outp[b, half * HALF:(half + 1) * HALF] = res.results[core]["out"]
    return outp
```

```python
import math
import numpy as np
from contextlib import ExitStack
import concourse.bass as bass
import concourse.mybir as mybir
from concourse.bass_utils import run_bass_kernel_spmd

F32 = mybir.dt.float32
BF16 = mybir.dt.bfloat16
I32 = mybir.dt.int32
ALU = mybir.AluOpType
AF = mybir.ActivationFunctionType
AX = mybir.AxisListType

SEM_EPOCH = 4000
DMA_RING = 8
DMA_EPOCH = 200


class Op:
    __slots__ = ("stream", "fn", "deps", "is_dma", "signal", "sig", "know", "oid")

    def __init__(self, stream, fn, is_dma):
        self.stream = stream
        self.fn = fn
        self.deps = set()
        self.is_dma = is_dma
        self.signal = is_dma
        self.sig = None
        self.know = None


class Sched:
    def __init__(self, nc, same_engine_sync=True):
        self.nc = nc
        self.ops = []
        self.last_w = {}
        self.readers = {}
        self.same_engine_sync = same_engine_sync
        self.last_op = {}
        self.dmas = []
        self._cap = None

    def capture(self):
        self._cap = []

    def end_capture(self):
        c = self._cap
        self._cap = None
        return c

    def replay(self, lists):
        lists = [l for l in lists if l]
        idx = [0] * len(lists)
        while True:
            best = None
            for i, l in enumerate(lists):
                if idx[i] < len(l):
                    frac = idx[i] / len(l)
                    if best is None or frac < best[0]:
                        best = (frac, i)
            if best is None:
                break
            i = best[1]
            stream, fn, reads, writes, dma, nobar = lists[i][idx[i]]
            idx[i] += 1
            self.add(stream, fn, reads, writes, dma, nobar)

    def barrier(self):
        deps = set(v for v in self.last_op.values())
        deps |= set(self.dmas)
        self.dmas = []
        keep = dict(self.last_op)
        for s_ in ("pe", "act", "dve", "pool", "sp"):
            op = self.add(s_, lambda e: None)
            op.deps |= deps
        self.last_op = keep

    def add(self, stream, fn, reads=(), writes=(), dma=False, nobar=False):
        if self._cap is not None:
            self._cap.append((stream, fn, tuple(reads), tuple(writes), dma, nobar))
            return None
        op = Op(stream, fn, dma)
        op.oid = len(self.ops)
        for k in reads:
            w = self.last_w.get(k)
            if w is not None:
                op.deps.add(w)
        for k in writes:
            w = self.last_w.get(k)
            if w is not None:
                op.deps.add(w)
            for r in self.readers.get(k, ()):
                op.deps.add(r)
        for k in writes:
            self.last_w[k] = op.oid
            self.readers[k] = []
        for k in reads:
            self.readers.setdefault(k, []).append(op.oid)
        op.deps.discard(op.oid)
        self.ops.append(op)
        if dma:
            if not nobar:
                self.dmas.append(op.oid)
        else:
            self.last_op[stream] = op.oid
        return op

    def pe(self, fn, reads=(), writes=()):
        return self.add("pe", fn, reads, writes)

    def act(self, fn, reads=(), writes=()):
        return self.add("act", fn, reads, writes)

    def dve(self, fn, reads=(), writes=()):
        return self.add("dve", fn, reads, writes)

    def pool(self, fn, reads=(), writes=()):
        return self.add("pool", fn, reads, writes)

    def dma(self, fn, reads=(), writes=(), queue="sp", nobar=False):
        return self.add(queue, fn, reads, writes, dma=True, nobar=nobar)

    def emit(self, stack):
        nc = self.nc
        ops = self.ops
        for op in ops:
            for d in op.deps:
                dop = ops[d]
                if dop.is_dma:
                    continue
                if dop.stream == op.stream and not op.is_dma:
                    if dop.stream == "pe" or not self.same_engine_sync:
                        continue
                dop.signal = True
        sems = {}
        cnt = {}
        for op in ops:
            if op.is_dma:
                q = op.stream
                j = cnt.get(("dma", q), 0)
                cnt[("dma", q)] = j + 1
                ring = j % DMA_RING
                n = j // DMA_RING
                ep = n // DMA_EPOCH
                op.sig = ("d_%s_%d_%d" % (q, ring, ep), 16 * (n % DMA_EPOCH + 1))
            elif op.signal:
                s = op.stream
                j = cnt.get(s, 0)
                cnt[s] = j + 1
                ep = j // SEM_EPOCH
                op.sig = ("c_%s_%d" % (s, ep), j % SEM_EPOCH + 1)
        know = {s: {} for s in ("pe", "act", "dve", "pool", "sp")}
        plan = {s: [] for s in know}
        dma_hist = {}
        for op in ops:
            s = op.stream
            K = know[s]
            waits = {}
            for d in sorted(op.deps):
                dop = ops[d]
                if not dop.is_dma and dop.stream == s and not op.is_dma:
                    if s == "pe" or not self.same_engine_sync:
                        continue
                if dop.sig is None:
                    continue
                src, val = dop.sig
                if K.get(src, 0) >= val:
                    continue
                if waits.get(src, 0) < val:
                    waits[src] = val
            if op.is_dma:
                hist = dma_hist.setdefault(s, [])
                if len(hist) >= DMA_RING:
                    pop = ops[hist[-DMA_RING]]
                    src, val = pop.sig
                    if K.get(src, 0) < val and waits.get(src, 0) < val:
                        waits[src] = val
                    op.deps.add(pop.oid)
                hist.append(op.oid)
            for d in op.deps:
                dop = ops[d]
                if dop.sig is None or dop.know is None:
                    continue
                src, val = dop.sig
                if waits.get(src, 0) >= val or K.get(src, 0) >= val:
                    for k2, v2 in dop.know.items():
                        if K.get(k2, 0) < v2:
                            K[k2] = v2
            for src, val in waits.items():
                if K.get(src, 0) < val:
                    K[src] = val
            if op.sig is not None:
                kn = dict(K)
                kn[op.sig[0]] = op.sig[1]
                op.know = kn
                if not op.is_dma and (s == "pe" or not self.same_engine_sync):
                    K[op.sig[0]] = op.sig[1]
            plan[s].append((op, sorted(waits.items())))
        for s in plan:
            for op, waits in plan[s]:
                if op.sig is not None and op.sig[0] not in sems:
                    sems[op.sig[0]] = stack.enter_context(nc.semaphore(op.sig[0]))
        block = stack.enter_context(nc.Block())

        def runner(s):
            def body(eng):
                for op, waits in plan[s]:
                    for src, val in waits:
                        eng.wait_ge(sems[src], val)
                    ins = op.fn(eng)
                    if op.sig is not None and ins is not None:
                        ins.then_inc(sems[op.sig[0]], 16 if op.is_dma else 1)
            return body

        block.tensor(runner("pe"))
        block.scalar(runner("act"))
        block.vector(runner("dve"))
        block.gpsimd(runner("pool"))
        block.sync(runner("sp"))
        self.stats = {s: len(plan[s]) for s in plan}
        self.stats["waits"] = sum(len(w) for s in plan for _, w in plan[s])
        self.stats["sems"] = len(sems)


D = 1024
SEQ = 8192
HALF = 4096
NT = 512
NTILES = HALF // NT
N_IN = 12288
COLS_A = 4608
COLS_B = 4096
G_BQ, G_BF, G_BI, G_BG = 9, 11, 13, 15
G_C = 17
G_GATE = 18
G_WB, G_WA, G_WC, G_WO, G_KV = 24, 26, 27, 28, 30
NGROUPS = 32
DN_ALPHA = 2 ** 0.25
LN_EPS = 1e-5
RMS_EPS = 1e-6
GROUPS_A = ((128, 1), (512, 4), (2048, 16))
NEG = -30000.0


def _t5_bucket_np(dist):
    dist = np.asarray(dist, np.int32)
    max_exact = 16
    d = np.maximum(dist, 1).astype(np.float32)
    large = max_exact + (np.log(d / max_exact) / math.log(2048 / max_exact) * (32 - max_exact)).astype(np.int32)
    large = np.minimum(large, 31)
    return np.where(dist < max_exact, dist, large).astype(np.int32)


def moe_phase(nc, S, L):
    PB, PT, HB, HF, lg, ones_bf, ustr, cb, ident = L["PB"], L["PT"], L["HB"], L["HF"], L["lg"], L["ones_bf"], L["ustr"], L["cb"], L["ident"]
    xres, lnrow, out, xbuf, ybuf, weg, weu, wed, vecs = L["xres"], L["lnrow"], L["out"], L["xbuf"], L["ybuf"], L["weg"], L["weu"], L["wed"], L["vecs"]
    N, NBLK, wt, sb, next_pb, layer_norm = L["NSUB"], L["NBLK"], L["wt"], L["sb"], L["next_pb"], L["layer_norm"]
    msm = sb("msm", [128, 1024])
    d1i = sb("d1i", [128, 32], I32)
    d2i = sb("d2i", [128, 32], I32)
    wix = sb("wix", [128, 96], I32)
    S.barrier()
    S.dma(lambda e: e.dma_start(out=lnrow[:], in_=vecs[5:7, :].partition_broadcast(128)), writes=["lnrow"])
    elm = HF[:, 0:N * 32].rearrange("p (s e) -> p s e", e=32)
    oh1 = HF[:, 1024:1024 + N * 32].rearrange("p (s e) -> p s e", e=32)
    oh2 = HF[:, 2048:2048 + N * 32].rearrange("p (s e) -> p s e", e=32)
    rank = HF[:, 3072:3072 + N * 32].rearrange("p (s e) -> p s e", e=32)
    trr = HF[:, 4096:4096 + N * 32].rearrange("p (s e) -> p s e", e=32)
    sm = lambda i, n=32: msm[:, i * 32:i * 32 + n]
    gmax, gsum, gp, m1, m2, w1, w2, d1, d2 = [sm(i, N) for i in range(9)]
    cnt, pc, pend, pstart, t1, one32 = [sm(i) for i in range(9, 15)]
    g1h = msm[:, 480:480 + N * 4].rearrange("p (s g) -> p s g", g=4)
    te = msm[:, 608:608 + N * 4].rearrange("p (s g) -> p s g", g=4)
    be = msm[:, 736:736 + 96]
    Mb = HB[:, 0:N * 32].rearrange("p (s e) -> p s e", e=32)
    Mcum = HB[:, 1024:1024 + (N + 1) * 32].rearrange("p (s e) -> p s e", e=32)
    K_ = ["moe"]
    gl = lg[:, 0:N, 0:4]
    bc = lambda ap, shape: ap.to_broadcast(shape)
    A3 = lambda ap: ap.rearrange("p (s o) -> p s o", o=1)
    S.dve(lambda e: e.tensor_reduce(out=gmax, in_=gl, axis=AX.X, op=ALU.max), reads=["lg"], writes=K_)
    S.dve(lambda e: e.tensor_tensor(out=g1h, in0=gl, in1=bc(A3(gmax), [128, N, 4]), op=ALU.is_equal), reads=K_, writes=K_)
    S.dve(lambda e: e.tensor_tensor(out=te, in0=gl, in1=bc(A3(gmax), [128, N, 4]), op=ALU.subtract), reads=K_, writes=K_)
    S.act(lambda e: e.activation(out=te, in_=te, func=AF.Exp), reads=K_, writes=K_)
    S.dve(lambda e: e.tensor_reduce(out=gsum, in_=te, axis=AX.X, op=ALU.add), reads=K_, writes=K_)
    S.dve(lambda e: e.reciprocal(out=gp, in_=gsum), reads=K_, writes=K_)
    S.dve(lambda e: e.tensor_scalar(out=g1h, in0=g1h, scalar1=BIG, scalar2=-BIG, op0=ALU.mult, op1=ALU.add), reads=K_, writes=K_)
    S.dve(lambda e: e.tensor_copy(out=elm, in_=lg[:, 0:N, 4:36]), reads=K_, writes=K_)
    S.dve(lambda e: e.tensor_tensor(out=elm.rearrange("p s (g e) -> p (s g) e", g=4), in0=elm.rearrange("p s (g e) -> p (s g) e", g=4),
                                    in1=bc(g1h.rearrange("p s (g o) -> p (s g) o", o=1), [128, N * 4, 8]), op=ALU.add), reads=K_, writes=K_)
    S.dve(lambda e: e.tensor_reduce(out=m1, in_=elm, axis=AX.X, op=ALU.max), reads=K_, writes=K_)
    S.dve(lambda e: e.tensor_tensor(out=oh1, in0=elm, in1=bc(A3(m1), [128, N, 32]), op=ALU.is_equal), reads=K_, writes=K_)
    S.dve(lambda e: e.scalar_tensor_tensor(out=elm, in0=oh1, scalar=-BIG, in1=elm, op0=ALU.mult, op1=ALU.add), reads=K_, writes=K_)
    S.dve(lambda e: e.tensor_reduce(out=m2, in_=elm, axis=AX.X, op=ALU.max), reads=K_, writes=K_)
    S.dve(lambda e: e.tensor_tensor(out=oh2, in0=elm, in1=bc(A3(m2), [128, N, 32]), op=ALU.is_equal), reads=K_, writes=K_)
    S.dve(lambda e: e.tensor_tensor(out=w1, in0=m2, in1=m1, op=ALU.subtract), reads=K_, writes=K_)
    S.act(lambda e: e.activation(out=w1, in_=w1, func=AF.Exp), reads=K_, writes=K_)
    S.dve(lambda e: e.tensor_scalar(out=w1, in0=w1, scalar1=1.0, scalar2=None, op0=ALU.add), reads=K_, writes=K_)
    S.dve(lambda e: e.reciprocal(out=w1, in_=w1), reads=K_, writes=K_)
    S.dve(lambda e: e.tensor_scalar(out=w2, in0=w1, scalar1=-1.0, scalar2=1.0, op0=ALU.mult, op1=ALU.add), reads=K_, writes=K_)
    S.dve(lambda e: e.tensor_tensor(out=w1, in0=w1, in1=gp, op=ALU.mult), reads=K_, writes=K_)
    S.dve(lambda e: e.tensor_tensor(out=w2, in0=w2, in1=gp, op=ALU.mult), reads=K_, writes=K_)
    S.dve(lambda e: e.tensor_tensor(out=Mb, in0=oh1, in1=oh2, op=ALU.add), reads=K_, writes=K_)
    S.dve(lambda e: e.memset(Mcum[:, 0, :], 0.0), reads=K_, writes=K_)
    S.dve(lambda e: e.memset(one32, 1.0), reads=K_, writes=K_)
    for s in range(N):
        S.dve(lambda e, s=s: e.tensor_tensor(out=Mcum[:, s + 1, :], in0=Mcum[:, s, :], in1=Mb[:, s, :], op=ALU.add), reads=K_, writes=K_)
    for s0 in range(0, N, 16):
        pr = next_pb()

        def rk(e, s0=s0, pr=pr):
            for s in range(s0, min(N, s0 + 16)):
                e.matmul(PB[pr][:, (s - s0) * 32:(s - s0 + 1) * 32], lhsT=ustr[:], rhs=Mb[:, s, :], start=True, stop=False)
                ins = e.matmul(PB[pr][:, (s - s0) * 32:(s - s0 + 1) * 32], lhsT=ones_bf[:], rhs=Mcum[:, s, :], start=False, stop=True)
            return ins
        S.pe(rk, reads=K_ + ["ustr", "ones_bf"], writes=[("pb", pr)])
        n_ = min(N, s0 + 16) - s0
        S.dve(lambda e, s0=s0, pr=pr, n_=n_: e.tensor_copy(out=rank[:, s0:s0 + n_, :], in_=PB[pr][:, 0:n_ * 32].rearrange("p (s e) -> p s e", e=32)), reads=[("pb", pr)] + K_, writes=K_)
    pcn = next_pb()
    S.pe(lambda e: e.matmul(PB[pcn][:, 0:32], lhsT=ones_bf[:], rhs=Mcum[:, N, :], start=True, stop=True), reads=K_ + ["ones_bf"], writes=[("pb", pcn)])
    S.dve(lambda e: e.tensor_copy(out=cnt, in_=PB[pcn][:, 0:32]), reads=[("pb", pcn)] + K_, writes=K_)
    cmpc = HB[:, 8192:8192 + 2048].rearrange("p (e k) -> p e k", k=64)
    S.dve(lambda e: e.tensor_tensor(out=cmpc, in0=bc(cnt.rearrange("p (e o) -> p e o", o=1), [128, 32, 64]),
                                    in1=bc(cb[:, 0:64].rearrange("p (o k) -> p o k", o=1), [128, 32, 64]), op=ALU.is_gt), reads=K_ + ["cb"], writes=K_)
    S.dve(lambda e: e.tensor_reduce(out=pc, in_=cmpc, axis=AX.X, op=ALU.add), reads=K_, writes=K_)
    S.dve(lambda e: e.tensor_scalar(out=pc, in0=pc, scalar1=128.0, scalar2=None, op0=ALU.mult), reads=K_, writes=K_)
    S.dve(lambda e: e.tensor_tensor_scan(out=pend, data0=one32, data1=pc, initial=0.0, op0=ALU.mult, op1=ALU.add), reads=K_, writes=K_)
    S.dve(lambda e: e.tensor_tensor(out=pstart, in0=pend, in1=pc, op=ALU.subtract), reads=K_, writes=K_)
    psb = lambda: bc(pstart.rearrange("p (o e) -> p o e", o=1), [128, N, 32])
    S.dve(lambda e: e.tensor_tensor(out=rank, in0=rank, in1=psb(), op=ALU.add), reads=K_, writes=K_)
    S.dve(lambda e: e.tensor_tensor(out=trr, in0=rank, in1=oh1, op=ALU.mult), reads=K_, writes=K_)
    S.dve(lambda e: e.tensor_reduce(out=d1, in_=trr, axis=AX.X, op=ALU.add), reads=K_, writes=K_)
    S.dve(lambda e: e.tensor_tensor(out=trr, in0=rank, in1=oh2, op=ALU.mult), reads=K_, writes=K_)
    S.dve(lambda e: e.tensor_reduce(out=d2, in_=trr, axis=AX.X, op=ALU.add), reads=K_, writes=K_)
    S.dve(lambda e: e.tensor_copy(out=d1i[:, 0:N], in_=d1), reads=K_, writes=K_)
    S.dve(lambda e: e.tensor_copy(out=d2i[:, 0:N], in_=d2), reads=K_, writes=K_)
    cmp3 = HF[:, 0:NBLK * 32].rearrange("p (b e) -> p b e", e=32)
    S.dve(lambda e: e.tensor_tensor(out=cmp3, in0=bc(cb[:, 0:NBLK].rearrange("p (b o) -> p b o", o=1), [128, NBLK, 32]),
                                    in1=bc(pend.rearrange("p (o e) -> p o e", o=1), [128, NBLK, 32]), op=ALU.is_ge), reads=K_ + ["cb"], writes=K_)
    S.dve(lambda e: e.tensor_reduce(out=be[:, 0:NBLK], in_=cmp3, axis=AX.X, op=ALU.add), reads=K_, writes=K_)
    S.dve(lambda e: e.tensor_scalar(out=be[:, 0:NBLK], in0=be[:, 0:NBLK], scalar1=31.0, scalar2=128.0, op0=ALU.min, op1=ALU.mult), reads=K_, writes=K_)
    S.dve(lambda e: e.tensor_scalar(out=be[:, 0:NBLK], in0=be[:, 0:NBLK], scalar1=cb[:, 96:97], scalar2=None, op0=ALU.add), reads=K_ + ["cb"], writes=K_)
    S.dve(lambda e: e.tensor_copy(out=wix[:, 0:NBLK], in_=be[:, 0:NBLK]), reads=K_, writes=K_)

    sc_keys = []
    xbr = [HB[:, 4096:5120], HB[:, 5120:6144]]
    for s in range(N):
        k = s % 4
        S.dma(lambda e, s=s, k=k: e.dma_start(out=xres[:, k, :], in_=out[s * 128:(s + 1) * 128, :]), reads=[("out", s)], writes=[("xres", k)])
        S.act(lambda e, s=s, k=k: e.activation(out=xbr[s % 2], in_=xres[:, k, :], func=AF.Copy), reads=[("xres", k)], writes=[("xbr", s % 2)])
        for di in (d1i, d2i):
            S.dma(lambda e, s=s, di=di: e.indirect_dma_start(out=xbuf[:, :], out_offset=bass.IndirectOffsetOnAxis(ap=di[:, s:s + 1], axis=0), in_=xbr[s % 2], in_offset=None),
                  reads=[("xbr", s % 2), "xbuf"] + K_, writes=[("xbufw", s, id(di))], queue="pool")
            sc_keys.append(("xbufw", s, id(di)))

    webf = L["webf"]
    wb3 = [HB[:, 6144:12288], HB[:, 12288:18432], HF[:, 0:3072].bitcast(BF16)]
    wbb = [[wb3[j][:, i * 2048:(i + 1) * 2048] for i in range(3)] for j in range(3)]
    S.barrier()
    xbk2 = [HB[:, 18432:19456], HB[:, 0:1024]]
    xbT2 = [HB[:, 19456:20480].rearrange("p (k t) -> p k t", k=8), HB[:, 1024:2048].rearrange("p (k t) -> p k t", k=8)]
    hT2 = [HB[:, 20480:20736].rearrange("p (n t) -> p n t", n=2), HB[:, 2048:2304].rearrange("p (n t) -> p n t", n=2)]
    sg22 = [HB[:, 20736:20992], HB[:, 2304:2560]]
    yb2 = [HB[:, 20992:22016], HB[:, 2560:3584]]
    def blk_gather(b):
        j3 = b % 3
        S.dma(lambda e, b=b, j3=j3: e.indirect_dma_start(out=wb3[j3], out_offset=None, in_=webf[:, :], in_offset=bass.IndirectOffsetOnAxis(ap=wix[:, b:b + 1], axis=0)),
              reads=K_ + [("webf", k) for k in range(3 * N_EXP)], writes=[("wbb", j3, 0), ("wbb", j3, 1), ("wbb", j3, 2)], queue="pool")

    def blk_front(b):
        j = b % 2
        j3 = b % 3
        xbk, xbT, hT, sg2 = xbk2[j], xbT2[j], hT2[j], sg22[j]
        S.dma(lambda e, b=b, xbk=xbk: e.dma_start(out=xbk, in_=xbuf[b * 128:(b + 1) * 128, :]), reads=["xbuf"] + sc_keys, writes=[("xbk", j)])

        def trb(e, xbk=xbk):
            for k in range(8):
                ins = e.transpose(PT[:, k * 128:(k + 1) * 128], xbk[:, k * 128:(k + 1) * 128], ident[:])
            return ins
        S.pe(trb, reads=[("xbk", j), "ident"], writes=["PT"])
        S.dve(lambda e, xbT=xbT: e.tensor_copy(out=xbT, in_=PT[:].rearrange("p (k t) -> p k t", k=8)), reads=["PT"], writes=[("xbT", j)])
        pg = next_pb()
        wg3 = wbb[j3][0].rearrange("p (k n) -> p k n", k=8)
        wu3 = wbb[j3][1].rearrange("p (k n) -> p k n", k=8)

        def gum(e, pg=pg, wg3=wg3, wu3=wu3, xbT=xbT):
            for q, w3 in enumerate((wg3, wu3)):
                for n_ in range(2):
                    for kc in range(8):
                        ins = e.matmul(PB[pg][:, (q * 2 + n_) * 128:(q * 2 + n_ + 1) * 128], lhsT=w3[:, kc, n_ * 128:(n_ + 1) * 128], rhs=xbT[:, kc, :], start=(kc == 0), stop=(kc == 7))
            return ins
        S.pe(gum, reads=[("wbb", j3, 0), ("wbb", j3, 1), ("xbT", j)], writes=[("pb", pg)])
        S.act(lambda e, pg=pg, sg2=sg2: e.activation(out=sg2, in_=PB[pg][:, 0:256], func=AF.Silu), reads=[("pb", pg)], writes=[("sg2", j)])
        S.dve(lambda e, pg=pg, sg2=sg2, hT=hT: e.tensor_tensor(out=hT, in0=sg2.rearrange("p (n t) -> p n t", n=2), in1=PB[pg][:, 256:512].rearrange("p (n t) -> p n t", n=2), op=ALU.mult), reads=[("pb", pg), ("sg2", j)], writes=[("hT", j)])

    def blk_back(b):
        j = b % 2
        hT, yb = hT2[j], yb2[j]
        j3 = b % 3
        wd3 = wbb[j3][2].rearrange("p (k n) -> p k n", k=2)
        for half in range(2):
            py = next_pb()

            def ym(e, py=py, half=half, wd3=wd3, hT=hT):
                e.matmul(PB[py][:], lhsT=hT[:, 0, :], rhs=wd3[:, 0, half * 512:(half + 1) * 512], start=True, stop=False)
                return e.matmul(PB[py][:], lhsT=hT[:, 1, :], rhs=wd3[:, 1, half * 512:(half + 1) * 512], start=False, stop=True)
            S.pe(ym, reads=[("hT", j), ("wbb", j3, 2)], writes=[("pb", py)])
            if half == 0:
                S.act(lambda e, py=py, yb=yb: e.activation(out=yb[:, 0:512], in_=PB[py][:], func=AF.Copy), reads=[("pb", py)], writes=[("yb", j, 0)])
            else:
                S.dve(lambda e, py=py, yb=yb: e.tensor_copy(out=yb[:, 512:1024], in_=PB[py][:]), reads=[("pb", py)], writes=[("yb", j, 1)])
        S.dma(lambda e, b=b, yb=yb: e.dma_start(out=ybuf[b * 128:(b + 1) * 128, :], in_=yb), reads=[("yb", j, 0), ("yb", j, 1)], writes=[("ybufw", b)])

    blk_gather(0)
    if NBLK > 1:
        blk_gather(1)
    blk_front(0)
    for b in range(NBLK):
        if b + 2 < NBLK:
            blk_gather(b + 2)
        if b + 1 < NBLK:
            blk_front(b + 1)
        blk_back(b)
    yb_keys = [("ybufw", b) for b in range(NBLK)]
    S.barrier()

    r12 = [HB[:, 0:1024], HB[:, 1024:2048], HB[:, 2048:3072], HB[:, 3072:4096]]

    def cmb_fetch(s):
        k = s % 4
        S.dma(lambda e, s=s, k=k: e.dma_start(out=xres[:, k, :], in_=out[s * 128:(s + 1) * 128, :]), reads=[("out", s)], writes=[("xres", k)])
        for q, di in enumerate((d1i, d2i)):
            S.dma(lambda e, s=s, di=di, q=q: e.indirect_dma_start(out=r12[(s % 2) * 2 + q], out_offset=None, in_=ybuf[:, :], in_offset=bass.IndirectOffsetOnAxis(ap=di[:, s:s + 1], axis=0)),
                  reads=yb_keys + K_, writes=[("r12", (s % 2) * 2 + q)], queue="pool")

    def cmb_compute(s):
        k = s % 4
        ya = HF[:, (s % 2) * 1024:(s % 2 + 1) * 1024]
        S.dve(lambda e, s=s, ya=ya: e.tensor_scalar(out=ya, in0=r12[(s % 2) * 2], scalar1=w1[:, s:s + 1], scalar2=None, op0=ALU.mult), reads=[("r12", (s % 2) * 2)] + K_, writes=[("ya", s % 2)])
        S.dve(lambda e, s=s, ya=ya: e.scalar_tensor_tensor(out=ya, in0=r12[(s % 2) * 2 + 1], scalar=w2[:, s:s + 1], in1=ya, op0=ALU.mult, op1=ALU.add), reads=[("r12", (s % 2) * 2 + 1), ("ya", s % 2)] + K_, writes=[("ya", s % 2)])
        S.dve(lambda e, k=k, ya=ya: e.scalar_tensor_tensor(out=xres[:, k, :], in0=xres[:, k, :], scalar=DN_ALPHA, in1=ya, op0=ALU.mult, op1=ALU.add), reads=[("xres", k), ("ya", s % 2)], writes=[("xres", k)])
        layer_norm(k, 1)
        S.dma(lambda e, s=s, k=k: e.dma_start(out=out[s * 128:(s + 1) * 128, :], in_=xres[:, k, :]), reads=[("xres", k)], writes=[("out", s)])

    for s in range(min(2, N)):
        cmb_fetch(s)
    for s in range(N):
        cmb_compute(s)
        if s + 2 < N:
            cmb_fetch(s + 2)


A_OFF = {"g2A": 0, "g1A": 640, "g0A": 1664, "g2B": 2688, "g1B": 2816, "g0B": 3328}
A_NB = 3840
N_EXP = 32
BIG = 1.0e4


def build_nc(stage="full"):
    quick = stage == "quick"
    n_ctx = 4 if quick else NTILES
    n_own = 2 if quick else NTILES
    NSUB = n_own * 4
    NBLK = (2 * 128 * NSUB + N_EXP * 127 + 127) // 128
    NSLOT = NBLK * 128

    nc = bass.Bass("TRN2", target_bir_lowering=False)
    din = lambda name, shape, dt=F32: nc.dram_tensor(name, list(shape), dt, kind="ExternalInput").ap()
    xT = din("xT", [D, 2 * HALF])
    xo = din("xo", [HALF, D])
    memT = din("memT", [D, 256])
    w_in = din("w_in", [D, N_IN])
    w_bb = din("w_bb", [D, D])
    w_ba = din("w_ba", [512, D])
    w_bc = din("w_bc", [512, D])
    w_o = din("w_o", [D, D])
    w_kv = din("w_kv", [D, D])
    w_r = din("w_r", [128, 8, 36])
    b_r = din("b_r", [1, 36])
    weg = din("weg", [N_EXP * 128, 2048])
    weu = din("weu", [N_EXP * 128, 2048])
    wed = din("wed", [N_EXP * 128, 2048])
    vecs = din("vecs", [8, D])
    pvec = din("pvec", [128, 3, 8])
    cmask = din("cmask", [128, 1024 + 128 + 97])
    ident_in = din("ident", [128, 128])
    abias = din("abias", [128, A_NB])
    out = nc.dram_tensor("out", [HALF, D], F32, kind="ExternalOutput").ap()
    wbf = nc.dram_tensor("wbf", [NGROUPS, 128, 4096], BF16).ap()
    KTd = nc.dram_tensor("KTd", [3, 4, 128, 2 * HALF], BF16).ap()
    Vd = nc.dram_tensor("Vd", [3, 2 * HALF, 512], BF16).ap()
    xbuf = nc.dram_tensor("xbuf", [NSLOT, D], BF16).ap()
    ybuf = nc.dram_tensor("ybuf", [NSLOT, D], BF16).ap()
    webf = nc.dram_tensor("webf", [N_EXP * 128, 6144], BF16).ap()

    with ExitStack() as st:
        def sb(name, shape, dt=F32):
            return st.enter_context(nc.sbuf_tensor("s_" + name, list(shape), dt))

        def psum(name, shape, dt=F32):
            return st.enter_context(nc.psum_tensor("p_" + name, list(shape), dt))

        S = Sched(nc)
        ident = sb("ident", [128, 128], BF16)
        identf = sb("identf", [128, 128])
        rmask = sb("rmask", [128, 512])
        cm4 = sb("cm4", [128, 512], BF16)
        ustr = sb("ustr", [128, 128], BF16)
        cb = sb("cb", [128, 97])
        ones_bf = sb("ones_bf", [128, 128], BF16)
        lbv = sb("lbv", [128, 8])
        omlv = sb("omlv", [128, 8])
        lgt = sb("lgt", [128, 2, 8])
        ngv = sb("ngv", [128, 8])
        lnrow = sb("lnrow", [128, 2, D])
        ab = sb("ab", [128, A_NB], BF16)
        wr = sb("wr", [128, 8, 36])
        brow = sb("brow", [128, 36])
        S.dma(lambda e: e.dma_start(out=ident[:], in_=ident_in), writes=["ident"], queue="pool")
        S.dma(lambda e: e.dma_start(out=identf[:], in_=ident_in), writes=["identf"])
        S.dma(lambda e: e.dma_start(out=rmask[:], in_=cmask[:, 0:512]), writes=["rmask"])
        S.dma(lambda e: e.dma_start(out=cm4[:], in_=cmask[:, 512:1024]), writes=["cm4"], queue="pool")
        S.dma(lambda e: e.dma_start(out=ustr[:], in_=cmask[:, 1024:1152]), writes=["ustr"], queue="pool")
        S.dma(lambda e: e.dma_start(out=cb[:], in_=cmask[:, 1152:1249]), writes=["cb"])
        S.dma(lambda e: e.dma_start(out=ab[:], in_=abias), writes=["ab"], queue="pool")
        S.dma(lambda e: e.dma_start(out=wr[:], in_=w_r), writes=["wr"])
        S.dma(lambda e: e.dma_start(out=brow[:], in_=b_r.partition_broadcast(128)), writes=["brow"])
        S.pool(lambda e: e.memset(ones_bf[:], 1.0), writes=["ones_bf"])
        S.dma(lambda e: e.dma_start(out=lgt[:], in_=pvec[:, 0:2, :]), writes=["lgt"])
        S.dma(lambda e: e.dma_start(out=ngv[:], in_=pvec[:, 2, :]), writes=["ngv"])
        S.dma(lambda e: e.dma_start(out=lnrow[:], in_=vecs[3:5, :].partition_broadcast(128)), writes=["lnrow"])
        S.dve(lambda e: e.tensor_tensor(out=lbv[:], in0=lgt[:, 0, :], in1=lgt[:, 1, :], op=ALU.subtract), reads=["lgt"], writes=["lbv"])
        S.act(lambda e: e.activation(out=lbv[:], in_=lbv[:], func=AF.Sigmoid), reads=["lbv"], writes=["lbv"])
        S.dve(lambda e: e.tensor_scalar(out=omlv[:], in0=lbv[:], scalar1=-1.0, scalar2=1.0, op0=ALU.mult, op1=ALU.add), reads=["lbv"], writes=["omlv"])

        xTb = [sb("xTb0", [128, 8, NT], BF16), sb("xTb1", [128, 8, NT], BF16)]
        wt = [sb("wt%d" % i, [128, 4096], BF16) for i in range(3)]
        srcs = []
        for j in range(24):
            srcs.append((w_in[:, j * 512:(j + 1) * 512].rearrange("(kc p) n -> p kc n", p=128), 8))
        srcs.append((w_bb[:, 0:512].rearrange("(kc p) n -> p kc n", p=128), 8))
        srcs.append((w_bb[:, 512:1024].rearrange("(kc p) n -> p kc n", p=128), 8))
        srcs.append((w_ba.rearrange("(kc p) n -> p kc n", p=128), 4))
        srcs.append((w_bc.rearrange("(kc p) n -> p kc n", p=128), 4))
        srcs.append((w_o[:, 0:512].rearrange("(kc p) n -> p kc n", p=128), 8))
        srcs.append((w_o[:, 512:1024].rearrange("(kc p) n -> p kc n", p=128), 8))
        srcs.append((w_kv[:, 0:512].rearrange("(kc p) n -> p kc n", p=128), 8))
        srcs.append((w_kv[:, 512:1024].rearrange("(kc p) n -> p kc n", p=128), 8))
        first = [30, 31, 11, 12, 13, 14, 9, 10, 15, 16, 20, 21, 24, 25, 28, 29]
        order = first + [g for g in range(NGROUPS) if g not in first]
        for n, g in enumerate(order):
            src, kcs = srcs[g]
            sg_ = wt[n % 3]
            S.dma(lambda e, src=src, sg_=sg_, kcs=kcs: e.dma_start(out=sg_[:].rearrange("p (kc n) -> p kc n", kc=kcs), in_=src),
                  writes=[("wt", n % 3)], queue="pool")
            S.dma(lambda e, g=g, sg_=sg_: e.dma_start(out=wbf[g], in_=sg_[:]), reads=[("wt", n % 3)], writes=[("wbf", g)])

        wt_n = [0]
        PB = [psum("pb%d" % i, [128, 512]) for i in range(7)]
        PT = psum("ptr", [128, 1024], BF16)
        HB = sb("HB", [128, 22528], BF16)
        HF = sb("HF", [128, 5120])
        khatT = HB[:, 0:4096].rearrange("p (h t) -> p h t", h=8)
        kt = HB[:, 4096:8192].rearrange("p (h t) -> p h t", h=8)
        qt = HB[:, 8192:12288].rearrange("p (h t) -> p h t", h=8)
        khat = HB[:, 12288:16384].rearrange("p (s d) -> p s d", s=4)
        Vt = HB[:, 16384:20480].rearrange("p (s d) -> p s d", s=4)
        ATs = HB[:, 20480:21504].rearrange("p (h t) -> p h t", h=8)
        tmp = [[HF[:, (i * 5 + j) * 512:(i * 5 + j + 1) * 512] for j in range(5)] for i in range(2)]
        QT = HB[:, 0:6144].rearrange("p (g t) -> p g t", g=12)
        Kwin = [HB[:, 6144:8704], HB[:, 8704:11264]]
        VA = [HB[:, 11264:11392], HB[:, 11392:11520], HB[:, 22016:22144]]
        VB = [HB[:, 11520:11648], HB[:, 11648:11776], HB[:, 22144:22272]]
        PTa = [HB[:, 11776:12032], HB[:, 12032:12288], HB[:, 22272:22528]]
        oaT = HB[:, 12288:14336].rearrange("p (h t) -> p h t", h=4)
        ocT = HB[:, 14336:16384].rearrange("p (h t) -> p h t", h=4)
        qcT = HB[:, 16384:16896]
        PcT = HB[:, 16896:17920].rearrange("p (m t) -> p m t", m=2)
        KTt = HB[:, 17920:19968].rearrange("p (h t) -> p h t", h=4)
        Vtl = HB[:, 19968:22016].rearrange("p (s d) -> p s d", s=4)
        acc_o = HF[:, 0:512]
        acc_l = HF[:, 512:1024]
        recA = HF[:, 1024:1536]
        x1Ts = sb("x1Ts", [128, 8, 128])
        x1T4 = [x1Ts, x1Ts]
        Sst = sb("Sst", [128, 8, 128])
        Sbf = sb("Sbf", [128, 8, 128], BF16)
        dec = sb("dec", [128, 8, 8])
        OT = sb("OT", [128, 8, NT])
        mrg = OT
        sgt = sb("sgt", [128, NT], BF16)
        obT = sb("obT", [128, 8, NT], BF16)
        mrgb = sb("mrgb", [128, 8, NT], BF16)
        gsb = sb("gsb", [128, NT])
        xres = sb("xres", [128, 4, D])
        bnst = sb("bnst", [128, 2, 6])
        mv = sb("mv", [128, 2])
        rstd = sb("rstd", [128, 1])
        sqb = sb("sqb", [128, NT], BF16)
        KcT = sb("KcT", [128, 4, 256], BF16)
        Vc = sb("Vc", [128, 2, 512], BF16)
        lg = sb("lg", [128, 32, 36])

        S.pool(lambda e: e.memset(gsb[:], 0.0), writes=["gsb"])
        S.dma(lambda e: e.dma_start(out=xbuf.rearrange("(b p) d -> p b d", p=128), in_=gsb[:].bitcast(BF16).rearrange("p (o d) -> p o d", o=1).to_broadcast([128, NBLK, 1024])), reads=["gsb"], writes=["xbuf"], nobar=True)
        S.pool(lambda e: e.memset(Sst[:], 0.0), writes=[("S", h) for h in range(8)])
        S.pool(lambda e: e.memset(Sbf[:], 0.0), writes=[("Sbf", h) for h in range(8)])

        wt_pool = [[0, 1, 2]]

        def load_w(g):
            i = wt_pool[0][wt_n[0] % len(wt_pool[0])]
            wt_n[0] += 1
            S.dma(lambda e, g=g, i=i: e.dma_start(out=wt[i][:], in_=wbf[g]), reads=[("wbf", g)], writes=[("wt", i)], nobar=True)
            return i

        def wview(i, kcs):
            return wt[i][:].rearrange("p (kc n) -> p kc n", kc=kcs)

        pb_n = [0]

        pb_pool = [3]
        POOLS = {3: [0, 1, 2], 7: [0, 1, 2, 3, 4, 5, 6], "H": [0, 2], "P": [1]}

        def next_pb():
            pl = POOLS[pb_pool[0]]
            pbi = pl[pb_n[0] % len(pl)]
            pb_n[0] += 1
            return pbi

        def proj_fm(wi, blk, xb, xkey, ncols=NT):
            pbi = next_pb()
            w3 = wview(wi, 8)

            def mm(e, pbi=pbi, blk=blk, xb=xb, w3=w3):
                for kc in range(8):
                    ins = e.matmul(PB[pbi][:, 0:ncols], lhsT=w3[:, kc, blk * 128:(blk + 1) * 128], rhs=xb[:, kc, 0:ncols], start=(kc == 0), stop=(kc == 7))
                return ins
            S.pe(mm, reads=[("wt", wi), xkey], writes=[("pb", pbi)])
            return pbi

        def proj_tm(wi, sub, xb, xkey):
            pbi = next_pb()
            w3 = wview(wi, 8)

            def mm(e, pbi=pbi, sub=sub, xb=xb, w3=w3):
                for kc in range(8):
                    ins = e.matmul(PB[pbi][:], lhsT=xb[:, kc, sub * 128:(sub + 1) * 128], rhs=w3[:, kc, :], start=(kc == 0), stop=(kc == 7))
                return ins
            S.pe(mm, reads=[("wt", wi), xkey], writes=[("pb", pbi)])
            return pbi

        memb = HB[:, 0:2048].rearrange("p (k m) -> p k m", k=8)
        S.dma(lambda e: e.dma_start(out=memb, in_=memT.rearrange("(kc p) m -> p kc m", p=128)), writes=["memb"], queue="pool")
        wk_ = load_w(G_KV)
        for hc in range(4):
            pk = proj_fm(wk_, hc, memb, "memb", ncols=256)
            S.act(lambda e, pk=pk, hc=hc: e.activation(out=KcT[:, hc, :], in_=PB[pk][:, 0:256], func=AF.Copy), reads=[("pb", pk)], writes=["KcT"])
        wv_ = load_w(G_KV + 1)
        for ms in range(2):
            pv = proj_tm(wv_, ms, memb, "memb")
            S.act(lambda e, pv=pv, ms=ms: e.activation(out=Vc[:, ms, :], in_=PB[pv][:], func=AF.Copy), reads=[("pb", pv)], writes=["Vc"])
        S.barrier()

        def phase_h(tix, is_ctx, xb, xkey, pre=None):
            for hg in range(2):
                if pre is not None and hg == 0:
                    wf, wq = pre
                else:
                    wf = load_w(G_BF + hg)
                    wq = load_w(G_BQ + hg) if not is_ctx else None
                for pr_ in range(2):
                    hs = [hg * 4 + pr_ * 2, hg * 4 + pr_ * 2 + 1]
                    Ts = {h: tmp[h % 2] for h in hs}
                    tk = lambda h, j: ("tmp", h % 2, j)
                    for h in hs:
                        T = Ts[h]
                        pf = proj_fm(wf, h % 4, xb, xkey)
                        S.act(lambda e, T=T, pf=pf: e.activation(out=T[0], in_=PB[pf][:], func=AF.Sigmoid), reads=[("pb", pf)], writes=[tk(h, 0)])
                        if not is_ctx:
                            pq = proj_fm(wq, h % 4, xb, xkey)
                            S.act(lambda e, T=T, pq=pq: e.activation(out=T[4], in_=PB[pq][:], func=AF.Silu), reads=[("pb", pq)], writes=[tk(h, 4)])
                    for h in hs:
                        T = Ts[h]
                        S.dve(lambda e, T=T, h=h: e.tensor_scalar(out=T[0], in0=T[0], scalar1=omlv[:, h:h + 1], scalar2=lbv[:, h:h + 1], op0=ALU.mult, op1=ALU.add),
                              reads=[tk(h, 0), "lbv", "omlv"], writes=[tk(h, 0)])
                    for h in hs:
                        T = Ts[h]
                        S.act(lambda e, T=T: e.activation(out=T[1], in_=T[0], func=AF.Ln), reads=[tk(h, 0)], writes=[tk(h, 1)])
                    for h in hs:
                        T = Ts[h]
                        S.dve(lambda e, T=T: e.tensor_tensor_scan(out=T[2], data0=rmask[:], data1=T[1], initial=0.0, op0=ALU.mult, op1=ALU.add),
                              reads=[tk(h, 1), "rmask"], writes=[tk(h, 2)])
                        S.pool(lambda e, T=T: e.tensor_scalar(out=T[0], in0=T[0], scalar1=-1.0, scalar2=1.0, op0=ALU.mult, op1=ALU.add), reads=[tk(h, 0)], writes=[tk(h, 0)])

                        def dfn(e, T=T):
                            b3 = T[2].rearrange("p (c s) -> p c s", s=64)
                            return e.tensor_tensor(out=T[3].rearrange("p (c s) -> p c s", s=64), in0=b3[:, :, 63:64].to_broadcast([128, 8, 64]), in1=b3, op=ALU.subtract)
                        S.dve(dfn, reads=[tk(h, 2)], writes=[tk(h, 3)])
                    for h in hs:
                        T = Ts[h]
                        S.act(lambda e, T=T: e.activation(out=T[3], in_=T[3], func=AF.Exp), reads=[tk(h, 3)], writes=[tk(h, 3)])
                        S.act(lambda e, T=T, h=h: e.activation(out=dec[:, h, :], in_=T[2][:, 63::64], func=AF.Exp), reads=[tk(h, 2)], writes=[("dec", h)])
                    for h in hs:
                        T = Ts[h]
                        S.dve(lambda e, T=T, h=h: e.tensor_tensor(out=khatT[:, h, :], in0=T[0], in1=T[3], op=ALU.mult), reads=[tk(h, 0), tk(h, 3)], writes=[("khatT", h)])
                    if not is_ctx:
                        for h in hs:
                            T = Ts[h]
                            S.act(lambda e, T=T: e.activation(out=T[3], in_=T[2], func=AF.Exp, scale=-1.0), reads=[tk(h, 2)], writes=[tk(h, 3)])
                        for h in hs:
                            T = Ts[h]
                            S.pool(lambda e, T=T, h=h: e.tensor_tensor(out=kt[:, h, :], in0=T[0], in1=T[3], op=ALU.mult), reads=[tk(h, 0), tk(h, 3)], writes=[("kt", h)])
                        for h in hs:
                            T = Ts[h]
                            S.act(lambda e, T=T: e.activation(out=T[2], in_=T[2], func=AF.Exp), reads=[tk(h, 2)], writes=[tk(h, 2)])
                        for h in hs:
                            T = Ts[h]
                            S.dve(lambda e, T=T, h=h: e.tensor_tensor(out=qt[:, h, :], in0=T[4], in1=T[2], op=ALU.mult), reads=[tk(h, 4), tk(h, 2)], writes=[("qt", h)])
            for hg in range(2):
                wi_ = load_w(G_BI + hg)
                for sub in range(4):
                    pv = proj_tm(wi_, sub, xb, xkey)
                    S.act(lambda e, pv=pv, sub=sub, hg=hg: e.activation(out=Vt[:, sub, hg * 512:(hg + 1) * 512], in_=PB[pv][:], func=AF.Copy),
                          reads=[("pb", pv)], writes=[("V", sub, hg)])
            for sub in range(4):
                for hg in range(2):
                    def trf(e, sub=sub, hg=hg):
                        for hh in range(4):
                            ins = e.transpose(PT[:, (hg * 4 + hh) * 128:(hg * 4 + hh + 1) * 128], khatT[:, hg * 4 + hh, sub * 128:(sub + 1) * 128], ident[:])
                        return ins
                    S.pe(trf, reads=[("khatT", hg * 4 + hh) for hh in range(4)] + ["ident"], writes=["PT"])
                    S.dve(lambda e, sub=sub, hg=hg: e.tensor_copy(out=khat[:, sub, hg * 512:(hg + 1) * 512], in_=PT[:, hg * 512:(hg + 1) * 512]),
                          reads=["PT"], writes=[("khat", sub, hg)])
            for sub in range(4):
                if not is_ctx:
                    for hg in range(2):
                        def amm(e, sub=sub, hg=hg):
                            for hh in range(4):
                                h = hg * 4 + hh
                                ins = e.matmul(PB[3][:, hh * 128:(hh + 1) * 128], lhsT=kt[:, h, sub * 128:(sub + 1) * 128], rhs=qt[:, h, sub * 128:(sub + 1) * 128], start=True, stop=True)
                            return ins
                        S.pe(amm, reads=[("kt", hg * 4 + hh) for hh in range(4)] + [("qt", hg * 4 + hh) for hh in range(4)], writes=[("pb", 3)])
                        S.dve(lambda e, hg=hg: e.tensor_tensor(out=ATs[:, hg * 4:(hg + 1) * 4, :], in0=PB[3][:].rearrange("p (h t) -> p h t", h=4), in1=cm4[:].rearrange("p (h t) -> p h t", h=4), op=ALU.mult),
                              reads=[("pb", 3), "cm4"], writes=[("ATs", hg)])
                    for hg in range(2):
                        def omm(e, sub=sub, hg=hg):
                            for hh in range(4):
                                h = hg * 4 + hh
                                ins = e.matmul(PB[5 + hg][:, hh * 128:(hh + 1) * 128], lhsT=Vt[:, sub, h * 128:(h + 1) * 128], rhs=ATs[:, h, :], start=(hh == 0), stop=False)
                            return ins
                        S.pe(omm, reads=[("V", sub, hg), ("ATs", hg)], writes=[("ot", hg)])
                for c in range(2):
                    ch = sub * 2 + c
                    r0 = c * 64
                    for hg in range(2):
                        if not is_ctx:
                            def sqm(e, sub=sub, hg=hg, c=c):
                                for hh in range(4):
                                    h = hg * 4 + hh
                                    ins = e.matmul(PB[5 + hg][:, hh * 128 + c * 64:hh * 128 + c * 64 + 64], lhsT=Sbf[:, h, :], rhs=qt[:, h, sub * 128 + c * 64:sub * 128 + c * 64 + 64], start=False, stop=(c == 1 and hh == 3))
                                return ins
                            S.pe(sqm, reads=[("Sbf", hg * 4 + hh) for hh in range(4)] + [("qt", hg * 4 + hh) for hh in range(4)], writes=[("ot", hg)])

                        def umm(e, sub=sub, hg=hg, r0=r0):
                            for hh in range(4):
                                h = hg * 4 + hh
                                ins = e.matmul(PB[4][:, hh * 128:(hh + 1) * 128] if hg == 0 else PT_U[:, hh * 128:(hh + 1) * 128], lhsT=khat[r0:r0 + 64, sub, h * 128:(h + 1) * 128], rhs=Vt[r0:r0 + 64, sub, h * 128:(h + 1) * 128], start=True, stop=True)
                            return ins
                        S.pe(umm, reads=[("khat", sub, hg), ("V", sub, hg)], writes=[("pb", 4 if hg == 0 else 2)])
                        for hh in range(4):
                            h = hg * 4 + hh
                            S.dve(lambda e, h=h, hg=hg, hh=hh, ch=ch: e.scalar_tensor_tensor(out=Sst[:, h, :], in0=Sst[:, h, :], scalar=dec[:, h, ch:ch + 1], in1=(PB[4] if hg == 0 else PT_U)[:, hh * 128:(hh + 1) * 128], op0=ALU.mult, op1=ALU.add),
                                  reads=[("pb", 4 if hg == 0 else 2), ("dec", h), ("S", h)], writes=[("S", h)])
                            if (not is_ctx) or (sub == 3 and c == 1):
                                S.act(lambda e, h=h: e.activation(out=Sbf[:, h, :], in_=Sst[:, h, :], func=AF.Copy), reads=[("S", h)], writes=[("Sbf", h)])
                if not is_ctx:
                    for hg in range(2):
                        S.act(lambda e, hg=hg, sub=sub: e.activation(out=OT[:, hg * 4:(hg + 1) * 4, sub * 128:(sub + 1) * 128], in_=PB[5 + hg][:].rearrange("p (h t) -> p h t", h=4), func=AF.Copy),
                              reads=[("ot", hg)], writes=[("OT", hg * 4 + hh) for hh in range(4)])
            if is_ctx:
                return
            for hg in range(2):
                wg = load_w(G_BG + hg)
                for hh in range(4):
                    h = hg * 4 + hh
                    okeys = [("OT", h)]
                    pg = proj_fm(wg, hh, xb, xkey)
                    S.act(lambda e, pg=pg: e.activation(out=sgt[:], in_=PB[pg][:], func=AF.Silu), reads=[("pb", pg)], writes=["sgt"])
                    S.pool(lambda e, h=h: e.tensor_tensor(out=sqb[:], in0=OT[:, h, :], in1=OT[:, h, :], op=ALU.mult), reads=okeys, writes=["sqb"])
                    pbi = next_pb()
                    S.pe(lambda e, pbi=pbi: e.matmul(PB[pbi][:], lhsT=ones_bf[:], rhs=sqb[:], start=True, stop=True), reads=["sqb", "ones_bf"], writes=[("pb", pbi)])
                    S.dve(lambda e, pbi=pbi: e.tensor_scalar(out=gsb[:], in0=PB[pbi][:], scalar1=1.0 / 128.0, scalar2=RMS_EPS, op0=ALU.mult, op1=ALU.add), reads=[("pb", pbi)], writes=["gsb"])
                    S.act(lambda e: e.activation(out=gsb[:], in_=gsb[:], func=AF.Ln), reads=["gsb"], writes=["gsb"])
                    S.act(lambda e: e.activation(out=gsb[:], in_=gsb[:], func=AF.Exp, scale=-0.5), reads=["gsb"], writes=["gsb"])
                    S.dve(lambda e, h=h: e.tensor_tensor(out=OT[:, h, :], in0=OT[:, h, :], in1=gsb[:], op=ALU.mult), reads=okeys + ["gsb"], writes=okeys)
                    S.dve(lambda e, h=h: e.scalar_tensor_tensor(out=obT[:, h, :], in0=OT[:, h, :], scalar=ngv[:, h:h + 1], in1=sgt[:], op0=ALU.mult, op1=ALU.mult),
                          reads=okeys + ["sgt", "ngv"], writes=[("obT", h)])

        PT_U = PB[2]

        def phase_a_proj(tok0, is_ctx, groups, xb, xkey):
            for g in groups:
                if not is_ctx:
                    wq = load_w(3 * g)
                    for hh in range(4):
                        pq = proj_fm(wq, hh, xb, xkey)
                        S.act(lambda e, pq=pq, g=g, hh=hh: e.activation(out=QT[:, g * 4 + hh, :], in_=PB[pq][:], func=AF.Copy, scale=128.0 ** -0.5), reads=[("pb", pq)], writes=[("QT", g * 4 + hh)])
                wk = load_w(3 * g + 1)
                for hh in range(4):
                    pk = proj_fm(wk, hh, xb, xkey)
                    S.dve(lambda e, pk=pk, hh=hh: e.tensor_copy(out=KTt[:, hh, :], in_=PB[pk][:]), reads=[("pb", pk)], writes=[("KTt", hh)])
                S.dma(lambda e, g=g, tok0=tok0: e.dma_start(out=KTd[g, :, :, tok0:tok0 + NT].rearrange("h p t -> p h t"), in_=KTt), reads=[("KTt", hh) for hh in range(4)], writes=[("KTd", g)])
                wv = load_w(3 * g + 2)
                for sub in range(4):
                    pv = proj_tm(wv, sub, xb, xkey)
                    S.act(lambda e, pv=pv, sub=sub: e.activation(out=Vtl[:, sub, :], in_=PB[pv][:], func=AF.Copy), reads=[("pb", pv)], writes=[("Vtl", sub)])
                S.dma(lambda e, g=g, tok0=tok0: e.dma_start(out=Vd[g, tok0:tok0 + NT, :].rearrange("(s p) d -> p s d", p=128), in_=Vtl), reads=[("Vtl", sub) for sub in range(4)], writes=[("Vd", g)])

        def phase_a_attn(tix):
            tok0 = HALF + tix * NT
            kw_n = [0]
            un_ = [0]
            for h in range(4):
                first = True
                for g, (win, r) in enumerate(GROUPS_A):
                    Hh = win
                    W = Hh + NT
                    nqb = min(128, NT // r)
                    ki = kw_n[0] % 2
                    kw_n[0] += 1
                    S.dma(lambda e, g=g, h=h, ki=ki, W=W, Hh=Hh: e.dma_start(out=Kwin[ki][:, 0:W], in_=KTd[g, h, :, tok0 - Hh:tok0 + NT]), reads=[("KTd", g)], writes=[("Kwin", ki)])
                    kwv = Kwin[ki]
                    gname = "g%d" % g
                    units = [(c, 0) for c in range(r)] if r > 1 else [(0, qb) for qb in range(4)]
                    for (c, qb) in units:
                        pA = c + r * 128 * qb
                        pBq = pA + 128 * r
                        q0 = c + 128 * qb
                        if g == 2:
                            var = min(tix, 4)
                        elif g == 1:
                            var = 0 if tix == 0 else 1
                        else:
                            var = 0 if (tix == 0 and qb == 0) else 1
                        offA = A_OFF[gname + "A"] + (var * 4 + h) * nqb
                        offB = A_OFF[gname + "B"] + h * nqb
                        vi = un_[0] % 3
                        un_[0] += 1
                        rowA = tok0 - Hh + pA
                        S.dma(lambda e, g=g, h=h, vi=vi, rowA=rowA, r=r: e.dma_start(out=VA[vi], in_=Vd[g, rowA:rowA + 127 * r + 1:r, h * 128:(h + 1) * 128]), reads=[("Vd", g)], writes=[("VA", vi)])
                        S.dma(lambda e, g=g, h=h, vi=vi, rowA=rowA, r=r, nqb=nqb: e.dma_start(out=VB[vi][0:nqb, :], in_=Vd[g, rowA + 128 * r:rowA + 128 * r + (nqb - 1) * r + 1:r, h * 128:(h + 1) * 128]), reads=[("Vd", g)], writes=[("VB", vi)])
                        ps = next_pb()

                        def smm(e, ps=ps, kwv=kwv, pA=pA, pBq=pBq, r=r, nqb=nqb, g=g, h=h, q0=q0, offA=offA, offB=offB):
                            qv = QT[:, g * 4 + h, q0:q0 + (nqb - 1) * r + 1:r]
                            e.matmul(PB[ps][:, 0:nqb], lhsT=kwv[:, pA:pA + 127 * r + 1:r], rhs=qv, start=True, stop=False)
                            e.matmul(PB[ps][:, 0:nqb], lhsT=ident[:], rhs=ab[:, offA:offA + nqb], start=False, stop=True)
                            e.matmul(PB[ps][0:nqb, 128:128 + nqb], lhsT=kwv[:, pBq:pBq + (nqb - 1) * r + 1:r], rhs=qv, start=True, stop=False)
                            return e.matmul(PB[ps][0:nqb, 128:128 + nqb], lhsT=ident[0:nqb, 0:nqb], rhs=ab[0:nqb, offB:offB + nqb], start=False, stop=True)
                        S.pe(smm, reads=[("Kwin", ki), ("QT", g * 4 + h), "ab", "ident"], writes=[("pb", ps)])
                        pi = vi

                        def efn(e, ps=ps, pi=pi, nqb=nqb):
                            e.activation(out=PTa[pi][:, 0:nqb], in_=PB[ps][:, 0:nqb], func=AF.Exp)
                            return e.activation(out=PTa[pi][0:nqb, 128:128 + nqb], in_=PB[ps][0:nqb, 128:128 + nqb], func=AF.Exp)
                        S.act(efn, reads=[("pb", ps)], writes=[("PTa", pi)])
                        po = next_pb()

                        def omm2(e, po=po, pi=pi, vi=vi, nqb=nqb):
                            e.matmul(PB[po][:, 0:nqb], lhsT=VA[vi], rhs=PTa[pi][:, 0:nqb], start=True, stop=False)
                            e.matmul(PB[po][:, 0:nqb], lhsT=VB[vi][0:nqb, :], rhs=PTa[pi][0:nqb, 128:128 + nqb], start=False, stop=True)
                            e.matmul(PB[po][:, 128:128 + nqb], lhsT=ones_bf[:], rhs=PTa[pi][:, 0:nqb], start=True, stop=False)
                            return e.matmul(PB[po][:, 128:128 + nqb], lhsT=ones_bf[0:nqb, :], rhs=PTa[pi][0:nqb, 128:128 + nqb], start=False, stop=True)
                        S.pe(omm2, reads=[("PTa", pi), ("VA", vi), ("VB", vi), "ones_bf"], writes=[("pb", po)])
                        qsl = slice(q0, q0 + (nqb - 1) * r + 1, r)
                        if first:
                            S.dve(lambda e, po=po, qsl=qsl, nqb=nqb: e.tensor_copy(out=acc_o[:, qsl], in_=PB[po][:, 0:nqb]), reads=[("pb", po)], writes=["acc_o"])
                            S.dve(lambda e, po=po, qsl=qsl, nqb=nqb: e.tensor_copy(out=acc_l[:, qsl], in_=PB[po][:, 128:128 + nqb]), reads=[("pb", po)], writes=["acc_l"])
                        else:
                            S.dve(lambda e, po=po, qsl=qsl, nqb=nqb: e.tensor_tensor(out=acc_o[:, qsl], in0=acc_o[:, qsl], in1=PB[po][:, 0:nqb], op=ALU.add), reads=[("pb", po)], writes=["acc_o"])
                            S.dve(lambda e, po=po, qsl=qsl, nqb=nqb: e.tensor_tensor(out=acc_l[:, qsl], in0=acc_l[:, qsl], in1=PB[po][:, 128:128 + nqb], op=ALU.add), reads=[("pb", po)], writes=["acc_l"])
                    first = False
                S.dve(lambda e: e.reciprocal(out=recA, in_=acc_l), reads=["acc_l"], writes=["recA"])
                S.dve(lambda e, h=h: e.tensor_tensor(out=oaT[:, h, :], in0=acc_o, in1=recA, op=ALU.mult), reads=["acc_o", "recA"], writes=[("oaT", h)])

        def phase_c(xb, xkey):
            wq = load_w(G_C)
            for hc in range(4):
                pq = proj_fm(wq, hc, xb, xkey)
                S.act(lambda e, pq=pq: e.activation(out=qcT, in_=PB[pq][:], func=AF.Copy, scale=128.0 ** -0.5), reads=[("pb", pq)], writes=["qcT"])
                for mc in range(2):
                    ps = next_pb()
                    S.pe(lambda e, ps=ps, hc=hc, mc=mc: e.matmul(PB[ps][:], lhsT=KcT[:, hc, mc * 128:(mc + 1) * 128], rhs=qcT, start=True, stop=True), reads=["KcT", "qcT"], writes=[("pb", ps)])
                    S.act(lambda e, ps=ps, mc=mc: e.activation(out=PcT[:, mc, :], in_=PB[ps][:], func=AF.Exp), reads=[("pb", ps)], writes=[("PcT", mc)])
                po = next_pb()
                pl = next_pb()

                def cmm(e, po=po, hc=hc):
                    e.matmul(PB[po][:], lhsT=Vc[:, 0, hc * 128:(hc + 1) * 128], rhs=PcT[:, 0, :], start=True, stop=False)
                    return e.matmul(PB[po][:], lhsT=Vc[:, 1, hc * 128:(hc + 1) * 128], rhs=PcT[:, 1, :], start=False, stop=True)
                S.pe(cmm, reads=["Vc", ("PcT", 0), ("PcT", 1)], writes=[("pb", po)])

                def lmm(e, pl=pl):
                    e.matmul(PB[pl][:], lhsT=ones_bf[:], rhs=PcT[:, 0, :], start=True, stop=False)
                    return e.matmul(PB[pl][:], lhsT=ones_bf[:], rhs=PcT[:, 1, :], start=False, stop=True)
                S.pe(lmm, reads=["ones_bf", ("PcT", 0), ("PcT", 1)], writes=[("pb", pl)])
                S.dve(lambda e, pl=pl: e.reciprocal(out=recA, in_=PB[pl][:]), reads=[("pb", pl)], writes=["recA"])
                S.dve(lambda e, po=po, hc=hc: e.tensor_tensor(out=ocT[:, hc, :], in0=PB[po][:], in1=recA, op=ALU.mult), reads=[("pb", po), "recA"], writes=[("ocT", hc)])

        def branch_merge(src, nk, wgroup, gate_group0, xb, xkey, srckeys, first, pre=None):
            if pre is not None:
                wis = pre[0:2]
            elif nk == 8:
                wis = [load_w(wgroup), load_w(wgroup + 1)]
            else:
                wis = [load_w(wgroup)]
            for half in range(2):
                wgt = pre[2] if (pre is not None and half == 0) else load_w(gate_group0 + half)
                for blk in range(4):
                    fc = half * 4 + blk
                    pgate = proj_fm(wgt, blk, xb, xkey)
                    S.act(lambda e, pgate=pgate: e.activation(out=gsb[:], in_=PB[pgate][:], func=AF.Sigmoid), reads=[("pb", pgate)], writes=["gsb"])
                    pbi = next_pb()
                    if nk == 8:
                        w3 = wview(wis[half], 8)
                        c0 = blk * 128
                        wkey = ("wt", wis[half])
                    else:
                        w3 = wview(wis[0], 4)
                        c0 = fc * 128
                        wkey = ("wt", wis[0])

                    def mmb(e, pbi=pbi, w3=w3, c0=c0):
                        for k in range(nk):
                            ins = e.matmul(PB[pbi][:], lhsT=w3[:, k, c0:c0 + 128], rhs=src[:, k, :], start=(k == 0), stop=(k == nk - 1))
                        return ins
                    S.pe(mmb, reads=[wkey] + srckeys, writes=[("pb", pbi)])
                    if first:
                        S.dve(lambda e, pbi=pbi, fc=fc: e.tensor_tensor(out=mrg[:, fc, :], in0=PB[pbi][:], in1=gsb[:], op=ALU.mult), reads=[("pb", pbi), "gsb"], writes=[("OT", fc)])
                    else:
                        S.dve(lambda e, pbi=pbi: e.tensor_tensor(out=gsb[:], in0=PB[pbi][:], in1=gsb[:], op=ALU.mult), reads=[("pb", pbi), "gsb"], writes=["gsb"])
                        S.pool(lambda e, fc=fc: e.tensor_tensor(out=mrg[:, fc, :], in0=mrg[:, fc, :], in1=gsb[:], op=ALU.add), reads=["gsb", ("OT", fc)], writes=[("OT", fc)])

        def layer_norm(sub, which):
            xk = ("xres", sub)

            def st_(e, sub=sub):
                e.bn_stats(out=bnst[:, 0, :], in_=xres[:, sub, 0:512])
                return e.bn_stats(out=bnst[:, 1, :], in_=xres[:, sub, 512:1024])
            S.dve(st_, reads=[xk], writes=["bnst"])
            S.dve(lambda e: e.bn_aggr(out=mv[:], in_=bnst[:].rearrange("p a b -> p (a b)")), reads=["bnst"], writes=["mv"])
            S.dve(lambda e: e.tensor_scalar(out=rstd[:], in0=mv[:, 1:2], scalar1=LN_EPS, scalar2=None, op0=ALU.add), reads=["mv"], writes=["rstd"])
            S.act(lambda e: e.activation(out=rstd[:], in_=rstd[:], func=AF.Ln), reads=["rstd"], writes=["rstd"])
            S.act(lambda e: e.activation(out=rstd[:], in_=rstd[:], func=AF.Exp, scale=-0.5), reads=["rstd"], writes=["rstd"])
            S.dve(lambda e, sub=sub: e.tensor_scalar(out=xres[:, sub, :], in0=xres[:, sub, :], scalar1=mv[:, 0:1], scalar2=rstd[:], op0=ALU.subtract, op1=ALU.mult), reads=[xk, "mv", "rstd"], writes=[xk])
            S.dve(lambda e, sub=sub: e.tensor_tensor(out=xres[:, sub, :], in0=xres[:, sub, :], in1=lnrow[:, 0, :], op=ALU.mult), reads=[xk, "lnrow"], writes=[xk])
            S.pool(lambda e, sub=sub: e.tensor_tensor(out=xres[:, sub, :], in0=xres[:, sub, :], in1=lnrow[:, 1, :], op=ALU.add), reads=[xk, "lnrow"], writes=[xk])

        def phase_out(tix):
            for fc in range(8):
                S.pool(lambda e, fc=fc: e.tensor_copy(out=mrgb[:, fc, :], in_=mrg[:, fc, :]), reads=[("OT", fc)], writes=[("mrgb", fc)])
            S.dma(lambda e, tix=tix: e.dma_start(out=xres[:], in_=xo[tix * NT:(tix + 1) * NT, :].rearrange("(s p) d -> p s d", p=128)), writes=[("xres", s_) for s_ in range(4)])
            for half in range(2):
                wo_ = load_w(G_WO + half)
                for sub in range(4):
                    pbi = next_pb()
                    w3 = wview(wo_, 8)

                    def mmo(e, pbi=pbi, sub=sub, w3=w3):
                        for kc in range(8):
                            ins = e.matmul(PB[pbi][:], lhsT=mrgb[:, kc, sub * 128:(sub + 1) * 128], rhs=w3[:, kc, :], start=(kc == 0), stop=(kc == 7))
                        return ins
                    S.pe(mmo, reads=[("wt", wo_)] + [("mrgb", fc) for fc in range(8)], writes=[("pb", pbi)])
                    S.dve(lambda e, pbi=pbi, sub=sub, half=half: e.scalar_tensor_tensor(out=xres[:, sub, half * 512:(half + 1) * 512], in0=xres[:, sub, half * 512:(half + 1) * 512], scalar=DN_ALPHA, in1=PB[pbi][:], op0=ALU.mult, op1=ALU.add),
                          reads=[("pb", pbi), ("xres", sub)], writes=[("xres", sub)])
            for sub in range(4):
                layer_norm(sub, 0)
            for sub in range(4):
                gs = tix * 4 + sub
                for half in range(2):
                    pt_ = next_pb()

                    def trx(e, pt_=pt_, sub=sub, half=half):
                        for k in range(4):
                            kc = half * 4 + k
                            ins = e.transpose(PB[pt_][:, k * 128:(k + 1) * 128], xres[:, sub, kc * 128:(kc + 1) * 128], identf[:])
                        return ins
                    S.pe(trx, reads=[("xres", sub), "identf"], writes=[("pb", pt_)])
                    S.act(lambda e, pt_=pt_, half=half, sub=sub: e.activation(out=x1T4[sub % 2][:, half * 4:(half + 1) * 4, :], in_=PB[pt_][:].rearrange("p (k t) -> p k t", k=4), func=AF.Copy), reads=[("pb", pt_)], writes=[("x1T", 0, half)])
                pl_ = next_pb()

                def rmm(e, pl_=pl_, sub=sub):
                    for kc in range(8):
                        ins = e.matmul(PB[pl_][:, 0:36], lhsT=x1T4[sub % 2][:, kc, :], rhs=wr[:, kc, :], start=(kc == 0), stop=(kc == 7))
                    return ins
                S.pe(rmm, reads=[("x1T", 0, 0), ("x1T", 0, 1), "wr"], writes=[("pb", pl_)])
                S.dve(lambda e, pl_=pl_, gs=gs: e.tensor_tensor(out=lg[:, gs, :], in0=PB[pl_][:, 0:36], in1=brow[:], op=ALU.add), reads=[("pb", pl_), "brow"], writes=["lg"])
            S.dma(lambda e, tix=tix: e.dma_start(out=out[tix * NT:(tix + 1) * NT, :].rearrange("(s p) d -> p s d", p=128), in_=xres[:]),
                  reads=[("xres", s_) for s_ in range(4)], writes=[("out", tix * 4 + s_) for s_ in range(4)])

        xn = [0]

        def load_x(tok0):
            i = xn[0] % 2
            xn[0] += 1
            xb = xTb[i]
            S.dma(lambda e, xb=xb, tok0=tok0: e.dma_start(out=xb[:], in_=xT[:, tok0:tok0 + NT].rearrange("(kc p) t -> p kc t", p=128)),
                  writes=[("xTb", i)], queue="pool", nobar=True)
            return xb, ("xTb", i)

        tiles = [(t, True) for t in range(NTILES - n_ctx, NTILES)] + [(t, False) for t in range(n_own)]
        tok_of = lambda t, c: t * NT + (0 if c else HALF)
        nxt = load_x(tok_of(*tiles[0]))
        ecast_n = [0]

        estg = [sb("estg0", [128, 2048], BF16)]
        estg = [estg[0], estg[0]]

        def expert_casts(n):
            for _ in range(n):
                k = ecast_n[0]
                if k >= 3 * N_EXP:
                    return
                ecast_n[0] += 1
                wi, ex = k % 3, k // 3
                i = k % 2
                src = (weg, weu, wed)[wi]
                S.dma(lambda e, src=src, ex=ex, i=i: e.dma_start(out=estg[i][:], in_=src[ex * 128:(ex + 1) * 128, :]), writes=[("estg", 0)], queue="pool", nobar=True)
                S.dma(lambda e, wi=wi, ex=ex, i=i: e.dma_start(out=webf[ex * 128:(ex + 1) * 128, wi * 2048:(wi + 1) * 2048], in_=estg[i][:]), reads=[("estg", 0)], writes=[("webf", k)], nobar=True)

        pending_out = None
        pre_h = None
        for n_, (t, is_ctx) in enumerate(tiles):
            xb, xkey = nxt
            if n_ + 1 < len(tiles):
                nxt = load_x(tok_of(*tiles[n_ + 1]))
            pb_pool[0] = 3
            if is_ctx:
                expert_casts(1)
                phase_h(t, True, xb, xkey)
                S.barrier()
                expert_casts(1)
                groups = ([2] if t >= 4 else []) + ([1, 0] if t == 7 else [])
                if groups:
                    phase_a_proj(t * NT, True, groups, xb, xkey)
                expert_casts(1)
                S.barrier()
            else:
                expert_casts(1)
                pb_pool[0] = "H"
                wt_pool[0] = [0, 1]
                S.capture()
                phase_h(t, False, xb, xkey, pre=pre_h)
                hl = S.end_capture()
                pre_h = None
                wt_pool[0] = [0, 1, 2]
                S.replay([hl] + ([pending_out] if pending_out else []))
                pending_out = None
                pre = [load_w(G_WB), load_w(G_WB + 1), load_w(G_GATE + 2)]
                S.barrier()
                pb_pool[0] = 7
                expert_casts(1)
                branch_merge(obT, 8, G_WB, G_GATE + 2, xb, xkey, [("obT", h) for h in range(8)], True, pre=pre)
                expert_casts(1)
                phase_c(xb, xkey)
                expert_casts(1)
                branch_merge(ocT, 4, G_WC, G_GATE + 4, xb, xkey, [("ocT", h) for h in range(4)], False)
                expert_casts(1)
                phase_a_proj(HALF + t * NT, False, [0, 1, 2], xb, xkey)
                expert_casts(1)
                phase_a_attn(t)
                expert_casts(1)
                branch_merge(oaT, 4, G_WA, G_GATE, xb, xkey, [("oaT", h) for h in range(4)], False)
                expert_casts(1)
                if n_ + 1 < len(tiles):
                    wt_pool[0] = [0, 1]
                    pre_h = (load_w(G_BF), load_w(G_BQ))
                    wt_pool[0] = [0, 1, 2]
                S.barrier()
                pb_pool[0] = "P"
                wt_pool[0] = [2]
                S.capture()
                phase_out(t)
                pending_out = S.end_capture()
                wt_pool[0] = [0, 1, 2]
                expert_casts(1)
        if pending_out:
            S.replay([pending_out])
        S.barrier()
        pb_pool[0] = 7
        expert_casts(3 * N_EXP)

        moe_phase(nc, S, locals())
        S.add("sp", lambda e: None, reads=[("out", s_) for s_ in range(NSUB)])
        S.emit(st)
        print("sched stats", S.stats, flush=True)
    return nc


def _consts():
    cm = np.zeros((128, 1024 + 128 + 97), np.float32)
    rm = np.ones(512, np.float32)
    rm[0::64] = 0.0
    cm[:, 0:512] = rm[None, :]
    s = np.arange(128)[:, None]
    t = np.arange(128)[None, :]
    m = ((s // 64) == (t // 64)) & (s <= t)
    cm[:, 512:1024] = np.tile(m.astype(np.float32), (1, 4))
    cm[:, 1024:1152] = (s < t).astype(np.float32)
    cm[:, 1152:1248] = (128.0 * np.arange(96))[None, :]
    cm[:, 1248] = np.arange(128)
    return cm, np.eye(128, dtype=np.float32)


def _abias(rel_bias, half):
    ab = np.full((128, A_NB), NEG, np.float32)
    for g, (win, r) in enumerate(GROUPS_A):
        nqb = min(128, NT // r)
        gname = "g%d" % g
        jj = np.arange(128)[:, None]
        mm = np.arange(nqb)[None, :]
        uA = 128 + mm - jj
        validA = jj >= mm
        bA = _t5_bucket_np(np.clip(uA, 0, 128) * r)
        jb = np.arange(nqb)[:, None]
        uB = mm - jb
        validB = jb <= mm
        bB = _t5_bucket_np(np.clip(uB, 0, 128) * r)
        nvar = 5 if g == 2 else 2
        for h in range(4):
            tabA = np.where(validA, rel_bias[bA, g * 4 + h], NEG).astype(np.float32)
            tabB = np.where(validB, rel_bias[bB, g * 4 + h], NEG).astype(np.float32)
            for var in range(nvar):
                t_ = tabA.copy()
                if half == 0:
                    if g == 2 and var < 4:
                        t_[0:128 - 32 * var, :] = NEG
                    elif g != 2 and var == 0:
                        t_[:, :] = NEG
                o = A_OFF[gname + "A"] + (var * 4 + h) * nqb
                ab[:, o:o + nqb] = t_
            o = A_OFF[gname + "B"] + h * nqb
            ab[0:nqb, o:o + nqb] = tabB
    return ab


def make_in_maps(inputs):
    x = np.asarray(inputs["x"], np.float32)
    mem = np.asarray(inputs["mem"], np.float32)
    cm, ident = _consts()
    vecs = np.zeros((8, D), np.float32)
    vecs[0:2] = inputs["hgrn_lb_logits"]
    vecs[2] = inputs["hgrn_norm_g"][0]
    vecs[3] = inputs["ln1_g"][0]
    vecs[4] = inputs["ln1_b"][0]
    vecs[5] = inputs["ln2_g"][0]
    vecs[6] = inputs["ln2_b"][0]
    wr = np.concatenate([inputs["w_router_group"][0], inputs["w_router_expert"][0]], axis=1).astype(np.float32)
    br = np.concatenate([inputs["b_router_group"][0], inputs["b_router_expert"][0]])[None, :].astype(np.float32)

    def elay(w, kc):
        e_, k_, n_ = w.shape
        return np.ascontiguousarray(w.reshape(e_, kc, 128, n_).transpose(0, 2, 1, 3).reshape(e_ * 128, kc * n_), dtype=np.float32)
    shared = {
        "w_in": np.ascontiguousarray(inputs["w_in"][0], np.float32),
        "w_bb": np.ascontiguousarray(inputs["w_branch_b"][0], np.float32),
        "w_ba": np.ascontiguousarray(inputs["w_branch_a"][0], np.float32),
        "w_bc": np.ascontiguousarray(inputs["w_branch_c"][0], np.float32),
        "w_o": np.ascontiguousarray(inputs["w_out"][0], np.float32),
        "w_kv": np.ascontiguousarray(inputs["w_mem_kv"][0], np.float32),
        "w_r": np.ascontiguousarray(wr.reshape(8, 128, 36).transpose(1, 0, 2)),
        "b_r": br,
        "weg": elay(np.asarray(inputs["w_exp_gate"][0]), 8),
        "weu": elay(np.asarray(inputs["w_exp_up"][0]), 8),
        "wed": elay(np.asarray(inputs["w_exp_down"][0]), 2),
        "vecs": vecs, "cmask": cm, "ident": ident,
        "pvec": np.ascontiguousarray(vecs[0:3].reshape(3, 8, 128).transpose(2, 0, 1)),
    }
    rel_bias = np.asarray(inputs["rel_bias"], np.float32)
    abs_ = [_abias(rel_bias, 0), _abias(rel_bias, 1)]
    maps = []
    for core in range(8):
        b, half = core // 2, core % 2
        own = x[b, half * HALF:(half + 1) * HALF]
        ctx = x[b, 0:HALF] if half == 1 else np.zeros((HALF, D), np.float32)
        m = dict(shared)
        m["xT"] = np.ascontiguousarray(np.concatenate([ctx, own], axis=0).T)
        m["xo"] = np.ascontiguousarray(own)
        m["memT"] = np.ascontiguousarray(mem[b].T)
        m["abias"] = abs_[half]
        maps.append(m)
    return maps


def kernel(**inputs):
    nc = build_nc("full")
    maps = make_in_maps(inputs)
    res = run_bass_kernel_spmd(nc, maps, core_ids=list(range(8)))
    outp = np.zeros((4, SEQ, D), np.float32)
    for core in range(8):
        b, half = core // 2, core % 2
        outp[b, half * HALF:(half + 1) * HALF] = res.results[core]["out"]
    return outp
```
